# Optimizing a Trainium2 kernel written in Bass

```python
import math
import jax
import jax.numpy as jnp
from jax import lax
import numpy as np

D_MODEL = 1024
BATCH = 4
SEQ = 4096
DEPTH = 1

NORM_EPS = 1e-6
GMLP_WIDTH = 1024
GMLP_GROUPS = 8
GMLP_GROUP_DIM = GMLP_WIDTH // GMLP_GROUPS
GMLP_CHUNK = 128
ATT_HEADS = 8
HEAD_DIM = 128
ATT_WIDTH = ATT_HEADS * HEAD_DIM
MOBA_BLOCK = 256
MOBA_TOPK = 3
MOBA_QCHUNK = 32
REL_BUCKETS = 32
REL_MAX_DIST = 128
N_GROUPS = 4
EXPERTS_PER_GROUP = 8
N_EXPERTS = N_GROUPS * EXPERTS_PER_GROUP
TOPK_IN_GROUP = 2
D_EXPERT = 256
IN_SPLITS = (GMLP_WIDTH, GMLP_WIDTH, ATT_WIDTH, ATT_WIDTH, ATT_WIDTH, D_MODEL, D_MODEL)
IN_COLS = 2 * GMLP_WIDTH + 3 * ATT_WIDTH + 2 * D_MODEL

kernel_name = "hybrid_gmlp_moba_hmoe_block"


def rmsnorm(x, g):
    xf = x.astype(jnp.float32)
    y = xf * lax.rsqrt(jnp.mean(xf * xf, axis=-1, keepdims=True) + NORM_EPS)
    return (y * g.astype(jnp.float32)).astype(x.dtype)


def layernorm(x, g, b):
    xf = x.astype(jnp.float32)
    mu = jnp.mean(xf, axis=-1, keepdims=True)
    var = jnp.mean(jnp.square(xf - mu), axis=-1, keepdims=True)
    y = (xf - mu) * lax.rsqrt(var + NORM_EPS)
    return (y * g.astype(jnp.float32) + b.astype(jnp.float32)).astype(x.dtype)


def t5_bucket(n):
    n = jnp.maximum(n, 0)
    max_exact = REL_BUCKETS // 2
    nf = jnp.maximum(n, max_exact).astype(jnp.float32)
    large = max_exact + (jnp.log(nf / max_exact) / math.log(REL_MAX_DIST / max_exact)
                         * (REL_BUCKETS - max_exact)).astype(jnp.int32)
    large = jnp.minimum(large, REL_BUCKETS - 1)
    return jnp.where(n < max_exact, n, large)


def gmlp_spatial_gating(u, v, ln_g, ln_b, w_spatial, b_spatial):
    B, S, W = u.shape
    v = layernorm(v, ln_g, ln_b)
    n_chunks = S // GMLP_CHUNK
    vc = v.reshape(B, n_chunks, GMLP_CHUNK, GMLP_GROUPS, GMLP_GROUP_DIM)
    causal = jnp.tril(jnp.ones((GMLP_CHUNK, GMLP_CHUNK), dtype=bool))
    ws = jnp.where(causal[None], w_spatial, 0.0).astype(v.dtype)
    mixed = jnp.einsum('gts,bnsgc->bntgc', ws, vc)
    mixed = mixed + b_spatial.T.astype(v.dtype)[None, None, :, :, None]
    return u * mixed.reshape(B, S, W)


def moba_attention(q, k, v, rel_bias):
    B, H, S, Dh = q.shape
    nb = -(-S // MOBA_BLOCK)
    s_pad = nb * MOBA_BLOCK
    pad = ((0, 0), (0, 0), (0, s_pad - S), (0, 0))
    kb = jnp.pad(k, pad).reshape(B, H, nb, MOBA_BLOCK, Dh)
    vb = jnp.pad(v, pad).reshape(B, H, nb, MOBA_BLOCK, Dh)
    k_mean = jnp.mean(kb.astype(jnp.float32), axis=3)
    pos = jnp.arange(S, dtype=jnp.int32)
    q_blk = pos // MOBA_BLOCK
    gate = jnp.einsum('bhsd,bhnd->bhsn', q.astype(jnp.float32), k_mean)
    fully_past = jnp.arange(nb, dtype=jnp.int32)[None, :] < q_blk[:, None]
    gate = jnp.where(fully_past[None, None], gate, -jnp.inf)
    k_sel = min(MOBA_TOPK, nb)
    _, sel_idx = lax.top_k(gate, k_sel)
    sel_valid = jnp.arange(k_sel, dtype=jnp.int32)[None, :] < q_blk[:, None]
    sel_idx = jnp.where(sel_valid[None, None], sel_idx, 0)

    scale = HEAD_DIM ** -0.5
    offs = jnp.arange(MOBA_BLOCK, dtype=jnp.int32)
    b_ix = jnp.arange(B)[:, None, None, None]
    h_ix = jnp.arange(H)[None, :, None, None]
    h_ix5 = jnp.arange(H)[None, :, None, None, None]
    n_qc = S // MOBA_QCHUNK

    def one_chunk(c):
        q0 = c * MOBA_QCHUNK
        qc = lax.dynamic_slice_in_dim(q, q0, MOBA_QCHUNK, axis=2)
        idx = lax.dynamic_slice_in_dim(sel_idx, q0, MOBA_QCHUNK, axis=2)
        valid = lax.dynamic_slice_in_dim(sel_valid, q0, MOBA_QCHUNK, axis=0)
        qpos = q0 + jnp.arange(MOBA_QCHUNK, dtype=jnp.int32)
        k_past = kb[b_ix, h_ix, idx]
        v_past = vb[b_ix, h_ix, idx]
        s_past = jnp.einsum('bhqd,bhqkjd->bhqkj', qc, k_past).astype(jnp.float32) * scale
        kpos_past = idx[..., None] * MOBA_BLOCK + offs
        bucket_past = t5_bucket(qpos[None, None, :, None, None] - kpos_past)
        s_past = s_past + rel_bias[bucket_past, h_ix5].astype(jnp.float32)
        s_past = jnp.where(valid[None, None, :, :, None], s_past, -jnp.inf)
        blk = q0 // MOBA_BLOCK
        k_own = lax.dynamic_index_in_dim(kb, blk, axis=2, keepdims=False)
        v_own = lax.dynamic_index_in_dim(vb, blk, axis=2, keepdims=False)
        rel = qpos[:, None] - (blk * MOBA_BLOCK + offs)[None, :]
        bias_own = jnp.moveaxis(rel_bias[t5_bucket(rel)], -1, 0).astype(jnp.float32)
        s_own = jnp.einsum('bhqd,bhjd->bhqj', qc, k_own).astype(jnp.float32) * scale + bias_own[None]
        s_own = jnp.where((rel >= 0)[None, None], s_own, -jnp.inf)
        logits = jnp.concatenate([s_past.reshape(B, H, MOBA_QCHUNK, k_sel * MOBA_BLOCK), s_own], axis=-1)
        p = jax.nn.softmax(logits, axis=-1).astype(q.dtype)
        p_past = p[..., :k_sel * MOBA_BLOCK].reshape(B, H, MOBA_QCHUNK, k_sel, MOBA_BLOCK)
        p_own = p[..., k_sel * MOBA_BLOCK:]
        out = (jnp.einsum('bhqkj,bhqkjd->bhqd', p_past, v_past)
               + jnp.einsum('bhqj,bhjd->bhqd', p_own, v_own))
        return out.astype(q.dtype)

    outs = lax.map(one_chunk, jnp.arange(n_qc, dtype=jnp.int32))
    return jnp.transpose(outs, (1, 2, 0, 3, 4)).reshape(B, H, S, Dh)


def hierarchical_moe(xn, w_gr, b_gr, w_er, b_er, w1, w3, w2):
    B, S, D = xn.shape
    xt = xn.reshape(B * S, D)
    g_prob = jax.nn.softmax((xt @ w_gr + b_gr).astype(jnp.float32), axis=-1)
    g_w, g_idx = lax.top_k(g_prob, 1)
    e_logits = (jnp.einsum('td,gde->tge', xt, w_er) + b_er).astype(jnp.float32)
    e_logits = jnp.take_along_axis(e_logits, g_idx[:, :, None], axis=1)[:, 0]
    e_val, e_idx = lax.top_k(e_logits, TOPK_IN_GROUP)
    e_w = jax.nn.softmax(e_val, axis=-1) * g_w
    expert_id = g_idx * EXPERTS_PER_GROUP + e_idx
    combine = jnp.sum(jax.nn.one_hot(expert_id, N_EXPERTS, dtype=jnp.float32) * e_w[..., None],
                      axis=1).astype(xt.dtype)
    y = jnp.zeros_like(xt)
    for g in range(N_GROUPS):
        sl = slice(g * EXPERTS_PER_GROUP, (g + 1) * EXPERTS_PER_GROUP)
        a = jnp.einsum('td,edf->tef', xt, w1[sl])
        b = jnp.einsum('td,edf->tef', xt, w3[sl])
        hid = jax.nn.silu(a) * b * combine[:, sl, None]
        y = y + jnp.einsum('tef,efd->td', hid, w2[sl])
    return y.reshape(B, S, D)


def setup_inputs(seed: int = 0) -> dict:
    key = jax.random.key(seed)
    ks = jax.random.split(key, 20)
    L = DEPTH

    def nrm(k, shape, scale):
        return jax.random.normal(k, shape, jnp.float32) * scale

    return {
        "x": nrm(ks[0], (BATCH, SEQ, D_MODEL), 1.0),
        "norm_mix_g": 1.0 + nrm(ks[1], (L, D_MODEL), 0.02),
        "w_in": nrm(ks[2], (L, D_MODEL, IN_COLS), D_MODEL ** -0.5),
        "b_gates": nrm(ks[3], (L, 2 * D_MODEL), 0.1),
        "gmlp_ln_g": 1.0 + nrm(ks[4], (L, GMLP_WIDTH), 0.02),
        "gmlp_ln_b": nrm(ks[5], (L, GMLP_WIDTH), 0.02),
        "w_spatial": nrm(ks[6], (L, GMLP_GROUPS, GMLP_CHUNK, GMLP_CHUNK), GMLP_CHUNK ** -0.5),
        "b_spatial": 1.0 + nrm(ks[7], (L, GMLP_GROUPS, GMLP_CHUNK), 0.1),
        "rel_bias": nrm(ks[8], (REL_BUCKETS, ATT_HEADS), 0.5),
        "w_out": nrm(ks[9], (L, D_MODEL, D_MODEL), D_MODEL ** -0.5),
        "norm_ffn_g": 1.0 + nrm(ks[10], (L, D_MODEL), 0.02),
        "w_group_router": nrm(ks[11], (L, D_MODEL, N_GROUPS), D_MODEL ** -0.5),
        "b_group_router": nrm(ks[12], (L, N_GROUPS), 0.01),
        "w_expert_router": nrm(ks[13], (L, N_GROUPS, D_MODEL, EXPERTS_PER_GROUP), D_MODEL ** -0.5),
        "b_expert_router": nrm(ks[14], (L, N_GROUPS, EXPERTS_PER_GROUP), 0.01),
        "w1": nrm(ks[15], (L, N_EXPERTS, D_MODEL, D_EXPERT), D_MODEL ** -0.5),
        "w3": nrm(ks[16], (L, N_EXPERTS, D_MODEL, D_EXPERT), D_MODEL ** -0.5),
        "w2": nrm(ks[17], (L, N_EXPERTS, D_EXPERT, D_MODEL), D_EXPERT ** -0.5),
        "norm_final_g": 1.0 + nrm(ks[18], (D_MODEL,), 0.02),
    }


def reference(x, norm_mix_g, w_in, b_gates, gmlp_ln_g, gmlp_ln_b, w_spatial, b_spatial, rel_bias,
              w_out, norm_ffn_g, w_group_router, b_group_router, w_expert_router, b_expert_router,
              w1, w3, w2, norm_final_g):
    B, S, _ = x.shape
    split_at = [int(c) for c in np.cumsum(IN_SPLITS)[:-1]]
    h = x
    for l in range(DEPTH):
        xn = rmsnorm(h, norm_mix_g[l])
        proj = xn @ w_in[l]
        u, v_g, q, k, v_a, gate_a, gate_b = jnp.split(proj, split_at, axis=-1)
        y_a = gmlp_spatial_gating(jax.nn.gelu(u, approximate=False), jax.nn.gelu(v_g, approximate=False),
                                  gmlp_ln_g[l], gmlp_ln_b[l], w_spatial[l], b_spatial[l])
        heads = lambda t: jnp.transpose(t.reshape(B, S, ATT_HEADS, HEAD_DIM), (0, 2, 1, 3))
        y_b = moba_attention(heads(q), heads(k), heads(v_a), rel_bias)
        y_b = jnp.transpose(y_b, (0, 2, 1, 3)).reshape(B, S, ATT_WIDTH)
        gates = jax.nn.sigmoid(jnp.concatenate([gate_a, gate_b], axis=-1) + b_gates[l])
        g_a, g_b = gates[..., :D_MODEL], gates[..., D_MODEL:]
        h = h + (g_a * y_a + g_b * y_b) @ w_out[l]
        xn = rmsnorm(h, norm_ffn_g[l])
        h = h + hierarchical_moe(xn, w_group_router[l], b_group_router[l], w_expert_router[l],
                                 b_expert_router[l], w1[l], w3[l], w2[l])
    return rmsnorm(h, norm_final_g)
```

```python
import math
from contextlib import ExitStack

import numpy as np
import ml_dtypes
import concourse.bass as bass
import concourse.mybir as mybir
from concourse.bass_utils import run_bass_kernel_spmd

F32 = mybir.dt.float32
BF16 = mybir.dt.bfloat16
ALU = mybir.AluOpType
AF = mybir.ActivationFunctionType
AX = mybir.AxisListType

D = 1024
SEQ = 4096
NB = 16
TOWN = 2048
NT_OWN = 16
NT_ALL = 32
EPS = 1e-6
SCALE = 128 ** -0.5
BIGM = 30000.0
BIGG = 1.0e9
NE = 32
NTILE = 64
OOB_ROW = 8192
NSLOT = NTILE * 128
I32 = mybir.dt.int32
DEBUG = False
import os
SKIP = set(os.environ.get('SKIP', '').split(','))


class Sched:
    def __init__(self, nc):
        self.nc = nc
        self.names = ['pe', 'act', 'dve', 'pool', 'sp']
        self.ops = {k: [] for k in self.names}
        self.sem = {k: nc.alloc_semaphore("s_" + k) for k in self.names}
        self.cnt = {k: 0 for k in self.names}
        self.last_w = {}
        self.readers = {}
        self.dsem = {}
        self.dcnt = {}
        self.waited = {k: {} for k in self.names}

    def _semh(self, sk):
        return self.sem[sk] if sk in self.sem else self.dsem[sk]

    def _deps(self, eng, reads, writes):
        need = {}

        def add(sk, v):
            if need.get(sk, 0) < v:
                need[sk] = v
        for k in reads:
            if k in self.last_w:
                add(*self.last_w[k])
        for k in writes:
            if k in self.last_w:
                add(*self.last_w[k])
            for sk, v in self.readers.get(k, {}).items():
                add(sk, v)
        waits = []
        for sk, v in need.items():
            if sk == eng and v > self.cnt[eng]:
                continue
            if self.waited[eng].get(sk, 0) >= v:
                continue
            self.waited[eng][sk] = v
            waits.append((sk, v))
        return waits

    def _record(self, tag, reads, writes):
        for k in reads:
            r = self.readers.setdefault(k, {})
            if r.get(tag[0], 0) < tag[1]:
                r[tag[0]] = tag[1]
        for k in writes:
            self.last_w[k] = tag
            self.readers[k] = {}

    def op(self, eng, fn, reads=(), writes=(), signal=True):
        waits = self._deps(eng, reads, writes)
        if signal:
            self.cnt[eng] += 1
            seq = self.cnt[eng]
        else:
            seq = self.cnt[eng] + 1
        self._record((eng, seq), reads, writes)
        self.ops[eng].append((waits, fn, self.sem[eng] if signal else None, 1))

    def dma(self, q, items, sem):
        if sem not in self.dsem:
            self.dsem[sem] = self.nc.alloc_semaphore("d_" + sem)
            self.dcnt[sem] = 0
        allr, allw = [], []
        for fn, reads, writes in items:
            allr += list(reads)
            allw += list(writes)
        waits = self._deps(q, allr, allw)
        first = True
        for fn, reads, writes in items:
            self.dcnt[sem] += 16
            self.ops[q].append((waits if first else [], fn, self.dsem[sem], 16))
            first = False
        self._record((sem, self.dcnt[sem]), allr, allw)

    def barrier(self):
        for e in self.names:
            waits = []
            for o in self.names:
                if self.cnt[o] > self.waited[e].get(o, 0):
                    self.waited[e][o] = self.cnt[o]
                    waits.append((o, self.cnt[o]))
            for s, c in self.dcnt.items():
                if c > self.waited[e].get(s, 0):
                    self.waited[e][s] = c
                    waits.append((s, c))
            self.ops[e].append((waits, None, None, 0))

    def build(self):
        nc = self.nc
        with nc.Block() as block:
            def mk(name):
                def body(e):
                    for waits, fn, sem, inc in self.ops[name]:
                        for sk, v in waits:
                            e.wait_ge(self._semh(sk), v)
                        if fn is not None:
                            ins = fn(e)
                            if sem is not None:
                                ins.then_inc(sem, inc)
                return body
            block.tensor(mk('pe'))
            block.scalar(mk('act'))
            block.vector(mk('dve'))
            block.gpsimd(mk('pool'))
            block.sync(mk('sp'))


def build_program(debug=False, stop=None):
    nc = bass.Bass("TRN2", target_bir_lowering=False)

    def din(name, shape, dt=F32):
        return nc.dram_tensor(name, list(shape), dt, kind="ExternalInput").ap()

    xa = din("xa", [SEQ, D])
    w_in = din("w_in", [D, 7 * D])
    w_out = din("w_out", [D, D])
    w13r = din("w13r", [NE * 128, 8 * 512])
    w2r = din("w2r", [NE * 128, 2 * D])
    ub_d = din("ub", [128, 256], BF16)
    tri32_d = din("tri32", [32, 32], BF16)
    cst_d = din("cst", [128, 128])
    xs_d = nc.dram_tensor("xs_scr", [NSLOT, D], BF16).ap()
    w13b = nc.dram_tensor("w13b_scr", [NE * 128, 8 * 512], BF16).ap()
    w2b = nc.dram_tensor("w2b_scr", [NE * 128, 2 * D], BF16).ap()
    outs_d = nc.dram_tensor("outs_scr", [NSLOT, D], F32).ap()
    wr = din("wr", [D, 36])
    pn = din("pn", [128, 16 * 16])
    gT = din("gT", [128, 16])
    rowb = din("rowb", [6, 128, D])
    brb = din("brb", [128, 36])
    wsp = din("wsp", [8, 128, 128])
    wspT = din("wspT", [8, 128, 128])
    bspT = din("bspT", [128, 8])
    rb0 = din("rb0", [8, 128, 128])
    rb1 = din("rb1", [8, 128, 128])
    chb = din("chb", [128, 8])
    cmk = din("cmk", [3, 128, 128])
    ident_d = din("ident", [128, 128], BF16)
    e16_d = din("e16", [16, 16 * 128], BF16)
    out = nc.dram_tensor("out", [TOWN, D], F32, kind="ExternalOutput").ap()
    dbg = {}
    if debug:
        dbg['yb'] = nc.dram_tensor("dbg_yb", [128, NT_OWN * D], BF16, kind="ExternalOutput").ap()
        dbg['M'] = nc.dram_tensor("dbg_M", [128, NT_OWN * D], BF16, kind="ExternalOutput").ap()
        dbg['H'] = nc.dram_tensor("dbg_H", [128, NT_OWN * D], F32, kind="ExternalOutput").ap()
        dbg['comb'] = nc.dram_tensor("dbg_comb", [128, NT_OWN * NE], F32, kind="ExternalOutput").ap()

    S = Sched(nc)
    es_all = ExitStack()

    def sb(es, name, shape, dt):
        return es.enter_context(nc.sbuf_tensor("sb_" + name, list(shape), dt)).ap()

    pt = [nc.alloc_psum_tensor("pt%d" % i, [128, 1024], BF16).ap() for i in range(2)]
    ps = [nc.alloc_psum_tensor("ps%d" % i, [128, 512], F32).ap() for i in range(6)]
    PT = [('pt', i) for i in range(2)]
    PS = [('ps', i) for i in range(6)]

    ident = sb(es_all, "ident", [128, 128], BF16)
    e16 = sb(es_all, "e16", [16, 16 * 128], BF16)
    cmk_t = sb(es_all, "cmk", [128, 3, 128], F32)
    gT_t = sb(es_all, "gT", [128, 16], F32)
    chb_t = sb(es_all, "chb", [128, 8], F32)
    pn_t = sb(es_all, "pn", [128, 256], F32)
    st = [sb(es_all, "st%d" % i, [128, 8], F32) for i in range(2)]
    M = sb(es_all, "M", [128, NT_OWN, D], BF16)
    xin = [sb(es_all, "xin%d" % i, [128, D], F32) for i in range(2)]
    junk = sb(es_all, "junk", [128, D], F32)
    zt = junk.bitcast(BF16)[:, 0:D]
    xb = [sb(es_all, "xb%d" % i, [128, D], BF16) for i in range(2)]

    S.dma('sp', [
        (lambda e: e.dma_start(out=ident, in_=ident_d), [], ['ident']),
        (lambda e: e.dma_start(out=e16, in_=e16_d), [], ['e16']),
        (lambda e: e.dma_start(out=cmk_t, in_=cmk.rearrange("a p q -> p a q")), [], ['cmk']),
        (lambda e: e.dma_start(out=gT_t, in_=gT), [], ['gT']),
        (lambda e: e.dma_start(out=chb_t, in_=chb), [], ['chb']),
        (lambda e: e.dma_start(out=pn_t, in_=pn), [], ['pn']),
    ], sem='const')

    def rms_tile(src_fn, src_keys, b, gcol, dstT, dst_key, tok0, q='sp', xb_dst=None, xb_key=None, stage='PQ'):
        xt = src_fn
        xbd = xb[b] if xb_dst is None else xb_dst
        xbk = ('xb', b) if xb_key is None else xb_key
        s_ = st[b]
        sk = ('st', b)
        if 'P' in stage:
            S.op('dve', lambda e: e.scalar_tensor_tensor(out=junk, in0=xt, scalar=1.0, in1=xt, op0=ALU.mult, op1=ALU.mult,
                                                         accum_out=s_[:, 0:1]), reads=src_keys, writes=['junk', sk])
            S.op('dve', lambda e: e.tensor_scalar(out=s_[:, 1:2], in0=s_[:, 0:1], scalar1=1.0 / D, scalar2=EPS,
                                                  op0=ALU.mult, op1=ALU.add), reads=[sk], writes=[sk])
            S.op('act', lambda e: e.activation(out=s_[:, 3:4], in_=s_[:, 1:2], func=AF.Sqrt), reads=[sk], writes=[sk])
            S.op('dve', lambda e: e.reciprocal(out=s_[:, 2:3], in_=s_[:, 3:4]), reads=[sk], writes=[sk])
            S.op('act', lambda e: e.activation(out=xbd, in_=xt, func=AF.Copy, scale=s_[:, 2:3]),
                 reads=src_keys + [sk], writes=[xbk])
        if 'Q' not in stage:
            return
        for kc in range(8):
            S.op('pe', lambda e, kc=kc: e.transpose(out=pt[b][:, kc * 128:(kc + 1) * 128], in_=xbd[:, kc * 128:(kc + 1) * 128],
                                                    identity=ident),
                 reads=[xbk, 'ident'], writes=[PT[b]], signal=(kc == 7))
        S.op('dve', lambda e: e.tensor_tensor(out=dstT[:, :, tok0:tok0 + 128],
                                              in0=pt[b].rearrange("p (k t) -> p k t", k=8),
                                              in1=gT_t[:, gcol:gcol + 8].unsqueeze(2).to_broadcast([128, 8, 128]), op=ALU.mult),
             reads=[PT[b], 'gT'], writes=[dst_key])

    es_x = ExitStack()
    xnT_own = sb(es_x, "xnT_own", [128, 8, TOWN], BF16)
    es_x2 = ExitStack()
    xnT_oth = sb(es_x2, "xnT_oth", [128, 8, TOWN], BF16)

    def xs(kc, tok0, n):
        if tok0 < TOWN:
            return xnT_own[:, kc, tok0:tok0 + n]
        return xnT_oth[:, kc, tok0 - TOWN:tok0 - TOWN + n]
    es_a = ExitStack()
    V4 = sb(es_a, "V4", [128, NT_ALL, 4, 130], BF16)
    Wv4 = sb(es_a, "Wv4", [128, 8, 512], BF16)
    KT = sb(es_a, "KT", [128, SEQ], BF16)
    QT = sb(es_a, "QT", [128, TOWN], BF16)
    Wqk = [sb(es_a, "Wqk%d" % i, [128, 2, 8, 128], BF16) for i in range(2)]
    Tt = [sb(es_a, "Tt%d" % i, [128, 2, 256], F32) for i in range(2)]
    kmsf = sb(es_a, "kmsf", [128, 16], F32)
    kms = sb(es_a, "kms", [128, 16], BF16)
    gm = sb(es_a, "gm", [128, 16, 16], F32)
    ga = sb(es_a, "ga", [128, 16, 16], F32)
    gb_ = sb(es_a, "gb_", [128, 16, 16], F32)
    top8 = sb(es_a, "top8", [128, 16, 8], F32)
    PTs = [sb(es_a, "PTs%d" % i, [128, 512], BF16) for i in range(4)]
    stmp = [sb(es_a, "stmp%d" % i, [128, 256], F32) for i in range(2)]
    rden = sb(es_a, "rden", [128, 4], F32)
    acc_sb = sb(es_a, "acc_sb", [128, 4, 130], F32)

    S.op('pool', lambda e: e.memset(V4[:, :, :, 128:130], 1.0), writes=['V4ones'])
    S.op('pool', lambda e: e.memset(junk.bitcast(BF16), 0.0), writes=['junk'])

    def v4_tile(tt):
        pb = tt % 2
        for kc in range(8):
            S.op('pe', lambda e, kc=kc, tt=tt, pb=pb: e.matmul(ps[pb], lhsT=xs(kc, tt * 128, 128), rhs=Wv4[:, kc, :],
                                                               start=(kc == 0), stop=(kc == 7)),
                 reads=[('xnT', tt), 'Wv4'], writes=[PS[pb]], signal=(kc == 7))
        S.op('dve', lambda e, tt=tt, pb=pb: e.tensor_copy(out=V4[:, tt, :, 0:128], in_=ps[pb].rearrange("p (h c) -> p h c", h=4)),
             reads=[PS[pb]], writes=[('V4', tt)])

    S.dma('pool', [(lambda e: e.dma_start(out=Wv4, in_=w_in[:, 4096:4096 + 512].rearrange("(k p) c -> p k c", p=128)), [], ['Wv4'])], sem='wv')
    def a1(tt, stage):
        b = tt % 2
        if 'P' in stage:
            S.dma('sp', [(lambda e, tt=tt, b=b: e.dma_start(out=xin[b], in_=xa[tt * 128:(tt + 1) * 128, :]), [], [('xin', b)])],
                  sem='xin%d' % b)
        rms_tile(xin[b], [('xin', b)], b, 0, xnT_own if tt < 16 else xnT_oth, ('xnT', tt), (tt % 16) * 128, stage=stage)

    a1(0, 'P')
    for tt in range(NT_ALL):
        if tt + 1 < NT_ALL:
            a1(tt + 1, 'P')
        a1(tt, 'Q')
        if tt >= 1:
            v4_tile(tt - 1)
    v4_tile(NT_ALL - 1)
    all_xnT = [('xnT', tt) for tt in range(NT_ALL)]
    stb = [ps[0], ps[1], pt[0].bitcast(F32), pt[1].bitcast(F32)]
    STK = [PS[0], PS[1], PT[0], PT[1]]
    ptc = 0
    psc = 0
    stc = 0
    gsel = 0
    for hg in range(2):
        c0 = 4096 + hg * 512
        if hg == 1:
            S.dma('pool', [(lambda e, c0=c0: e.dma_start(out=Wv4, in_=w_in[:, c0:c0 + 512].rearrange("(k p) c -> p k c", p=128)),
                            [], ['Wv4'])], sem='wv')
            for tt in range(NT_ALL):
                v4_tile(tt)
        if stop == 'V':
            S.barrier(); S.build(); return nc
        for hh in range(4):
            h = hg * 4 + hh
            wb = h % 2
            cq = 2048 + h * 128
            ck = 3072 + h * 128
            S.dma('pool', [
                (lambda e, cq=cq, wb=wb: e.dma_start(out=Wqk[wb][:, 0], in_=w_in[:, cq:cq + 128].rearrange("(k p) c -> p k c", p=128)), [], [('Wqk', wb)]),
                (lambda e, ck=ck, wb=wb: e.dma_start(out=Wqk[wb][:, 1], in_=w_in[:, ck:ck + 128].rearrange("(k p) c -> p k c", p=128)), [], []),
            ], sem='wqk%d' % wb)
            for ex_ in range(4 * h, 4 * h + 4):
                S.dma('pool', [
                    (lambda e, ex_=ex_: e.dma_start(out=w13b[ex_ * 128:(ex_ + 1) * 128, :], in_=w13r[ex_ * 128:(ex_ + 1) * 128, :]), [], [('wcast', ex_)]),
                    (lambda e, ex_=ex_: e.dma_start(out=w2b[ex_ * 128:(ex_ + 1) * 128, :], in_=w2r[ex_ * 128:(ex_ + 1) * 128, :]), [], []),
                ], sem='wcast')
            S.dma('sp', [(lambda e, a=a: e.dma_start(out=xs_d[a * 128:(a + 1) * 128, :], in_=zt), ['junk'], [('xs_zero', a)])
                         for a in range(8 * h, 8 * h + 8)], sem='xz')
            S.dma('sp', [
                (lambda e, h=h, wb=wb: e.dma_start(out=Tt[wb][:, 0, 0:128], in_=rb0[h]), [], [('Tt', wb)]),
                (lambda e, h=h, wb=wb: e.dma_start(out=Tt[wb][:, 0, 128:256], in_=rb1[h]), [], []),
                (lambda e, h=h, wb=wb: e.dma_start(out=Tt[wb][:, 1, 0:128], in_=rb1[h]), [], []),
            ], sem='tt%d' % wb)
            if 'tt1' not in SKIP:
              S.op('dve', lambda e, wb=wb: e.tensor_tensor(out=Tt[wb][:, 0, 0:128], in0=Tt[wb][:, 0, 0:128], in1=cmk_t[:, 0, :], op=ALU.add),
                 reads=[('Tt', wb), 'cmk'], writes=[('Tt', wb)])
            if 'tt2' not in SKIP:
              S.op('dve', lambda e, wb=wb, h=h: e.tensor_scalar(out=Tt[wb][:, 1, 128:256], in0=cmk_t[:, 1, :], scalar1=0.0, scalar2=chb_t[:, h:h + 1],
                                                            op0=ALU.mult, op1=ALU.add),
                 reads=[('Tt', wb), 'cmk', 'chb'], writes=[('Tt', wb)])
            for tg in range(8):
                pb = tg % 2
                for kc in range(8):
                    S.op('pe', lambda e, kc=kc, tg=tg, pb=pb, wb=wb: e.matmul(ps[pb], lhsT=Wqk[wb][:, 1, kc, :], rhs=xs(kc, tg * 512, 512),
                                                                              start=(kc == 0), stop=(kc == 7)),
                         reads=all_xnT[tg * 4:(tg + 1) * 4] + [('Wqk', wb)], writes=[PS[pb]], signal=(kc == 7))
                for bk in range(2):
                    S.op('act', lambda e, tg=tg, pb=pb, bk=bk: e.activation(out=KT[:, tg * 512 + bk * 256:tg * 512 + (bk + 1) * 256],
                                                                         in_=ps[pb][:, bk * 256:(bk + 1) * 256], func=AF.Copy,
                                                                         accum_out=kmsf[:, 2 * tg + bk:2 * tg + bk + 1]),
                         reads=[PS[pb]], writes=[('KT', tg), 'kmsf'])
            S.op('dve', lambda e: e.tensor_copy(out=kms, in_=kmsf), reads=['kmsf'], writes=['kms'])
            for tg in range(4):
                pb = tg % 2
                for kc in range(8):
                    S.op('pe', lambda e, kc=kc, tg=tg, pb=pb, wb=wb: e.matmul(ps[pb], lhsT=Wqk[wb][:, 0, kc, :], rhs=xs(kc, tg * 512, 512),
                                                                              start=(kc == 0), stop=(kc == 7)),
                         reads=all_xnT[tg * 4:(tg + 1) * 4] + [('Wqk', wb)], writes=[PS[pb]], signal=(kc == 7))
                S.op('dve', lambda e, tg=tg, pb=pb: e.tensor_copy(out=QT[:, tg * 512:(tg + 1) * 512], in_=ps[pb]),
                     reads=[PS[pb]], writes=[('QT', tg)])
            all_QT = [('QT', tg) for tg in range(4)]
            if stop == 'KQ':
                S.barrier(); S.build(); return nc
            for qt in range(16):
                S.op('pe', lambda e, qt=qt: e.matmul(ps[5][:, qt * 16:(qt + 1) * 16], lhsT=QT[:, qt * 128:(qt + 1) * 128], rhs=kms, start=True, stop=True),
                     reads=[('QT', qt // 4), 'kms'], writes=[PS[5]], signal=(qt == 15))
            S.op('dve', lambda e: e.tensor_tensor(out=gm, in0=ps[5][:, 0:256].rearrange("p (a b) -> p a b", a=16),
                                                  in1=pn_t.rearrange("p (a b) -> p a b", a=16), op=ALU.add),
                 reads=[PS[5], 'pn'], writes=['gm'])
            for qt in range(16):
                S.op('dve', lambda e, qt=qt: e.max(out=top8[:, qt, :], in_=gm[:, qt, :]), reads=['gm'], writes=['top8'])
            S.op('dve', lambda e: e.tensor_tensor(out=ga, in0=gm, in1=top8[:, :, 2:3].to_broadcast([128, 16, 16]), op=ALU.is_ge),
                 reads=['gm', 'top8'], writes=['ga'])
            S.op('dve', lambda e: e.tensor_scalar(out=gb_, in0=gm, scalar1=-0.5 * BIGG, scalar2=None, op0=ALU.is_gt),
                 reads=['gm'], writes=['gb_'])
            S.op('dve', lambda e: e.tensor_tensor(out=ga, in0=ga, in1=gb_, op=ALU.mult), reads=['ga', 'gb_'], writes=['ga'])
            if stop == 'SEL':
                S.barrier(); S.build(); return nc
            for pr in range(4):
                i0, i1 = 2 * pr, 2 * pr + 1
                adj0 = i0 - 1 if i0 >= 1 else 15
                groups = []
                for s_ in [x for x in range(i0)] + [x for x in range(8, 16)]:
                    us = []
                    for kt in range(2):
                        sp = [('T1', 0, 256)] if (s_ == adj0 and kt == 1) else []
                        us.append(dict(q0=i0 * 256, n=512, ktile=s_ * 2 + kt, special=sp, accs=[(0, 0), (1, 128), (2, 256), (3, 384)]))
                    groups.append(dict(units=us, sel=s_, accs=[0, 1, 2, 3]))
                groups.append(dict(units=[
                    dict(q0=i1 * 256, n=256, ktile=i0 * 2, special=[], accs=[(2, 0), (3, 128)]),
                    dict(q0=i1 * 256, n=256, ktile=i0 * 2 + 1, special=[('T1', 0, 256)], accs=[(2, 0), (3, 128)])], sel=i0, accs=[2, 3]))
                for ii, ab in ((i0, 0), (i1, 2)):
                    groups.append(dict(units=[
                        dict(q0=ii * 256, n=256, ktile=ii * 2, special=[('T0', 0, 256)], accs=[(ab, 0), (ab + 1, 128)]),
                        dict(q0=ii * 256 + 128, n=128, ktile=ii * 2 + 1, special=[('T0', 0, 128)], accs=[(ab + 1, 0)])], sel=None, accs=[ab, ab + 1]))
                units = []
                for gi, g_ in enumerate(groups):
                    g_['bs'] = gsel % 2
                    gsel += 1
                    for u in g_['units']:
                        u['g'] = g_
                        units.append(u)
                    g_['last'] = units[-1]
                    g_['banks_started'] = set()
                    g_['lastmm'] = {}
                    for u in g_['units']:
                        for a_, _ in u['accs']:
                            g_['lastmm'][a_] = id(u)
                inited = set()

                def pbank(g_, a_):
                    return 2 + 2 * g_['bs'] + a_ // 2

                def pacc(g_, a_):
                    return ps[pbank(g_, a_)][:, (a_ % 2) * 256:(a_ % 2) * 256 + 129]

                def stage1(u):
                    pb = u['pb']
                    n, q0, kt_ = u['n'], u['q0'], u['ktile']
                    qkeys = [('QT', (q0 + c) // 512) for c in range(0, n, 256)] if n >= 256 else [('QT', q0 // 512)]
                    S.op('pe', lambda e, pb=pb, n=n, q0=q0, kt_=kt_: e.matmul(
                        stb[pb][:, 0:n], lhsT=KT[:, kt_ * 128:(kt_ + 1) * 128], rhs=QT[:, q0:q0 + n], start=True, stop=True),
                        reads=[('KT', kt_ // 4)] + qkeys, writes=[STK[pb]], signal=True)

                def stage2(u):
                    pb, pj, n = u['pb'], u['pj'], u['n']
                    c_done = 0
                    for (tk, c0_, c1_) in u['special']:
                        sj = u['sj']
                        tsel = 0 if tk == 'T0' else 1
                        S.op('dve', lambda e, pb=pb, sj=sj, c0_=c0_, c1_=c1_, tsel=tsel, wb=wb: e.scalar_tensor_tensor(
                            out=stmp[sj][:, c0_:c1_], in0=stb[pb][:, c0_:c1_], scalar=SCALE, in1=Tt[wb][:, tsel, 0:c1_ - c0_], op0=ALU.mult, op1=ALU.add),
                            reads=[STK[pb], ('Tt', wb)], writes=[('stmp', sj)])
                        S.op('act', lambda e, sj=sj, pj=pj, c0_=c0_, c1_=c1_: e.activation(out=PTs[pj][:, c0_:c1_], in_=stmp[sj][:, c0_:c1_], func=AF.Exp),
                             reads=[('stmp', sj)], writes=[('PTs', pj)])
                        c_done = c1_
                    if c_done < n:
                        S.op('act', lambda e, pb=pb, pj=pj, c_done=c_done, n=n, h=h: e.activation(
                            out=PTs[pj][:, c_done:n], in_=stb[pb][:, c_done:n], func=AF.Exp, bias=chb_t[:, h:h + 1], scale=SCALE),
                            reads=[STK[pb], 'chb'], writes=[('PTs', pj)])

                def stage3(g_):
                    for a_ in g_['accs']:
                        bk = pbank(g_, a_)
                        ua = [(u, off) for u in g_['units'] for (aa, off) in u['accs'] if aa == a_]
                        for k_, (u, off) in enumerate(ua):
                            pj, kt_ = u['pj'], u['ktile']
                            S.op('pe', lambda e, g_=g_, a_=a_, off=off, pj=pj, kt_=kt_, st_=(k_ == 0), sp_=(k_ == len(ua) - 1), hh=hh: e.matmul(
                                pacc(g_, a_), lhsT=PTs[pj][:, off:off + 128], rhs=V4[:, kt_, hh, 0:129], start=st_, stop=sp_),
                                reads=[('PTs', pj), ('V4', kt_), 'V4ones'], writes=[PS[bk]], signal=(k_ == len(ua) - 1))
                    for a_ in g_['accs']:
                        qt = i0 * 2 + a_
                        bk = pbank(g_, a_)
                        dst = acc_sb[:, a_, 0:129]
                        if g_['sel'] is None:
                            S.op('dve', lambda e, g_=g_, a_=a_, dst=dst: e.tensor_tensor(out=dst, in0=pacc(g_, a_), in1=dst, op=ALU.add),
                                 reads=[PS[bk], ('acc_sb', a_)], writes=[('acc_sb', a_)])
                        elif a_ not in inited:
                            S.op('dve', lambda e, g_=g_, a_=a_, dst=dst, qt=qt: e.tensor_scalar(
                                out=dst, in0=pacc(g_, a_), scalar1=ga[:, qt, g_['sel']:g_['sel'] + 1], scalar2=None, op0=ALU.mult),
                                reads=[PS[bk], 'ga'], writes=[('acc_sb', a_)])
                        else:
                            S.op('dve', lambda e, g_=g_, a_=a_, dst=dst, qt=qt: e.scalar_tensor_tensor(
                                out=dst, in0=pacc(g_, a_), scalar=ga[:, qt, g_['sel']:g_['sel'] + 1], in1=dst, op0=ALU.mult, op1=ALU.add),
                                reads=[PS[bk], 'ga', ('acc_sb', a_)], writes=[('acc_sb', a_)])
                        inited.add(a_)

                def s12(g_):
                    nonlocal_counters = None
                    for u in g_['units']:
                        stage1(u)
                        stage2(u)

                for g_ in groups:
                    for u in g_['units']:
                        u['pb'] = psc % 4
                        psc += 1
                        u['pj'] = ptc % 4
                        ptc += 1
                        if u['special']:
                            u['sj'] = stc % 2
                            stc += 1
                s12(groups[0])
                for gi, g_ in enumerate(groups):
                    if gi + 1 < len(groups):
                        s12(groups[gi + 1])
                    stage3(g_)
                for a_ in range(4):
                    tl = i0 * 2 + a_
                    S.op('dve', lambda e, a_=a_: e.reciprocal(out=rden[:, a_:a_ + 1], in_=acc_sb[:, a_, 128:129]),
                         reads=[('acc_sb', a_)], writes=['rden'])
                    S.op('dve', lambda e, a_=a_, tl=tl, h=h: e.tensor_scalar(out=M[:, tl, h * 128:(h + 1) * 128], in0=acc_sb[:, a_, 0:128],
                                                                            scalar1=rden[:, a_:a_ + 1], scalar2=None, op0=ALU.mult),
                         reads=[('acc_sb', a_), 'rden'], writes=[('M', tl)])
    S.barrier()
    if debug:
        S.dma('sp', [(lambda e: e.dma_start(out=dbg['yb'], in_=M.rearrange("p t d -> p (t d)")), [('M', t) for t in range(16)], [])], sem='dbg')
        S.barrier()
    es_a.close()
    es_x2.close()
    if stop == 'ATT':
        S.barrier(); S.build(); return nc

    es_g = ExitStack()
    Wseg = [sb(es_g, "Wseg%d" % i, [128, 8, D], BF16) for i in range(2)]
    rows = sb(es_g, "rows", [128, 4, D], F32)
    lnbt = sb(es_g, "lnbt", [128, D], F32)
    wtmp = sb(es_g, "wtmp", [128, 8, 128], F32)
    wsT = sb(es_g, "wsT", [128, 8, 128], BF16)
    rs = sb(es_g, "rs", [128, 8], F32)
    bsp_t = sb(es_g, "bsp", [128, 8], F32)
    vg2 = [sb(es_g, "vg%d" % i, [128, D], F32) for i in range(2)]
    vhat2 = [sb(es_g, "vhat%d" % i, [128, D], BF16) for i in range(2)]
    sm2 = [sb(es_g, "sm%d" % i, [128, 8], F32) for i in range(2)]
    tmpa = [sb(es_g, "tmpa%d" % i, [128, 512], F32) for i in range(2)]
    tmpb = [sb(es_g, "tmpb%d" % i, [128, 512], F32) for i in range(2)]
    M2 = sb(es_g, "M2", [128, NT_OWN, D], BF16)

    S.dma('sp', [
        (lambda e: e.dma_start(out=rows[:, 0, :], in_=rowb[0]), [], ['rows']),
        (lambda e: e.dma_start(out=lnbt, in_=rowb[1]), [], ['lnbt']),
        (lambda e: e.dma_start(out=rows[:, 2, :], in_=rowb[2]), [], []),
        (lambda e: e.dma_start(out=rows[:, 3, :], in_=rowb[3]), [], []),
        (lambda e: e.dma_start(out=bsp_t, in_=bspT), [], ['bsp']),
        (lambda e: e.dma_start(out=wtmp, in_=wsp.rearrange("g t s -> t g s")), [], ['wtmp']),
    ], sem='gconst')
    S.op('dve', lambda e: e.tensor_tensor(out=wtmp, in0=wtmp, in1=cmk_t[:, 1:2, :].to_broadcast([128, 8, 128]), op=ALU.mult),
         reads=['wtmp', 'cmk'], writes=['wtmp'])
    S.op('dve', lambda e: e.reduce_sum(out=rs, in_=wtmp, axis=AX.X), reads=['wtmp'], writes=['rs'])
    for g in range(8):
        S.op('dve', lambda e, g=g: e.tensor_scalar(out=rows[:, 1, g * 128:(g + 1) * 128], in0=lnbt[:, g * 128:(g + 1) * 128],
                                                  scalar1=rs[:, g:g + 1], scalar2=bsp_t[:, g:g + 1], op0=ALU.mult, op1=ALU.add),
             reads=['lnbt', 'rs', 'bsp', 'rows'], writes=['rows'])
    S.dma('sp', [(lambda e: e.dma_start(out=wtmp, in_=wspT.rearrange("g s t -> s g t")), [], ['wtmp'])], sem='gconst')
    S.op('dve', lambda e: e.tensor_tensor(out=wsT, in0=wtmp, in1=cmk_t[:, 2:3, :].to_broadcast([128, 8, 128]), op=ALU.mult),
         reads=['wtmp', 'cmk'], writes=['wsT'])

    seg_cols = {'v': 1024, 'u': 0, 'ga': 5120, 'gb': 6144}
    pend_mix = None
    for si, seg in enumerate(['v', 'u', 'ga', 'gb']):
        wbuf = si % 2
        c0 = seg_cols[seg]
        S.dma('pool', [(lambda e, c0=c0, wbuf=wbuf: e.dma_start(out=Wseg[wbuf], in_=w_in[:, c0:c0 + D].rearrange("(k p) c -> p k c", p=128)),
                        [], [('Wseg', wbuf)])], sem='wseg%d' % wbuf)
        for tl in range(NT_OWN):
            pp = (tl % 2) * 2 if seg == 'v' else (tl % 3) * 2
            for half in range(2):
                pb = pp + half
                for kc in range(8):
                    S.op('pe', lambda e, kc=kc, tl=tl, pb=pb, half=half, wbuf=wbuf: e.matmul(
                        ps[pb], lhsT=xs(kc, tl * 128, 128), rhs=Wseg[wbuf][:, kc, half * 512:(half + 1) * 512],
                        start=(kc == 0), stop=(kc == 7)),
                        reads=[('xnT', tl), ('Wseg', wbuf)], writes=[PS[pb]], signal=(kc == 7))
            if seg == 'v':
                vb = tl % 2
                vg, vhat, sm = vg2[vb], vhat2[vb], sm2[vb]
                kvg, kvh, ksm = ('vg', vb), ('vhat', vb), ('sm', vb)
                for half in range(2):
                    pb = pp + half
                    S.op('act', lambda e, half=half, pb=pb, vg=vg, sm=sm: e.activation(out=vg[:, half * 512:(half + 1) * 512], in_=ps[pb], func=AF.Gelu,
                                                                                     accum_out=sm[:, half:half + 1]),
                         reads=[PS[pb]], writes=[kvg, ksm])
                S.op('dve', lambda e, vg=vg, sm=sm: e.scalar_tensor_tensor(out=junk, in0=vg, scalar=1.0, in1=vg, op0=ALU.mult, op1=ALU.mult, accum_out=sm[:, 2:3]),
                     reads=[kvg], writes=['junk', ksm])
                S.op('dve', lambda e, sm=sm: e.tensor_tensor(out=sm[:, 3:4], in0=sm[:, 0:1], in1=sm[:, 1:2], op=ALU.add), reads=[ksm], writes=[ksm])
                S.op('dve', lambda e, sm=sm: e.tensor_scalar(out=sm[:, 3:4], in0=sm[:, 3:4], scalar1=1.0 / D, scalar2=None, op0=ALU.mult), reads=[ksm], writes=[ksm])
                S.op('dve', lambda e, sm=sm: e.tensor_tensor(out=sm[:, 4:5], in0=sm[:, 3:4], in1=sm[:, 3:4], op=ALU.mult), reads=[ksm], writes=[ksm])
                S.op('dve', lambda e, sm=sm: e.scalar_tensor_tensor(out=sm[:, 5:6], in0=sm[:, 2:3], scalar=1.0 / D, in1=sm[:, 4:5], op0=ALU.mult, op1=ALU.subtract),
                     reads=[ksm], writes=[ksm])
                S.op('dve', lambda e, sm=sm: e.tensor_scalar(out=sm[:, 5:6], in0=sm[:, 5:6], scalar1=EPS, scalar2=None, op0=ALU.add), reads=[ksm], writes=[ksm])
                S.op('act', lambda e, sm=sm: e.activation(out=sm[:, 6:7], in_=sm[:, 5:6], func=AF.Sqrt), reads=[ksm], writes=[ksm])
                S.op('dve', lambda e, sm=sm: e.reciprocal(out=sm[:, 7:8], in_=sm[:, 6:7]), reads=[ksm], writes=[ksm])
                S.op('dve', lambda e, vg=vg, vhat=vhat, sm=sm: e.tensor_scalar(out=vhat, in0=vg, scalar1=sm[:, 3:4], scalar2=sm[:, 7:8], op0=ALU.subtract, op1=ALU.mult),
                     reads=[kvg, ksm], writes=[kvh])

                def mix(tl=tl, vhat=vhat, kvh=kvh):
                    for g in range(8):
                        S.op('pe', lambda e, g=g: e.matmul(ps[4 + g // 4][:, (g % 4) * 128:(g % 4 + 1) * 128], lhsT=wsT[:, g, :], rhs=vhat[:, g * 128:(g + 1) * 128],
                                                          start=True, stop=True),
                             reads=['wsT', kvh], writes=[PS[4 + g // 4]], signal=(g % 4 == 3))
                    for half in range(2):
                        S.op('dve', lambda e, half=half: e.tensor_tensor(out=tmpa[half], in0=ps[4 + half], in1=rows[:, 0, half * 512:(half + 1) * 512], op=ALU.mult),
                             reads=[PS[4 + half], 'rows'], writes=[('tmpa', half)])
                        S.op('dve', lambda e, half=half: e.tensor_tensor(out=M2[:, tl, half * 512:(half + 1) * 512], in0=tmpa[half],
                                                                        in1=rows[:, 1, half * 512:(half + 1) * 512], op=ALU.add),
                             reads=[('tmpa', half), 'rows'], writes=[('M2', tl)])
                if pend_mix is not None:
                    pend_mix()
                pend_mix = mix
                if tl == NT_OWN - 1:
                    pend_mix()
                    pend_mix = None
            elif seg == 'u':
                for half in range(2):
                    pb = pp + half
                    S.op('act', lambda e, half=half, pb=pb: e.activation(out=tmpa[half], in_=ps[pb], func=AF.Gelu),
                         reads=[PS[pb]], writes=[('tmpa', half)])
                    S.op('dve', lambda e, half=half, tl=tl: e.tensor_tensor(out=M2[:, tl, half * 512:(half + 1) * 512], in0=M2[:, tl, half * 512:(half + 1) * 512],
                                                                            in1=tmpa[half], op=ALU.mult),
                         reads=[('tmpa', half), ('M2', tl)], writes=[('M2', tl)])
            elif seg == 'ga':
                for half in range(2):
                    pb = pp + half
                    S.op('dve', lambda e, half=half, pb=pb: e.tensor_tensor(out=tmpa[half], in0=ps[pb], in1=rows[:, 2, half * 512:(half + 1) * 512], op=ALU.add),
                         reads=[PS[pb], 'rows'], writes=[('tmpa', half)])
                    S.op('act', lambda e, half=half: e.activation(out=tmpb[half], in_=tmpa[half], func=AF.Sigmoid),
                         reads=[('tmpa', half)], writes=[('tmpb', half)])
                    S.op('dve', lambda e, half=half, tl=tl: e.tensor_tensor(out=M2[:, tl, half * 512:(half + 1) * 512], in0=M2[:, tl, half * 512:(half + 1) * 512],
                                                                            in1=tmpb[half], op=ALU.mult),
                         reads=[('tmpb', half), ('M2', tl)], writes=[('M2', tl)])
            else:
                for half in range(2):
                    pb = pp + half
                    S.op('dve', lambda e, half=half, pb=pb: e.tensor_tensor(out=tmpa[half], in0=ps[pb], in1=rows[:, 3, half * 512:(half + 1) * 512], op=ALU.add),
                         reads=[PS[pb], 'rows'], writes=[('tmpa', half)])
                    S.op('act', lambda e, half=half: e.activation(out=tmpb[half], in_=tmpa[half], func=AF.Sigmoid),
                         reads=[('tmpa', half)], writes=[('tmpb', half)])
                    S.op('dve', lambda e, half=half, tl=tl: e.tensor_tensor(out=tmpb[half], in0=tmpb[half], in1=M[:, tl, half * 512:(half + 1) * 512], op=ALU.mult),
                         reads=[('tmpb', half), ('M', tl)], writes=[('tmpb', half)])
                    S.op('dve', lambda e, half=half, tl=tl: e.tensor_tensor(out=M[:, tl, half * 512:(half + 1) * 512], in0=tmpb[half],
                                                                            in1=M2[:, tl, half * 512:(half + 1) * 512], op=ALU.add),
                         reads=[('tmpb', half), ('M2', tl)], writes=[('M', tl)])
    S.barrier()
    if debug:
        S.dma('sp', [(lambda e: e.dma_start(out=dbg['M'], in_=M.rearrange("p t d -> p (t d)")), [('M', t) for t in range(16)], [])], sem='dbg')
        S.barrier()
    es_g.close()
    es_x.close()
    if stop == 'G':
        S.barrier(); S.build(); return nc

    es_h = ExitStack()
    H = sb(es_h, "H", [128, NT_OWN, D], F32)
    COMB = sb(es_h, "COMB", [128, NT_OWN, NE], F32)
    gf_t = sb(es_h, "gf", [128, D], F32)
    MselF = sb(es_h, "MselF", [128, NT_OWN, NE], F32)
    MselB = sb(es_h, "MselB", [128, NT_OWN, NE], BF16)
    RANK = sb(es_h, "RANK", [128, NT_OWN, NE], F32)
    POSI = sb(es_h, "POSI", [128, NT_OWN, 2], I32)
    CW2 = sb(es_h, "CW2", [128, NT_OWN, 2], F32)
    IDXW = sb(es_h, "IDXW", [128, NTILE], I32)
    cst_t = sb(es_h, "cst", [128, 128], F32)
    es_o = ExitStack()
    xn2T = sb(es_o, "xn2T", [128, 8, TOWN], BF16)
    ub_t = sb(es_o, "ub", [128, 256], BF16)
    tri32_t = sb(es_o, "tri32", [32, 32], BF16)
    cnt = sb(es_o, "cnt", [128, NE], F32)
    tle = sb(es_o, "tle", [128, NE], F32)
    tlb = sb(es_o, "tlb", [128, NE], BF16)
    tT = sb(es_o, "tT", [32, 128], BF16)
    start = sb(es_o, "start", [128, NE], F32)
    start128 = sb(es_o, "start128", [128, NE], F32)
    ej = sb(es_o, "ej", [128, NTILE], F32)
    ej2 = sb(es_o, "ej2", [128, NTILE], F32)
    tmp32 = sb(es_o, "tmp32", [128, NE], F32)
    p8 = sb(es_o, "p8", [128, 8], F32)
    Wout = sb(es_o, "Wout", [128, 8, D], BF16)
    MT = [sb(es_o, "MT%d" % i, [128, 8, 128], BF16) for i in range(2)]
    Wr = sb(es_o, "Wr", [128, 8, 36], BF16)
    brb_t = sb(es_o, "brb", [128, 36], F32)
    lg2 = [sb(es_o, "lg%d" % i, [128, 36], F32) for i in range(2)]
    r_2 = [sb(es_o, "r_%d" % i, [128, 8], F32) for i in range(2)]
    oh2 = [sb(es_o, "oh%d" % i, [128, 4], F32) for i in range(2)]
    ge2 = [sb(es_o, "ge%d" % i, [128, 4], F32) for i in range(2)]
    elm2 = [sb(es_o, "elm%d" % i, [128, 32], F32) for i in range(2)]
    t82 = [sb(es_o, "t8%d" % i, [128, 8], F32) for i in range(2)]
    pe_2 = [sb(es_o, "pe_%d" % i, [128, 32], F32) for i in range(2)]
    pm2 = [sb(es_o, "pm%d" % i, [128, 32], F32) for i in range(2)]

    S.dma('pool', [
        (lambda e: e.dma_start(out=Wout, in_=w_out.rearrange("(k p) c -> p k c", p=128)), [], ['Wout']),
        (lambda e: e.dma_start(out=Wr, in_=wr.rearrange("(k p) c -> p k c", p=128)), [], ['Wr']),
    ], sem='wout')
    S.dma('sp', [
        (lambda e: e.dma_start(out=brb_t, in_=brb), [], ['brb']),
        (lambda e: e.dma_start(out=gf_t, in_=rowb[4]), [], ['gf']),
        (lambda e: e.dma_start(out=ub_t, in_=ub_d), [], ['ub']),
        (lambda e: e.dma_start(out=tri32_t, in_=tri32_d), [], ['tri32']),
        (lambda e: e.dma_start(out=cst_t, in_=cst_d), [], ['cst']),
    ], sem='oconst')
    def o_T1(tl):
        b = tl % 2
        pp = (tl % 2) * 2
        for kc in range(8):
            S.op('pe', lambda e, kc=kc, tl=tl, b=b: e.transpose(out=pt[b][:, kc * 128:(kc + 1) * 128], in_=M[:, tl, kc * 128:(kc + 1) * 128], identity=ident),
                 reads=[('M', tl), 'ident'], writes=[PT[b]], signal=(kc == 7))
        S.op('act', lambda e, b=b: e.activation(out=MT[b], in_=pt[b].rearrange("p (k t) -> p k t", k=8), func=AF.Copy),
             reads=[PT[b]], writes=[('MT', b)])
        S.dma('sp', [(lambda e, tl=tl, b=b: e.dma_start(out=xin[b], in_=xa[tl * 128:(tl + 1) * 128, :]), [], [('xin', b)])], sem='xin%d' % b)
        for half in range(2):
            pb = pp + half
            for kc in range(8):
                S.op('pe', lambda e, kc=kc, b=b, pb=pb, half=half: e.matmul(ps[pb], lhsT=MT[b][:, kc, :], rhs=Wout[:, kc, half * 512:(half + 1) * 512],
                                                                            start=(kc == 0), stop=(kc == 7)),
                     reads=[('MT', b), 'Wout'], writes=[PS[pb]], signal=(kc == 7))
            S.op('dve', lambda e, tl=tl, half=half, pb=pb, b=b: e.tensor_tensor(out=H[:, tl, half * 512:(half + 1) * 512], in0=ps[pb],
                                                                                 in1=xin[b][:, half * 512:(half + 1) * 512], op=ALU.add),
                 reads=[PS[pb], ('xin', b)], writes=[('H', tl)])
    def o_T23(tl):
        b = tl % 2
        lg = lg2[tl % 2]
        r_ = r_2[tl % 2]
        oh = oh2[tl % 2]
        ge = ge2[tl % 2]
        elm = elm2[tl % 2]
        t8 = t82[tl % 2]
        pe_ = pe_2[tl % 2]
        pm = pm2[tl % 2]
        rms_tile(H[:, tl, :], [('H', tl)], b, 8, xn2T, ('xn2T', tl), tl * 128, xb_dst=M[:, tl, :], xb_key=('M', tl))
        for kc in range(8):
            S.op('pe', lambda e, kc=kc, tl=tl: e.matmul(ps[4][:, 0:36], lhsT=xn2T[:, kc, tl * 128:(tl + 1) * 128], rhs=Wr[:, kc, :],
                                                        start=(kc == 0), stop=(kc == 7)),
                 reads=[('xn2T', tl), 'Wr'], writes=[PS[4]], signal=(kc == 7))
        S.op('dve', lambda e: e.tensor_tensor(out=lg, in0=ps[4][:, 0:36], in1=brb_t, op=ALU.add), reads=[PS[4], 'brb'], writes=[('lg', tl % 2)])
        S.op('dve', lambda e: e.reduce_max(out=r_[:, 0:1], in_=lg[:, 0:4], axis=AX.X), reads=[('lg', tl % 2)], writes=[('r_', tl % 2)])
        S.op('dve', lambda e: e.tensor_scalar(out=r_[:, 1:2], in0=r_[:, 0:1], scalar1=-1.0, scalar2=None, op0=ALU.mult), reads=[('r_', tl % 2)], writes=[('r_', tl % 2)])
        S.op('act', lambda e: e.activation(out=ge, in_=lg[:, 0:4], func=AF.Exp, bias=r_[:, 1:2], accum_out=r_[:, 2:3]),
             reads=[('lg', tl % 2), ('r_', tl % 2)], writes=[('ge', tl % 2), ('r_', tl % 2)])
        S.op('dve', lambda e: e.tensor_scalar(out=oh, in0=lg[:, 0:4], scalar1=r_[:, 0:1], scalar2=None, op0=ALU.is_ge), reads=[('lg', tl % 2), ('r_', tl % 2)], writes=[('oh', tl % 2)])
        S.op('dve', lambda e: e.tensor_scalar(out=oh, in0=oh, scalar1=BIGG, scalar2=-BIGG, op0=ALU.mult, op1=ALU.add), reads=[('oh', tl % 2)], writes=[('oh', tl % 2)])
        S.op('dve', lambda e: e.tensor_tensor(out=elm.rearrange("p (g x) -> p g x", g=4), in0=lg[:, 4:36].rearrange("p (g x) -> p g x", g=4),
                                              in1=oh.unsqueeze(2).to_broadcast([128, 4, 8]), op=ALU.add),
             reads=[('lg', tl % 2), ('oh', tl % 2)], writes=[('elm', tl % 2)])
        S.op('dve', lambda e: e.max(out=t8, in_=elm), reads=[('elm', tl % 2)], writes=[('t8', tl % 2)])
        S.op('dve', lambda e: e.tensor_scalar(out=r_[:, 6:7], in0=t8[:, 0:1], scalar1=-1.0, scalar2=None, op0=ALU.mult), reads=[('t8', tl % 2), ('r_', tl % 2)], writes=[('r_', tl % 2)])
        S.op('act', lambda e: e.activation(out=pe_, in_=elm, func=AF.Exp, bias=r_[:, 6:7]), reads=[('elm', tl % 2), ('r_', tl % 2)], writes=[('pe_', tl % 2)])
        S.op('dve', lambda e: e.scalar_tensor_tensor(out=pm, in0=elm, scalar=t8[:, 1:2], in1=pe_, op0=ALU.is_ge, op1=ALU.mult, accum_out=r_[:, 3:4]),
             reads=[('elm', tl % 2), ('t8', tl % 2), ('pe_', tl % 2), ('r_', tl % 2)], writes=[('pm', tl % 2), ('r_', tl % 2)])
        S.op('dve', lambda e: e.tensor_tensor(out=r_[:, 4:5], in0=r_[:, 3:4], in1=r_[:, 2:3], op=ALU.mult), reads=[('r_', tl % 2)], writes=[('r_', tl % 2)])
        S.op('dve', lambda e: e.reciprocal(out=r_[:, 5:6], in_=r_[:, 4:5]), reads=[('r_', tl % 2)], writes=[('r_', tl % 2)])
        S.op('dve', lambda e, tl=tl: e.tensor_scalar(out=COMB[:, tl, :], in0=pm, scalar1=r_[:, 5:6], scalar2=None, op0=ALU.mult),
             reads=[('pm', tl % 2), ('r_', tl % 2)], writes=[('COMB', tl)])
        S.op('dve', lambda e, tl=tl: e.tensor_scalar(out=MselF[:, tl, :], in0=elm, scalar1=t8[:, 1:2], scalar2=None, op0=ALU.is_ge),
             reads=[('elm', tl % 2), ('t8', tl % 2)], writes=[('MselF', tl)])
        S.op('dve', lambda e, tl=tl: e.tensor_copy(out=MselB[:, tl, :], in_=MselF[:, tl, :]), reads=[('MselF', tl)], writes=[('MselB', tl)])
    o_T1(0)
    for tl in range(NT_OWN):
        if tl + 1 < NT_OWN:
            o_T1(tl + 1)
        o_T23(tl)
    allMB = [('MselB', t) for t in range(NT_OWN)]
    for tl in range(NT_OWN):
        pb = 4 + tl % 2
        for j in range(tl):
            S.op('pe', lambda e, j=j, pb=pb: e.matmul(ps[pb][:, 0:NE], lhsT=ub_t[:, 128:256], rhs=MselB[:, j, :], start=(j == 0), stop=False),
                 reads=['ub', ('MselB', j)], writes=[PS[pb]], signal=False)
        S.op('pe', lambda e, tl=tl, pb=pb: e.matmul(ps[pb][:, 0:NE], lhsT=ub_t[:, 0:128], rhs=MselB[:, tl, :], start=(tl == 0), stop=True),
             reads=['ub', ('MselB', tl)], writes=[PS[pb]])
        S.op('dve', lambda e, tl=tl, pb=pb: e.tensor_copy(out=RANK[:, tl, :], in_=ps[pb][:, 0:NE]), reads=[PS[pb]], writes=[('RANK', tl)])
    for j in range(NT_OWN):
        S.op('pe', lambda e, j=j: e.matmul(ps[0][:, 0:NE], lhsT=ub_t[:, 128:256], rhs=MselB[:, j, :], start=(j == 0), stop=(j == NT_OWN - 1)),
             reads=['ub', ('MselB', j)], writes=[PS[0]], signal=(j == NT_OWN - 1))
    S.op('dve', lambda e: e.tensor_copy(out=cnt, in_=ps[0][:, 0:NE]), reads=[PS[0]], writes=['cnt'])
    S.op('dve', lambda e: e.memset(tle, 0.0), writes=['tle'])
    for m in range(16):
        S.op('dve', lambda e, m=m: e.scalar_tensor_tensor(out=tle, in0=cnt, scalar=float(128 * m), in1=tle, op0=ALU.is_gt, op1=ALU.add),
             reads=['cnt', 'tle'], writes=['tle'])
    S.op('dve', lambda e: e.tensor_copy(out=tlb, in_=tle), reads=['tle'], writes=['tlb'])
    S.op('pe', lambda e: e.transpose(out=pt[0][0:32, 0:128], in_=tlb, identity=ident), reads=['tlb', 'ident'], writes=[PT[0]])
    S.op('dve', lambda e: e.tensor_copy(out=tT, in_=pt[0][0:32, 0:128]), reads=[PT[0]], writes=['tT'])
    S.op('pe', lambda e: e.matmul(ps[1][:, 0:NE], lhsT=tT, rhs=tri32_t, start=True, stop=True), reads=['tT', 'tri32'], writes=[PS[1]])
    S.op('dve', lambda e: e.tensor_copy(out=start, in_=ps[1][:, 0:NE]), reads=[PS[1]], writes=['start'])
    S.op('dve', lambda e: e.tensor_scalar(out=start128, in0=start, scalar1=128.0, scalar2=1.0, op0=ALU.mult, op1=ALU.add),
         reads=['start'], writes=['start128'])
    S.op('dve', lambda e: e.memset(ej, -1.0), writes=['ej'])
    for ex in range(NE):
        S.op('dve', lambda e, ex=ex: e.scalar_tensor_tensor(out=ej, in0=cst_t[:, 0:NTILE], scalar=start[:, ex:ex + 1], in1=ej, op0=ALU.is_ge, op1=ALU.add),
             reads=['cst', 'start', 'ej'], writes=['ej'])
    S.op('dve', lambda e: e.tensor_scalar(out=ej, in0=ej, scalar1=128.0, scalar2=cst_t[:, 64:65], op0=ALU.mult, op1=ALU.add),
         reads=['ej', 'cst'], writes=['ej'])
    S.op('dve', lambda e: e.tensor_tensor(out=tmp32[:, 0:1], in0=start[:, NE - 1:NE], in1=tle[:, NE - 1:NE], op=ALU.add),
         reads=['start', 'tle'], writes=['tmp32'])
    S.op('dve', lambda e: e.tensor_scalar(out=ej2, in0=cst_t[:, 0:NTILE], scalar1=tmp32[:, 0:1], scalar2=float(OOB_ROW), op0=ALU.is_ge, op1=ALU.mult),
         reads=['cst', 'tmp32'], writes=['ej2'])
    S.op('dve', lambda e: e.tensor_tensor(out=IDXW, in0=ej, in1=ej2, op=ALU.add), reads=['ej', 'ej2'], writes=['IDXW'])
    for tl in range(NT_OWN):
        S.op('dve', lambda e, tl=tl: e.tensor_tensor(out=RANK[:, tl, :], in0=RANK[:, tl, :], in1=start128, op=ALU.add),
             reads=[('RANK', tl), 'start128'], writes=[('RANK', tl)])
        S.op('dve', lambda e, tl=tl: e.tensor_tensor(out=RANK[:, tl, :], in0=RANK[:, tl, :], in1=MselF[:, tl, :], op=ALU.mult),
             reads=[('RANK', tl), ('MselF', tl)], writes=[('RANK', tl)])
        S.op('dve', lambda e, tl=tl: e.max(out=p8, in_=RANK[:, tl, :]), reads=[('RANK', tl)], writes=['p8'])
        for k in range(2):
            S.op('dve', lambda e, tl=tl, k=k: e.scalar_tensor_tensor(out=tmp32, in0=RANK[:, tl, :], scalar=p8[:, k:k + 1], in1=COMB[:, tl, :],
                                                                    op0=ALU.is_equal, op1=ALU.mult, accum_out=CW2[:, tl, k:k + 1]),
                 reads=[('RANK', tl), 'p8', ('COMB', tl)], writes=['tmp32', ('CW2', tl)])
        S.op('dve', lambda e, tl=tl: e.tensor_scalar(out=POSI[:, tl, :], in0=p8[:, 0:2], scalar1=-1.0, scalar2=None, op0=ALU.add),
             reads=['p8'], writes=[('POSI', tl)])
        for k in range(2):
            S.dma('pool', [(lambda e, tl=tl, k=k: e.indirect_dma_start(out=xs_d, out_offset=bass.IndirectOffsetOnAxis(ap=POSI[:, tl, k:k + 1], axis=0),
                                                                       in_=M[:, tl, :], in_offset=None),
                            [('POSI', tl), ('M', tl)] + [('xs_zero', a) for a in range(NTILE)], [('xs_sc', tl, k)])], sem='sc')
    all_sc = [('xs_sc', tl, k) for tl in range(NT_OWN) for k in range(2)]
    S.barrier()
    if debug:
        S.dma('sp', [(lambda e: e.dma_start(out=dbg['H'], in_=H.rearrange("p t d -> p (t d)")), [('H', t) for t in range(16)], []),
                     (lambda e: e.dma_start(out=dbg['comb'], in_=COMB.rearrange("p t d -> p (t d)")), [('COMB', t) for t in range(16)], [])], sem='dbg')
        S.barrier()
    es_o.close()
    if stop == 'O':
        S.barrier(); S.build(); return nc

    es_m = ExitStack()
    W13g = [sb(es_m, "W13g%d" % i, [128, 8 * 512], BF16) for i in range(3)]
    W2g = [sb(es_m, "W2g%d" % i, [128, 2 * D], BF16) for i in range(3)]
    XS = [sb(es_m, "XS%d" % i, [128, D], BF16) for i in range(4)]
    xsT = [sb(es_m, "xsT%d" % i, [128, 8, 128], BF16) for i in range(2)]
    sa = [sb(es_m, "sa%d" % i, [128, 256], F32) for i in range(2)]
    hid = [sb(es_m, "hid%d" % i, [128, 256], BF16) for i in range(2)]
    hidT = [sb(es_m, "hidT%d" % i, [128, 2, 128], BF16) for i in range(2)]
    yt = [sb(es_m, "yt%d" % i, [128, D], F32) for i in range(2)]

    bcreg = {}
    all_wcast = [('wcast', ex_) for ex_ in range(NE)]

    def _mk_bcreg(e):
        bcreg['r'] = e.to_reg(NE * 128 - 1)
        return None
    S.ops['pool'].append(([], _mk_bcreg, None, 0))

    def moeA_pre(j):
        b = j % 2
        wb = j % 3
        S.dma('pool', [
            (lambda e, j=j, wb=wb: e.indirect_dma_start(out=W13g[wb], out_offset=None, in_=w13b,
                                                        in_offset=bass.IndirectOffsetOnAxis(ap=IDXW[:, j:j + 1], axis=0),
                                                        bounds_check=bcreg['r'], oob_is_err=False), ['IDXW'] + all_wcast, [('W13g', wb)]),
            (lambda e, j=j, wb=wb: e.indirect_dma_start(out=W2g[wb], out_offset=None, in_=w2b,
                                                        in_offset=bass.IndirectOffsetOnAxis(ap=IDXW[:, j:j + 1], axis=0),
                                                        bounds_check=bcreg['r'], oob_is_err=False), ['IDXW'], [('W2g', wb)]),
        ], sem='wg%d' % wb)
        xb4 = j % 4
        for kc in range(8):
            S.op('pe', lambda e, kc=kc, b=b, xb4=xb4: e.transpose(out=pt[b][:, kc * 128:(kc + 1) * 128], in_=XS[xb4][:, kc * 128:(kc + 1) * 128], identity=ident),
                 reads=[('XS', xb4), 'ident'], writes=[PT[b]], signal=(kc == 7))
        S.op('dve', lambda e, b=b: e.tensor_tensor(out=xsT[b], in0=pt[b].rearrange("p (k t) -> p k t", k=8),
                                                   in1=gT_t[:, 8:16].unsqueeze(2).to_broadcast([128, 8, 128]), op=ALU.mult),
             reads=[PT[b], 'gT'], writes=[('xsT', b)])

    def moeA_mm(j):
        b = j % 2
        wb = j % 3
        for kc in range(8):
            S.op('pe', lambda e, kc=kc, b=b, wb=wb: e.matmul(ps[b], lhsT=xsT[b][:, kc, :], rhs=W13g[wb][:, kc * 512:(kc + 1) * 512], start=(kc == 0), stop=(kc == 7)),
                 reads=[('xsT', b), ('W13g', wb)], writes=[PS[b]], signal=(kc == 7))
        S.op('act', lambda e, b=b: e.activation(out=sa[b], in_=ps[b][:, 0:256], func=AF.Silu), reads=[PS[b]], writes=[('sa', b)])
        S.op('dve', lambda e, b=b: e.tensor_tensor(out=hid[b], in0=sa[b], in1=ps[b][:, 256:512], op=ALU.mult),
             reads=[('sa', b), PS[b]], writes=[('hid', b)])

    def moeB(j):
        b = j % 2
        wb = j % 3
        for ft in range(2):
            S.op('pe', lambda e, ft=ft, b=b: e.transpose(out=pt[b][:, ft * 128:(ft + 1) * 128], in_=hid[b][:, ft * 128:(ft + 1) * 128], identity=ident),
                 reads=[('hid', b), 'ident'], writes=[PT[b]], signal=(ft == 1))
        S.op('act', lambda e, b=b: e.activation(out=hidT[b], in_=pt[b][:, 0:256].rearrange("p (f t) -> p f t", f=2), func=AF.Copy),
             reads=[PT[b]], writes=[('hidT', b)])
        for half in range(2):
            pb = 2 + 2 * b + half
            for ft in range(2):
                S.op('pe', lambda e, ft=ft, half=half, b=b, pb=pb, wb=wb: e.matmul(
                    ps[pb], lhsT=hidT[b][:, ft, :], rhs=W2g[wb][:, ft * D + half * 512:ft * D + (half + 1) * 512], start=(ft == 0), stop=(ft == 1)),
                    reads=[('hidT', b), ('W2g', wb)], writes=[PS[pb]], signal=(ft == 1))
            if half == 0:
                S.op('act', lambda e, b=b, pb=pb: e.activation(out=yt[b][:, 0:512], in_=ps[pb], func=AF.Copy), reads=[PS[pb]], writes=[('yt', b)])
            else:
                S.op('dve', lambda e, b=b, pb=pb: e.tensor_copy(out=yt[b][:, 512:1024], in_=ps[pb]), reads=[PS[pb]], writes=[('yt', b)])
        S.dma('sp', [(lambda e, j=j, b=b: e.dma_start(out=outs_d[j * 128:(j + 1) * 128, :], in_=yt[b]), [('yt', b)], [('outs', j)])], sem='ost')

    def xsload(j):
        xb4 = j % 4
        S.dma('act', [(lambda e, j=j, xb4=xb4: e.dma_start(out=XS[xb4], in_=xs_d[j * 128:(j + 1) * 128, :]), all_sc, [('XS', xb4)])], sem='xsl%d' % xb4)

    for j in range(3):
        xsload(j)
    moeA_pre(0)
    moeA_mm(0)
    for j in range(NTILE):
        if j + 3 < NTILE:
            xsload(j + 3)
        if j + 1 < NTILE:
            moeA_pre(j + 1)
        moeB(j)
        if j + 1 < NTILE:
            moeA_mm(j + 1)
    if os.environ.get('SBUFDBG'):
        print("sbuf remaining after MoE alloc", nc.sbuf_bytes_remaining)
    all_outs = [('outs', j) for j in range(NTILE)]

    O12 = [sb(es_m, "O12_%d" % i, [128, 2, D], F32) for i in range(2)]
    for tl in range(NT_OWN):
        b = tl % 2
        S.dma('pool', [
            (lambda e, tl=tl, b=b, k=k: e.indirect_dma_start(out=O12[b][:, k, :], out_offset=None, in_=outs_d,
                                                             in_offset=bass.IndirectOffsetOnAxis(ap=POSI[:, tl, k:k + 1], axis=0)),
             all_outs + [('POSI', tl)], [('O12', b)]) for k in range(2)], sem='og%d' % b)
        for k in range(2):
            S.op('dve', lambda e, tl=tl, b=b, k=k: e.scalar_tensor_tensor(out=H[:, tl, :], in0=O12[b][:, k, :], scalar=CW2[:, tl, k:k + 1], in1=H[:, tl, :],
                                                                         op0=ALU.mult, op1=ALU.add),
                 reads=[('O12', b), ('CW2', tl), ('H', tl)], writes=[('H', tl)])
        s_ = st[b]
        sk = ('st', b)
        S.op('dve', lambda e, tl=tl, s_=s_: e.scalar_tensor_tensor(out=junk, in0=H[:, tl, :], scalar=1.0, in1=H[:, tl, :], op0=ALU.mult, op1=ALU.mult,
                                                                  accum_out=s_[:, 0:1]), reads=[('H', tl)], writes=['junk', sk])
        S.op('dve', lambda e, s_=s_: e.tensor_scalar(out=s_[:, 1:2], in0=s_[:, 0:1], scalar1=1.0 / D, scalar2=EPS, op0=ALU.mult, op1=ALU.add),
             reads=[sk], writes=[sk])
        S.op('act', lambda e, s_=s_: e.activation(out=s_[:, 3:4], in_=s_[:, 1:2], func=AF.Sqrt), reads=[sk], writes=[sk])
        S.op('dve', lambda e, s_=s_: e.reciprocal(out=s_[:, 2:3], in_=s_[:, 3:4]), reads=[sk], writes=[sk])
        S.op('dve', lambda e, tl=tl, s_=s_, b=b: e.scalar_tensor_tensor(out=xin[b], in0=H[:, tl, :], scalar=s_[:, 2:3], in1=gf_t, op0=ALU.mult, op1=ALU.mult),
             reads=[('H', tl), sk, 'gf'], writes=[('xin', b)])
        S.dma('sp', [(lambda e, tl=tl, b=b: e.dma_start(out=out[tl * 128:(tl + 1) * 128, :], in_=xin[b]), [('xin', b)], [])], sem='out')
    S.barrier()
    S.build()
    return nc


def _t5_bucket_np(n):
    n = np.maximum(n, 0)
    nf = np.maximum(n, 16).astype(np.float32)
    large = 16 + (np.log(nf / np.float32(16)) / np.float32(math.log(128 / 16)) * np.float32(16)).astype(np.int32)
    large = np.minimum(large, 31)
    return np.where(n < 16, n, large)


_PROG = {}


def _get_prog(debug=False):
    if debug not in _PROG:
        _PROG[debug] = build_program(debug)
    return _PROG[debug]


def make_in_maps(x, norm_mix_g, w_in, b_gates, gmlp_ln_g, gmlp_ln_b, w_spatial, b_spatial, rel_bias,
                 w_out, norm_ffn_g, w_group_router, b_group_router, w_expert_router, b_expert_router,
                 w1, w3, w2, norm_final_g):
    f = lambda a: np.ascontiguousarray(np.asarray(a), dtype=np.float32)
    x = f(x)
    w_in0 = f(w_in[0]); w_out0 = f(w_out[0]); w1_0 = f(w1[0]); w3_0 = f(w3[0]); w2_0 = f(w2[0])
    w13 = np.concatenate([w1_0, w3_0], axis=2).reshape(NE, 8, 128, 512)
    w13r = np.ascontiguousarray(np.transpose(w13, (0, 2, 1, 3))).reshape(NE * 128, 8 * 512)
    w2r = np.ascontiguousarray(np.transpose(w2_0.reshape(NE, 2, 128, D), (0, 2, 1, 3))).reshape(NE * 128, 2 * D)
    ub = np.concatenate([np.triu(np.ones((128, 128), np.float32), 1), np.ones((128, 128), np.float32)], axis=1).astype(ml_dtypes.bfloat16)
    tri32 = np.triu(np.ones((32, 32), np.float32), 1).astype(ml_dtypes.bfloat16)
    cst = np.zeros((128, 128), np.float32)
    cst[:, 0:64] = np.arange(64, dtype=np.float32)[None, :]
    cst[:, 64] = np.arange(128, dtype=np.float32)
    cst[:, 65:81] = (128.0 * np.arange(16, dtype=np.float32))[None, :]
    wr = np.concatenate([f(w_group_router[0]), np.transpose(f(w_expert_router[0]), (1, 0, 2)).reshape(D, 32)], axis=1)
    br = np.concatenate([f(b_group_router[0]), f(b_expert_router[0]).reshape(32)])
    brb = np.ascontiguousarray(np.broadcast_to(br[None, :], (128, 36)))
    gT = np.concatenate([f(norm_mix_g[0]).reshape(8, 128).T, f(norm_ffn_g[0]).reshape(8, 128).T], axis=1)
    bg = f(b_gates[0])
    rows = [f(gmlp_ln_g[0]), f(gmlp_ln_b[0]), bg[:D], bg[D:], f(norm_final_g), np.zeros(D, np.float32)]
    rowb = np.ascontiguousarray(np.stack([np.broadcast_to(r[None, :], (128, D)) for r in rows]))
    wsp = f(w_spatial[0])
    wspT = np.ascontiguousarray(np.transpose(wsp, (0, 2, 1)))
    bspT = np.ascontiguousarray(f(b_spatial[0]).T)
    rb = f(rel_bias)
    kk = np.arange(128)[:, None]
    qq = np.arange(128)[None, :]
    b0 = _t5_bucket_np(qq - kk)
    b1 = _t5_bucket_np(qq - kk + 128)
    rb0 = np.ascontiguousarray(np.stack([rb[b0, h] for h in range(8)]))
    rb1 = np.ascontiguousarray(np.stack([rb[b1, h] for h in range(8)]))
    chb = np.ascontiguousarray(np.broadcast_to(rb[31][None, :], (128, 8)))
    causal_add = np.where(qq >= kk, 0.0, -BIGM).astype(np.float32)
    tril = (np.arange(128)[None, :] <= np.arange(128)[:, None]).astype(np.float32)
    triu = np.ascontiguousarray(tril.T)
    cmk = np.ascontiguousarray(np.stack([causal_add, tril, triu]))
    ident = np.eye(128, dtype=np.float32).astype(ml_dtypes.bfloat16)
    e16 = np.zeros((16, 16, 128), np.float32)
    for s in range(16):
        e16[s, s, :] = 1.0
    e16 = e16.reshape(16, 16 * 128).astype(ml_dtypes.bfloat16)
    in_maps = []
    for c in range(8):
        b, p = c // 2, c % 2
        own = x[b, p * TOWN:(p + 1) * TOWN]
        oth = x[b, (1 - p) * TOWN:(2 - p) * TOWN]
        xa = np.ascontiguousarray(np.concatenate([own, oth], axis=0))
        pnm = np.full((16, 16), -BIGG, np.float32)
        for qt in range(16):
            i = qt // 2
            pnm[qt, :i] = 0.0
            if p == 1:
                pnm[qt, 8:] = 0.0
        pn = np.ascontiguousarray(np.broadcast_to(pnm.reshape(1, 256), (128, 256)))
        in_maps.append({
            "xa": xa, "w_in": w_in0, "w_out": w_out0, "w13r": w13r, "w2r": w2r, "ub": ub, "tri32": tri32, "cst": cst, "wr": np.ascontiguousarray(wr),
            "pn": pn, "gT": np.ascontiguousarray(gT), "rowb": rowb, "brb": brb, "wsp": wsp, "wspT": wspT, "bspT": bspT,
            "rb0": rb0, "rb1": rb1, "chb": chb, "cmk": cmk, "ident": ident, "e16": e16,
        })
    return in_maps


def kernel(**inputs):
    nc = _get_prog(False)
    in_maps = make_in_maps(**inputs)
    res = run_bass_kernel_spmd(nc, in_maps, core_ids=list(range(8)))
    outp = np.empty((4, SEQ, D), np.float32)
    for c in range(8):
        b, p = c // 2, c % 2
        outp[b, p * TOWN:(p + 1) * TOWN] = np.asarray(res.results[c]["out"], dtype=np.float32)
    return outp
```

```python
import math
from contextlib import ExitStack

import numpy as np
import ml_dtypes
import concourse.bass as bass
import concourse.mybir as mybir
from concourse.bass_utils import run_bass_kernel_spmd

F32 = mybir.dt.float32
BF16 = mybir.dt.bfloat16
ALU = mybir.AluOpType
AF = mybir.ActivationFunctionType
AX = mybir.AxisListType

D = 1024
SEQ = 4096
NB = 16
TOWN = 2048
NT_OWN = 16
NT_ALL = 32
EPS = 1e-6
SCALE = 128 ** -0.5
BIGM = 30000.0
BIGG = 1.0e9
NE = 32
NTILE = 64
OOB_ROW = 8192
NSLOT = NTILE * 128
I32 = mybir.dt.int32
DEBUG = False
import os
SKIP = set(os.environ.get('SKIP', '').split(','))


class Sched:
    def __init__(self, nc):
        self.nc = nc
        self.names = ['pe', 'act', 'dve', 'pool', 'sp']
        self.ops = {k: [] for k in self.names}
        self.sem = {k: nc.alloc_semaphore("s_" + k) for k in self.names}
        self.cnt = {k: 0 for k in self.names}
        self.last_w = {}
        self.readers = {}
        self.dsem = {}
        self.dcnt = {}
        self.waited = {k: {} for k in self.names}

    def _semh(self, sk):
        return self.sem[sk] if sk in self.sem else self.dsem[sk]

    def _deps(self, eng, reads, writes):
        need = {}

        def add(sk, v):
            if need.get(sk, 0) < v:
                need[sk] = v
        for k in reads:
            if k in self.last_w:
                add(*self.last_w[k])
        for k in writes:
            if k in self.last_w:
                add(*self.last_w[k])
            for sk, v in self.readers.get(k, {}).items():
                add(sk, v)
        waits = []
        for sk, v in need.items():
            if sk == eng and v > self.cnt[eng]:
                continue
            if self.waited[eng].get(sk, 0) >= v:
                continue
            self.waited[eng][sk] = v
            waits.append((sk, v))
        return waits

    def _record(self, tag, reads, writes):
        for k in reads:
            r = self.readers.setdefault(k, {})
            if r.get(tag[0], 0) < tag[1]:
                r[tag[0]] = tag[1]
        for k in writes:
            self.last_w[k] = tag
            self.readers[k] = {}

    def op(self, eng, fn, reads=(), writes=(), signal=True):
        waits = self._deps(eng, reads, writes)
        if signal:
            self.cnt[eng] += 1
            seq = self.cnt[eng]
        else:
            seq = self.cnt[eng] + 1
        self._record((eng, seq), reads, writes)
        self.ops[eng].append((waits, fn, self.sem[eng] if signal else None, 1))

    def dma(self, q, items, sem):
        if sem not in self.dsem:
            self.dsem[sem] = self.nc.alloc_semaphore("d_" + sem)
            self.dcnt[sem] = 0
        allr, allw = [], []
        for fn, reads, writes in items:
            allr += list(reads)
            allw += list(writes)
        waits = self._deps(q, allr, allw)
        first = True
        for fn, reads, writes in items:
            self.dcnt[sem] += 16
            self.ops[q].append((waits if first else [], fn, self.dsem[sem], 16))
            first = False
        self._record((sem, self.dcnt[sem]), allr, allw)

    def barrier(self):
        for e in self.names:
            waits = []
            for o in self.names:
                if self.cnt[o] > self.waited[e].get(o, 0):
                    self.waited[e][o] = self.cnt[o]
                    waits.append((o, self.cnt[o]))
            for s, c in self.dcnt.items():
                if c > self.waited[e].get(s, 0):
                    self.waited[e][s] = c
                    waits.append((s, c))
            self.ops[e].append((waits, None, None, 0))

    def build(self):
        nc = self.nc
        with nc.Block() as block:
            def mk(name):
                def body(e):
                    for waits, fn, sem, inc in self.ops[name]:
                        for sk, v in waits:
                            e.wait_ge(self._semh(sk), v)
                        if fn is not None:
                            ins = fn(e)
                            if sem is not None:
                                ins.then_inc(sem, inc)
                return body
            block.tensor(mk('pe'))
            block.scalar(mk('act'))
            block.vector(mk('dve'))
            block.gpsimd(mk('pool'))
            block.sync(mk('sp'))


def build_program(debug=False, stop=None):
    nc = bass.Bass("TRN2", target_bir_lowering=False)

    def din(name, shape, dt=F32):
        return nc.dram_tensor(name, list(shape), dt, kind="ExternalInput").ap()

    xa = din("xa", [SEQ, D])
    w_in = din("w_in", [D, 7 * D])
    w_out = din("w_out", [D, D])
    w13r = din("w13r", [NE * 128, 8 * 512])
    w2r = din("w2r", [NE * 128, 2 * D])
    ub_d = din("ub", [128, 256], BF16)
    tri32_d = din("tri32", [32, 32], BF16)
    cst_d = din("cst", [128, 128])
    xs_d = nc.dram_tensor("xs_scr", [NSLOT, D], BF16).ap()
    w13b = nc.dram_tensor("w13b_scr", [NE * 128, 8 * 512], BF16).ap()
    w2b = nc.dram_tensor("w2b_scr", [NE * 128, 2 * D], BF16).ap()
    outs_d = nc.dram_tensor("outs_scr", [NSLOT, D], F32).ap()
    wr = din("wr", [D, 36])
    pn = din("pn", [128, 16 * 16])
    gT = din("gT", [128, 16])
    rowb = din("rowb", [6, 128, D])
    brb = din("brb", [128, 36])
    wsp = din("wsp", [8, 128, 128])
    wspT = din("wspT", [8, 128, 128])
    bspT = din("bspT", [128, 8])
    rb0 = din("rb0", [8, 128, 128])
    rb1 = din("rb1", [8, 128, 128])
    chb = din("chb", [128, 8])
    cmk = din("cmk", [3, 128, 128])
    ident_d = din("ident", [128, 128], BF16)
    e16_d = din("e16", [16, 16 * 128], BF16)
    out = nc.dram_tensor("out", [TOWN, D], F32, kind="ExternalOutput").ap()
    dbg = {}
    if debug:
        dbg['yb'] = nc.dram_tensor("dbg_yb", [128, NT_OWN * D], BF16, kind="ExternalOutput").ap()
        dbg['M'] = nc.dram_tensor("dbg_M", [128, NT_OWN * D], BF16, kind="ExternalOutput").ap()
        dbg['H'] = nc.dram_tensor("dbg_H", [128, NT_OWN * D], F32, kind="ExternalOutput").ap()
        dbg['comb'] = nc.dram_tensor("dbg_comb", [128, NT_OWN * NE], F32, kind="ExternalOutput").ap()

    S = Sched(nc)
    es_all = ExitStack()

    def sb(es, name, shape, dt):
        return es.enter_context(nc.sbuf_tensor("sb_" + name, list(shape), dt)).ap()

    pt = [nc.alloc_psum_tensor("pt%d" % i, [128, 1024], BF16).ap() for i in range(2)]
    ps = [nc.alloc_psum_tensor("ps%d" % i, [128, 512], F32).ap() for i in range(6)]
    PT = [('pt', i) for i in range(2)]
    PS = [('ps', i) for i in range(6)]

    ident = sb(es_all, "ident", [128, 128], BF16)
    e16 = sb(es_all, "e16", [16, 16 * 128], BF16)
    cmk_t = sb(es_all, "cmk", [128, 3, 128], F32)
    gT_t = sb(es_all, "gT", [128, 16], F32)
    chb_t = sb(es_all, "chb", [128, 8], F32)
    pn_t = sb(es_all, "pn", [128, 256], F32)
    st = [sb(es_all, "st%d" % i, [128, 8], F32) for i in range(2)]
    M = sb(es_all, "M", [128, NT_OWN, D], BF16)
    xin = [sb(es_all, "xin%d" % i, [128, D], F32) for i in range(2)]
    junk = sb(es_all, "junk", [128, D], F32)
    zt = junk.bitcast(BF16)[:, 0:D]
    xb = [sb(es_all, "xb%d" % i, [128, D], BF16) for i in range(2)]

    S.dma('sp', [
        (lambda e: e.dma_start(out=ident, in_=ident_d), [], ['ident']),
        (lambda e: e.dma_start(out=e16, in_=e16_d), [], ['e16']),
        (lambda e: e.dma_start(out=cmk_t, in_=cmk.rearrange("a p q -> p a q")), [], ['cmk']),
        (lambda e: e.dma_start(out=gT_t, in_=gT), [], ['gT']),
        (lambda e: e.dma_start(out=chb_t, in_=chb), [], ['chb']),
        (lambda e: e.dma_start(out=pn_t, in_=pn), [], ['pn']),
    ], sem='const')

    def rms_tile(src_fn, src_keys, b, gcol, dstT, dst_key, tok0, q='sp', xb_dst=None, xb_key=None, stage='PQ'):
        xt = src_fn
        xbd = xb[b] if xb_dst is None else xb_dst
        xbk = ('xb', b) if xb_key is None else xb_key
        s_ = st[b]
        sk = ('st', b)
        if 'P' in stage:
            S.op('dve', lambda e: e.scalar_tensor_tensor(out=junk, in0=xt, scalar=1.0, in1=xt, op0=ALU.mult, op1=ALU.mult,
                                                         accum_out=s_[:, 0:1]), reads=src_keys, writes=['junk', sk])
            S.op('dve', lambda e: e.tensor_scalar(out=s_[:, 1:2], in0=s_[:, 0:1], scalar1=1.0 / D, scalar2=EPS,
                                                  op0=ALU.mult, op1=ALU.add), reads=[sk], writes=[sk])
            S.op('act', lambda e: e.activation(out=s_[:, 3:4], in_=s_[:, 1:2], func=AF.Sqrt), reads=[sk], writes=[sk])
            S.op('dve', lambda e: e.reciprocal(out=s_[:, 2:3], in_=s_[:, 3:4]), reads=[sk], writes=[sk])
            S.op('act', lambda e: e.activation(out=xbd, in_=xt, func=AF.Copy, scale=s_[:, 2:3]),
                 reads=src_keys + [sk], writes=[xbk])
        if 'Q' not in stage:
            return
        for kc in range(8):
            S.op('pe', lambda e, kc=kc: e.transpose(out=pt[b][:, kc * 128:(kc + 1) * 128], in_=xbd[:, kc * 128:(kc + 1) * 128],
                                                    identity=ident),
                 reads=[xbk, 'ident'], writes=[PT[b]], signal=(kc == 7))
        S.op('dve', lambda e: e.tensor_tensor(out=dstT[:, :, tok0:tok0 + 128],
                                              in0=pt[b].rearrange("p (k t) -> p k t", k=8),
                                              in1=gT_t[:, gcol:gcol + 8].unsqueeze(2).to_broadcast([128, 8, 128]), op=ALU.mult),
             reads=[PT[b], 'gT'], writes=[dst_key])

    es_x = ExitStack()
    xnT_own = sb(es_x, "xnT_own", [128, 8, TOWN], BF16)
    es_x2 = ExitStack()
    xnT_oth = sb(es_x2, "xnT_oth", [128, 8, TOWN], BF16)

    def xs(kc, tok0, n):
        if tok0 < TOWN:
            return xnT_own[:, kc, tok0:tok0 + n]
        return xnT_oth[:, kc, tok0 - TOWN:tok0 - TOWN + n]
    es_a = ExitStack()
    V4 = sb(es_a, "V4", [128, NT_ALL, 4, 130], BF16)
    Wv4 = sb(es_a, "Wv4", [128, 8, 512], BF16)
    KT = sb(es_a, "KT", [128, SEQ], BF16)
    QT = sb(es_a, "QT", [128, TOWN], BF16)
    Wqk = [sb(es_a, "Wqk%d" % i, [128, 2, 8, 128], BF16) for i in range(2)]
    Tt = [sb(es_a, "Tt%d" % i, [128, 2, 256], F32) for i in range(2)]
    kmsf = sb(es_a, "kmsf", [128, 16], F32)
    kms = sb(es_a, "kms", [128, 16], BF16)
    gm = sb(es_a, "gm", [128, 16, 16], F32)
    ga = sb(es_a, "ga", [128, 16, 16], F32)
    gb_ = sb(es_a, "gb_", [128, 16, 16], F32)
    top8 = sb(es_a, "top8", [128, 16, 8], F32)
    PTs = [sb(es_a, "PTs%d" % i, [128, 512], BF16) for i in range(4)]
    stmp = [sb(es_a, "stmp%d" % i, [128, 256], F32) for i in range(2)]
    rden = sb(es_a, "rden", [128, 4], F32)
    acc_sb = sb(es_a, "acc_sb", [128, 4, 130], F32)

    S.op('pool', lambda e: e.memset(V4[:, :, :, 128:130], 1.0), writes=['V4ones'])
    S.op('pool', lambda e: e.memset(junk.bitcast(BF16), 0.0), writes=['junk'])

    def v4_tile(tt):
        pb = tt % 2
        for kc in range(8):
            S.op('pe', lambda e, kc=kc, tt=tt, pb=pb: e.matmul(ps[pb], lhsT=xs(kc, tt * 128, 128), rhs=Wv4[:, kc, :],
                                                               start=(kc == 0), stop=(kc == 7)),
                 reads=[('xnT', tt), 'Wv4'], writes=[PS[pb]], signal=(kc == 7))
        S.op('dve', lambda e, tt=tt, pb=pb: e.tensor_copy(out=V4[:, tt, :, 0:128], in_=ps[pb].rearrange("p (h c) -> p h c", h=4)),
             reads=[PS[pb]], writes=[('V4', tt)])

    S.dma('pool', [(lambda e: e.dma_start(out=Wv4, in_=w_in[:, 4096:4096 + 512].rearrange("(k p) c -> p k c", p=128)), [], ['Wv4'])], sem='wv')
    def a1(tt, stage):
        b = tt % 2
        if 'P' in stage:
            S.dma('sp', [(lambda e, tt=tt, b=b: e.dma_start(out=xin[b], in_=xa[tt * 128:(tt + 1) * 128, :]), [], [('xin', b)])],
                  sem='xin%d' % b)
        rms_tile(xin[b], [('xin', b)], b, 0, xnT_own if tt < 16 else xnT_oth, ('xnT', tt), (tt % 16) * 128, stage=stage)

    a1(0, 'P')
    for tt in range(NT_ALL):
        if tt + 1 < NT_ALL:
            a1(tt + 1, 'P')
        a1(tt, 'Q')
        if tt >= 1:
            v4_tile(tt - 1)
    v4_tile(NT_ALL - 1)
    all_xnT = [('xnT', tt) for tt in range(NT_ALL)]
    stb = [ps[0], ps[1], pt[0].bitcast(F32), pt[1].bitcast(F32)]
    STK = [PS[0], PS[1], PT[0], PT[1]]
    ptc = 0
    psc = 0
    stc = 0
    gsel = 0
    for hg in range(2):
        c0 = 4096 + hg * 512
        if hg == 1:
            S.dma('pool', [(lambda e, c0=c0: e.dma_start(out=Wv4, in_=w_in[:, c0:c0 + 512].rearrange("(k p) c -> p k c", p=128)),
                            [], ['Wv4'])], sem='wv')
            for tt in range(NT_ALL):
                v4_tile(tt)
        if stop == 'V':
            S.barrier(); S.build(); return nc
        for hh in range(4):
            h = hg * 4 + hh
            wb = h % 2
            cq = 2048 + h * 128
            ck = 3072 + h * 128
            S.dma('pool', [
                (lambda e, cq=cq, wb=wb: e.dma_start(out=Wqk[wb][:, 0], in_=w_in[:, cq:cq + 128].rearrange("(k p) c -> p k c", p=128)), [], [('Wqk', wb)]),
                (lambda e, ck=ck, wb=wb: e.dma_start(out=Wqk[wb][:, 1], in_=w_in[:, ck:ck + 128].rearrange("(k p) c -> p k c", p=128)), [], []),
            ], sem='wqk%d' % wb)
            for ex_ in range(4 * h, 4 * h + 4):
                S.dma('pool', [
                    (lambda e, ex_=ex_: e.dma_start(out=w13b[ex_ * 128:(ex_ + 1) * 128, :], in_=w13r[ex_ * 128:(ex_ + 1) * 128, :]), [], [('wcast', ex_)]),
                    (lambda e, ex_=ex_: e.dma_start(out=w2b[ex_ * 128:(ex_ + 1) * 128, :], in_=w2r[ex_ * 128:(ex_ + 1) * 128, :]), [], []),
                ], sem='wcast')
            S.dma('sp', [(lambda e, a=a: e.dma_start(out=xs_d[a * 128:(a + 1) * 128, :], in_=zt), ['junk'], [('xs_zero', a)])
                         for a in range(8 * h, 8 * h + 8)], sem='xz')
            S.dma('sp', [
                (lambda e, h=h, wb=wb: e.dma_start(out=Tt[wb][:, 0, 0:128], in_=rb0[h]), [], [('Tt', wb)]),
                (lambda e, h=h, wb=wb: e.dma_start(out=Tt[wb][:, 0, 128:256], in_=rb1[h]), [], []),
                (lambda e, h=h, wb=wb: e.dma_start(out=Tt[wb][:, 1, 0:128], in_=rb1[h]), [], []),
            ], sem='tt%d' % wb)
            if 'tt1' not in SKIP:
              S.op('dve', lambda e, wb=wb: e.tensor_tensor(out=Tt[wb][:, 0, 0:128], in0=Tt[wb][:, 0, 0:128], in1=cmk_t[:, 0, :], op=ALU.add),
                 reads=[('Tt', wb), 'cmk'], writes=[('Tt', wb)])
            if 'tt2' not in SKIP:
              S.op('dve', lambda e, wb=wb, h=h: e.tensor_scalar(out=Tt[wb][:, 1, 128:256], in0=cmk_t[:, 1, :], scalar1=0.0, scalar2=chb_t[:, h:h + 1],
                                                            op0=ALU.mult, op1=ALU.add),
                 reads=[('Tt', wb), 'cmk', 'chb'], writes=[('Tt', wb)])
            for tg in range(8):
                pb = tg % 2
                for kc in range(8):
                    S.op('pe', lambda e, kc=kc, tg=tg, pb=pb, wb=wb: e.matmul(ps[pb], lhsT=Wqk[wb][:, 1, kc, :], rhs=xs(kc, tg * 512, 512),
                                                                              start=(kc == 0), stop=(kc == 7)),
                         reads=all_xnT[tg * 4:(tg + 1) * 4] + [('Wqk', wb)], writes=[PS[pb]], signal=(kc == 7))
                for bk in range(2):
                    S.op('act', lambda e, tg=tg, pb=pb, bk=bk: e.activation(out=KT[:, tg * 512 + bk * 256:tg * 512 + (bk + 1) * 256],
                                                                         in_=ps[pb][:, bk * 256:(bk + 1) * 256], func=AF.Copy,
                                                                         accum_out=kmsf[:, 2 * tg + bk:2 * tg + bk + 1]),
                         reads=[PS[pb]], writes=[('KT', tg), 'kmsf'])
            S.op('dve', lambda e: e.tensor_copy(out=kms, in_=kmsf), reads=['kmsf'], writes=['kms'])
            for tg in range(4):
                pb = tg % 2
                for kc in range(8):
                    S.op('pe', lambda e, kc=kc, tg=tg, pb=pb, wb=wb: e.matmul(ps[pb], lhsT=Wqk[wb][:, 0, kc, :], rhs=xs(kc, tg * 512, 512),
                                                                              start=(kc == 0), stop=(kc == 7)),
                         reads=all_xnT[tg * 4:(tg + 1) * 4] + [('Wqk', wb)], writes=[PS[pb]], signal=(kc == 7))
                S.op('dve', lambda e, tg=tg, pb=pb: e.tensor_copy(out=QT[:, tg * 512:(tg + 1) * 512], in_=ps[pb]),
                     reads=[PS[pb]], writes=[('QT', tg)])
            all_QT = [('QT', tg) for tg in range(4)]
            if stop == 'KQ':
                S.barrier(); S.build(); return nc
            for qt in range(16):
                S.op('pe', lambda e, qt=qt: e.matmul(ps[5][:, qt * 16:(qt + 1) * 16], lhsT=QT[:, qt * 128:(qt + 1) * 128], rhs=kms, start=True, stop=True),
                     reads=[('QT', qt // 4), 'kms'], writes=[PS[5]], signal=(qt == 15))
            S.op('dve', lambda e: e.tensor_tensor(out=gm, in0=ps[5][:, 0:256].rearrange("p (a b) -> p a b", a=16),
                                                  in1=pn_t.rearrange("p (a b) -> p a b", a=16), op=ALU.add),
                 reads=[PS[5], 'pn'], writes=['gm'])
            for qt in range(16):
                S.op('dve', lambda e, qt=qt: e.max(out=top8[:, qt, :], in_=gm[:, qt, :]), reads=['gm'], writes=['top8'])
            S.op('dve', lambda e: e.tensor_tensor(out=ga, in0=gm, in1=top8[:, :, 2:3].to_broadcast([128, 16, 16]), op=ALU.is_ge),
                 reads=['gm', 'top8'], writes=['ga'])
            S.op('dve', lambda e: e.tensor_scalar(out=gb_, in0=gm, scalar1=-0.5 * BIGG, scalar2=None, op0=ALU.is_gt),
                 reads=['gm'], writes=['gb_'])
            S.op('dve', lambda e: e.tensor_tensor(out=ga, in0=ga, in1=gb_, op=ALU.mult), reads=['ga', 'gb_'], writes=['ga'])
            if stop == 'SEL':
                S.barrier(); S.build(); return nc
            for pr in range(4):
                i0, i1 = 2 * pr, 2 * pr + 1
                adj0 = i0 - 1 if i0 >= 1 else 15
                groups = []
                for s_ in [x for x in range(i0)] + [x for x in range(8, 16)]:
                    us = []
                    for kt in range(2):
                        sp = [('T1', 0, 256)] if (s_ == adj0 and kt == 1) else []
                        us.append(dict(q0=i0 * 256, n=512, ktile=s_ * 2 + kt, special=sp, accs=[(0, 0), (1, 128), (2, 256), (3, 384)]))
                    groups.append(dict(units=us, sel=s_, accs=[0, 1, 2, 3]))
                groups.append(dict(units=[
                    dict(q0=i1 * 256, n=256, ktile=i0 * 2, special=[], accs=[(2, 0), (3, 128)]),
                    dict(q0=i1 * 256, n=256, ktile=i0 * 2 + 1, special=[('T1', 0, 256)], accs=[(2, 0), (3, 128)])], sel=i0, accs=[2, 3]))
                for ii, ab in ((i0, 0), (i1, 2)):
                    groups.append(dict(units=[
                        dict(q0=ii * 256, n=256, ktile=ii * 2, special=[('T0', 0, 256)], accs=[(ab, 0), (ab + 1, 128)]),
                        dict(q0=ii * 256 + 128, n=128, ktile=ii * 2 + 1, special=[('T0', 0, 128)], accs=[(ab + 1, 0)])], sel=None, accs=[ab, ab + 1]))
                units = []
                for gi, g_ in enumerate(groups):
                    g_['bs'] = gsel % 2
                    gsel += 1
                    for u in g_['units']:
                        u['g'] = g_
                        units.append(u)
                    g_['last'] = units[-1]
                    g_['banks_started'] = set()
                    g_['lastmm'] = {}
                    for u in g_['units']:
                        for a_, _ in u['accs']:
                            g_['lastmm'][a_] = id(u)
                inited = set()

                def pbank(g_, a_):
                    return 2 + 2 * g_['bs'] + a_ // 2

                def pacc(g_, a_):
                    return ps[pbank(g_, a_)][:, (a_ % 2) * 256:(a_ % 2) * 256 + 129]

                def stage1(u):
                    pb = u['pb']
                    n, q0, kt_ = u['n'], u['q0'], u['ktile']
                    qkeys = [('QT', (q0 + c) // 512) for c in range(0, n, 256)] if n >= 256 else [('QT', q0 // 512)]
                    S.op('pe', lambda e, pb=pb, n=n, q0=q0, kt_=kt_: e.matmul(
                        stb[pb][:, 0:n], lhsT=KT[:, kt_ * 128:(kt_ + 1) * 128], rhs=QT[:, q0:q0 + n], start=True, stop=True),
                        reads=[('KT', kt_ // 4)] + qkeys, writes=[STK[pb]], signal=True)

                def stage2(u):
                    pb, pj, n = u['pb'], u['pj'], u['n']
                    c_done = 0
                    for (tk, c0_, c1_) in u['special']:
                        sj = u['sj']
                        tsel = 0 if tk == 'T0' else 1
                        S.op('dve', lambda e, pb=pb, sj=sj, c0_=c0_, c1_=c1_, tsel=tsel, wb=wb: e.scalar_tensor_tensor(
                            out=stmp[sj][:, c0_:c1_], in0=stb[pb][:, c0_:c1_], scalar=SCALE, in1=Tt[wb][:, tsel, 0:c1_ - c0_], op0=ALU.mult, op1=ALU.add),
                            reads=[STK[pb], ('Tt', wb)], writes=[('stmp', sj)])
                        S.op('act', lambda e, sj=sj, pj=pj, c0_=c0_, c1_=c1_: e.activation(out=PTs[pj][:, c0_:c1_], in_=stmp[sj][:, c0_:c1_], func=AF.Exp),
                             reads=[('stmp', sj)], writes=[('PTs', pj)])
                        c_done = c1_
                    if c_done < n:
                        S.op('act', lambda e, pb=pb, pj=pj, c_done=c_done, n=n, h=h: e.activation(
                            out=PTs[pj][:, c_done:n], in_=stb[pb][:, c_done:n], func=AF.Exp, bias=chb_t[:, h:h + 1], scale=SCALE),
                            reads=[STK[pb], 'chb'], writes=[('PTs', pj)])

                def stage3(g_):
                    for a_ in g_['accs']:
                        bk = pbank(g_, a_)
                        ua = [(u, off) for u in g_['units'] for (aa, off) in u['accs'] if aa == a_]
                        for k_, (u, off) in enumerate(ua):
                            pj, kt_ = u['pj'], u['ktile']
                            S.op('pe', lambda e, g_=g_, a_=a_, off=off, pj=pj, kt_=kt_, st_=(k_ == 0), sp_=(k_ == len(ua) - 1), hh=hh: e.matmul(
                                pacc(g_, a_), lhsT=PTs[pj][:, off:off + 128], rhs=V4[:, kt_, hh, 0:129], start=st_, stop=sp_),
                                reads=[('PTs', pj), ('V4', kt_), 'V4ones'], writes=[PS[bk]], signal=(k_ == len(ua) - 1))
                    for a_ in g_['accs']:
                        qt = i0 * 2 + a_
                        bk = pbank(g_, a_)
                        dst = acc_sb[:, a_, 0:129]
                        if g_['sel'] is None:
                            S.op('dve', lambda e, g_=g_, a_=a_, dst=dst: e.tensor_tensor(out=dst, in0=pacc(g_, a_), in1=dst, op=ALU.add),
                                 reads=[PS[bk], ('acc_sb', a_)], writes=[('acc_sb', a_)])
                        elif a_ not in inited:
                            S.op('dve', lambda e, g_=g_, a_=a_, dst=dst, qt=qt: e.tensor_scalar(
                                out=dst, in0=pacc(g_, a_), scalar1=ga[:, qt, g_['sel']:g_['sel'] + 1], scalar2=None, op0=ALU.mult),
                                reads=[PS[bk], 'ga'], writes=[('acc_sb', a_)])
                        else:
                            S.op('dve', lambda e, g_=g_, a_=a_, dst=dst, qt=qt: e.scalar_tensor_tensor(
                                out=dst, in0=pacc(g_, a_), scalar=ga[:, qt, g_['sel']:g_['sel'] + 1], in1=dst, op0=ALU.mult, op1=ALU.add),
                                reads=[PS[bk], 'ga', ('acc_sb', a_)], writes=[('acc_sb', a_)])
                        inited.add(a_)

                def s12(g_):
                    nonlocal_counters = None
                    for u in g_['units']:
                        stage1(u)
                        stage2(u)

                for g_ in groups:
                    for u in g_['units']:
                        u['pb'] = psc % 4
                        psc += 1
                        u['pj'] = ptc % 4
                        ptc += 1
                        if u['special']:
                            u['sj'] = stc % 2
                            stc += 1
                s12(groups[0])
                for gi, g_ in enumerate(groups):
                    if gi + 1 < len(groups):
                        s12(groups[gi + 1])
                    stage3(g_)
                for a_ in range(4):
                    tl = i0 * 2 + a_
                    S.op('dve', lambda e, a_=a_: e.reciprocal(out=rden[:, a_:a_ + 1], in_=acc_sb[:, a_, 128:129]),
                         reads=[('acc_sb', a_)], writes=['rden'])
                    S.op('dve', lambda e, a_=a_, tl=tl, h=h: e.tensor_scalar(out=M[:, tl, h * 128:(h + 1) * 128], in0=acc_sb[:, a_, 0:128],
                                                                            scalar1=rden[:, a_:a_ + 1], scalar2=None, op0=ALU.mult),
                         reads=[('acc_sb', a_), 'rden'], writes=[('M', tl)])
    S.barrier()
    if debug:
        S.dma('sp', [(lambda e: e.dma_start(out=dbg['yb'], in_=M.rearrange("p t d -> p (t d)")), [('M', t) for t in range(16)], [])], sem='dbg')
        S.barrier()
    es_a.close()
    es_x2.close()
    if stop == 'ATT':
        S.barrier(); S.build(); return nc

    es_g = ExitStack()
    Wseg = [sb(es_g, "Wseg%d" % i, [128, 8, D], BF16) for i in range(2)]
    rows = sb(es_g, "rows", [128, 4, D], F32)
    lnbt = sb(es_g, "lnbt", [128, D], F32)
    wtmp = sb(es_g, "wtmp", [128, 8, 128], F32)
    wsT = sb(es_g, "wsT", [128, 8, 128], BF16)
    rs = sb(es_g, "rs", [128, 8], F32)
    bsp_t = sb(es_g, "bsp", [128, 8], F32)
    vg2 = [sb(es_g, "vg%d" % i, [128, D], F32) for i in range(2)]
    vhat2 = [sb(es_g, "vhat%d" % i, [128, D], BF16) for i in range(2)]
    sm2 = [sb(es_g, "sm%d" % i, [128, 8], F32) for i in range(2)]
    tmpa = [sb(es_g, "tmpa%d" % i, [128, 512], F32) for i in range(2)]
    tmpb = [sb(es_g, "tmpb%d" % i, [128, 512], F32) for i in range(2)]
    M2 = sb(es_g, "M2", [128, NT_OWN, D], BF16)

    S.dma('sp', [
        (lambda e: e.dma_start(out=rows[:, 0, :], in_=rowb[0]), [], ['rows']),
        (lambda e: e.dma_start(out=lnbt, in_=rowb[1]), [], ['lnbt']),
        (lambda e: e.dma_start(out=rows[:, 2, :], in_=rowb[2]), [], []),
        (lambda e: e.dma_start(out=rows[:, 3, :], in_=rowb[3]), [], []),
        (lambda e: e.dma_start(out=bsp_t, in_=bspT), [], ['bsp']),
        (lambda e: e.dma_start(out=wtmp, in_=wsp.rearrange("g t s -> t g s")), [], ['wtmp']),
    ], sem='gconst')
    S.op('dve', lambda e: e.tensor_tensor(out=wtmp, in0=wtmp, in1=cmk_t[:, 1:2, :].to_broadcast([128, 8, 128]), op=ALU.mult),
         reads=['wtmp', 'cmk'], writes=['wtmp'])
    S.op('dve', lambda e: e.reduce_sum(out=rs, in_=wtmp, axis=AX.X), reads=['wtmp'], writes=['rs'])
    for g in range(8):
        S.op('dve', lambda e, g=g: e.tensor_scalar(out=rows[:, 1, g * 128:(g + 1) * 128], in0=lnbt[:, g * 128:(g + 1) * 128],
                                                  scalar1=rs[:, g:g + 1], scalar2=bsp_t[:, g:g + 1], op0=ALU.mult, op1=ALU.add),
             reads=['lnbt', 'rs', 'bsp', 'rows'], writes=['rows'])
    S.dma('sp', [(lambda e: e.dma_start(out=wtmp, in_=wspT.rearrange("g s t -> s g t")), [], ['wtmp'])], sem='gconst')
    S.op('dve', lambda e: e.tensor_tensor(out=wsT, in0=wtmp, in1=cmk_t[:, 2:3, :].to_broadcast([128, 8, 128]), op=ALU.mult),
         reads=['wtmp', 'cmk'], writes=['wsT'])

    seg_cols = {'v': 1024, 'u': 0, 'ga': 5120, 'gb': 6144}
    pend_mix = None
    for si, seg in enumerate(['v', 'u', 'ga', 'gb']):
        wbuf = si % 2
        c0 = seg_cols[seg]
        S.dma('pool', [(lambda e, c0=c0, wbuf=wbuf: e.dma_start(out=Wseg[wbuf], in_=w_in[:, c0:c0 + D].rearrange("(k p) c -> p k c", p=128)),
                        [], [('Wseg', wbuf)])], sem='wseg%d' % wbuf)
        for tl in range(NT_OWN):
            pp = (tl % 2) * 2 if seg == 'v' else (tl % 3) * 2
            for half in range(2):
                pb = pp + half
                for kc in range(8):
                    S.op('pe', lambda e, kc=kc, tl=tl, pb=pb, half=half, wbuf=wbuf: e.matmul(
                        ps[pb], lhsT=xs(kc, tl * 128, 128), rhs=Wseg[wbuf][:, kc, half * 512:(half + 1) * 512],
                        start=(kc == 0), stop=(kc == 7)),
                        reads=[('xnT', tl), ('Wseg', wbuf)], writes=[PS[pb]], signal=(kc == 7))
            if seg == 'v':
                vb = tl % 2
                vg, vhat, sm = vg2[vb], vhat2[vb], sm2[vb]
                kvg, kvh, ksm = ('vg', vb), ('vhat', vb), ('sm', vb)
                for half in range(2):
                    pb = pp + half
                    S.op('act', lambda e, half=half, pb=pb, vg=vg, sm=sm: e.activation(out=vg[:, half * 512:(half + 1) * 512], in_=ps[pb], func=AF.Gelu,
                                                                                     accum_out=sm[:, half:half + 1]),
                         reads=[PS[pb]], writes=[kvg, ksm])
                S.op('dve', lambda e, vg=vg, sm=sm: e.scalar_tensor_tensor(out=junk, in0=vg, scalar=1.0, in1=vg, op0=ALU.mult, op1=ALU.mult, accum_out=sm[:, 2:3]),
                     reads=[kvg], writes=['junk', ksm])
                S.op('dve', lambda e, sm=sm: e.tensor_tensor(out=sm[:, 3:4], in0=sm[:, 0:1], in1=sm[:, 1:2], op=ALU.add), reads=[ksm], writes=[ksm])
                S.op('dve', lambda e, sm=sm: e.tensor_scalar(out=sm[:, 3:4], in0=sm[:, 3:4], scalar1=1.0 / D, scalar2=None, op0=ALU.mult), reads=[ksm], writes=[ksm])
                S.op('dve', lambda e, sm=sm: e.tensor_tensor(out=sm[:, 4:5], in0=sm[:, 3:4], in1=sm[:, 3:4], op=ALU.mult), reads=[ksm], writes=[ksm])
                S.op('dve', lambda e, sm=sm: e.scalar_tensor_tensor(out=sm[:, 5:6], in0=sm[:, 2:3], scalar=1.0 / D, in1=sm[:, 4:5], op0=ALU.mult, op1=ALU.subtract),
                     reads=[ksm], writes=[ksm])
                S.op('dve', lambda e, sm=sm: e.tensor_scalar(out=sm[:, 5:6], in0=sm[:, 5:6], scalar1=EPS, scalar2=None, op0=ALU.add), reads=[ksm], writes=[ksm])
                S.op('act', lambda e, sm=sm: e.activation(out=sm[:, 6:7], in_=sm[:, 5:6], func=AF.Sqrt), reads=[ksm], writes=[ksm])
                S.op('dve', lambda e, sm=sm: e.reciprocal(out=sm[:, 7:8], in_=sm[:, 6:7]), reads=[ksm], writes=[ksm])
                S.op('dve', lambda e, vg=vg, vhat=vhat, sm=sm: e.tensor_scalar(out=vhat, in0=vg, scalar1=sm[:, 3:4], scalar2=sm[:, 7:8], op0=ALU.subtract, op1=ALU.mult),
                     reads=[kvg, ksm], writes=[kvh])

                def mix(tl=tl, vhat=vhat, kvh=kvh):
                    for g in range(8):
                        S.op('pe', lambda e, g=g: e.matmul(ps[4 + g // 4][:, (g % 4) * 128:(g % 4 + 1) * 128], lhsT=wsT[:, g, :], rhs=vhat[:, g * 128:(g + 1) * 128],
                                                          start=True, stop=True),
                             reads=['wsT', kvh], writes=[PS[4 + g // 4]], signal=(g % 4 == 3))
                    for half in range(2):
                        S.op('dve', lambda e, half=half: e.tensor_tensor(out=tmpa[half], in0=ps[4 + half], in1=rows[:, 0, half * 512:(half + 1) * 512], op=ALU.mult),
                             reads=[PS[4 + half], 'rows'], writes=[('tmpa', half)])
                        S.op('dve', lambda e, half=half: e.tensor_tensor(out=M2[:, tl, half * 512:(half + 1) * 512], in0=tmpa[half],
                                                                        in1=rows[:, 1, half * 512:(half + 1) * 512], op=ALU.add),
                             reads=[('tmpa', half), 'rows'], writes=[('M2', tl)])
                if pend_mix is not None:
                    pend_mix()
                pend_mix = mix
                if tl == NT_OWN - 1:
                    pend_mix()
                    pend_mix = None
            elif seg == 'u':
                for half in range(2):
                    pb = pp + half
                    S.op('act', lambda e, half=half, pb=pb: e.activation(out=tmpa[half], in_=ps[pb], func=AF.Gelu),
                         reads=[PS[pb]], writes=[('tmpa', half)])
                    S.op('dve', lambda e, half=half, tl=tl: e.tensor_tensor(out=M2[:, tl, half * 512:(half + 1) * 512], in0=M2[:, tl, half * 512:(half + 1) * 512],
                                                                            in1=tmpa[half], op=ALU.mult),
                         reads=[('tmpa', half), ('M2', tl)], writes=[('M2', tl)])
            elif seg == 'ga':
                for half in range(2):
                    pb = pp + half
                    S.op('dve', lambda e, half=half, pb=pb: e.tensor_tensor(out=tmpa[half], in0=ps[pb], in1=rows[:, 2, half * 512:(half + 1) * 512], op=ALU.add),
                         reads=[PS[pb], 'rows'], writes=[('tmpa', half)])
                    S.op('act', lambda e, half=half: e.activation(out=tmpb[half], in_=tmpa[half], func=AF.Sigmoid),
                         reads=[('tmpa', half)], writes=[('tmpb', half)])
                    S.op('dve', lambda e, half=half, tl=tl: e.tensor_tensor(out=M2[:, tl, half * 512:(half + 1) * 512], in0=M2[:, tl, half * 512:(half + 1) * 512],
                                                                            in1=tmpb[half], op=ALU.mult),
                         reads=[('tmpb', half), ('M2', tl)], writes=[('M2', tl)])
            else:
                for half in range(2):
                    pb = pp + half
                    S.op('dve', lambda e, half=half, pb=pb: e.tensor_tensor(out=tmpa[half], in0=ps[pb], in1=rows[:, 3, half * 512:(half + 1) * 512], op=ALU.add),
                         reads=[PS[pb], 'rows'], writes=[('tmpa', half)])
                    S.op('act', lambda e, half=half: e.activation(out=tmpb[half], in_=tmpa[half], func=AF.Sigmoid),
                         reads=[('tmpa', half)], writes=[('tmpb', half)])
                    S.op('dve', lambda e, half=half, tl=tl: e.tensor_tensor(out=tmpb[half], in0=tmpb[half], in1=M[:, tl, half * 512:(half + 1) * 512], op=ALU.mult),
                         reads=[('tmpb', half), ('M', tl)], writes=[('tmpb', half)])
                    S.op('dve', lambda e, half=half, tl=tl: e.tensor_tensor(out=M[:, tl, half * 512:(half + 1) * 512], in0=tmpb[half],
                                                                            in1=M2[:, tl, half * 512:(half + 1) * 512], op=ALU.add),
                         reads=[('tmpb', half), ('M2', tl)], writes=[('M', tl)])
    S.barrier()
    if debug:
        S.dma('sp', [(lambda e: e.dma_start(out=dbg['M'], in_=M.rearrange("p t d -> p (t d)")), [('M', t) for t in range(16)], [])], sem='dbg')
        S.barrier()
    es_g.close()
    es_x.close()
    if stop == 'G':
        S.barrier(); S.build(); return nc

    es_h = ExitStack()
    H = sb(es_h, "H", [128, NT_OWN, D], F32)
    COMB = sb(es_h, "COMB", [128, NT_OWN, NE], F32)
    gf_t = sb(es_h, "gf", [128, D], F32)
    MselF = sb(es_h, "MselF", [128, NT_OWN, NE], F32)
    MselB = sb(es_h, "MselB", [128, NT_OWN, NE], BF16)
    RANK = sb(es_h, "RANK", [128, NT_OWN, NE], F32)
    POSI = sb(es_h, "POSI", [128, NT_OWN, 2], I32)
    CW2 = sb(es_h, "CW2", [128, NT_OWN, 2], F32)
    IDXW = sb(es_h, "IDXW", [128, NTILE], I32)
    cst_t = sb(es_h, "cst", [128, 128], F32)
    es_o = ExitStack()
    xn2T = sb(es_o, "xn2T", [128, 8, TOWN], BF16)
    ub_t = sb(es_o, "ub", [128, 256], BF16)
    tri32_t = sb(es_o, "tri32", [32, 32], BF16)
    cnt = sb(es_o, "cnt", [128, NE], F32)
    tle = sb(es_o, "tle", [128, NE], F32)
    tlb = sb(es_o, "tlb", [128, NE], BF16)
    tT = sb(es_o, "tT", [32, 128], BF16)
    start = sb(es_o, "start", [128, NE], F32)
    start128 = sb(es_o, "start128", [128, NE], F32)
    ej = sb(es_o, "ej", [128, NTILE], F32)
    ej2 = sb(es_o, "ej2", [128, NTILE], F32)
    tmp32 = sb(es_o, "tmp32", [128, NE], F32)
    p8 = sb(es_o, "p8", [128, 8], F32)
    Wout = sb(es_o, "Wout", [128, 8, D], BF16)
    MT = [sb(es_o, "MT%d" % i, [128, 8, 128], BF16) for i in range(2)]
    Wr = sb(es_o, "Wr", [128, 8, 36], BF16)
    brb_t = sb(es_o, "brb", [128, 36], F32)
    lg2 = [sb(es_o, "lg%d" % i, [128, 36], F32) for i in range(2)]
    r_2 = [sb(es_o, "r_%d" % i, [128, 8], F32) for i in range(2)]
    oh2 = [sb(es_o, "oh%d" % i, [128, 4], F32) for i in range(2)]
    ge2 = [sb(es_o, "ge%d" % i, [128, 4], F32) for i in range(2)]
    elm2 = [sb(es_o, "elm%d" % i, [128, 32], F32) for i in range(2)]
    t82 = [sb(es_o, "t8%d" % i, [128, 8], F32) for i in range(2)]
    pe_2 = [sb(es_o, "pe_%d" % i, [128, 32], F32) for i in range(2)]
    pm2 = [sb(es_o, "pm%d" % i, [128, 32], F32) for i in range(2)]

    S.dma('pool', [
        (lambda e: e.dma_start(out=Wout, in_=w_out.rearrange("(k p) c -> p k c", p=128)), [], ['Wout']),
        (lambda e: e.dma_start(out=Wr, in_=wr.rearrange("(k p) c -> p k c", p=128)), [], ['Wr']),
    ], sem='wout')
    S.dma('sp', [
        (lambda e: e.dma_start(out=brb_t, in_=brb), [], ['brb']),
        (lambda e: e.dma_start(out=gf_t, in_=rowb[4]), [], ['gf']),
        (lambda e: e.dma_start(out=ub_t, in_=ub_d), [], ['ub']),
        (lambda e: e.dma_start(out=tri32_t, in_=tri32_d), [], ['tri32']),
        (lambda e: e.dma_start(out=cst_t, in_=cst_d), [], ['cst']),
    ], sem='oconst')
    def o_T1(tl):
        b = tl % 2
        pp = (tl % 2) * 2
        for kc in range(8):
            S.op('pe', lambda e, kc=kc, tl=tl, b=b: e.transpose(out=pt[b][:, kc * 128:(kc + 1) * 128], in_=M[:, tl, kc * 128:(kc + 1) * 128], identity=ident),
                 reads=[('M', tl), 'ident'], writes=[PT[b]], signal=(kc == 7))
        S.op('act', lambda e, b=b: e.activation(out=MT[b], in_=pt[b].rearrange("p (k t) -> p k t", k=8), func=AF.Copy),
             reads=[PT[b]], writes=[('MT', b)])
        S.dma('sp', [(lambda e, tl=tl, b=b: e.dma_start(out=xin[b], in_=xa[tl * 128:(tl + 1) * 128, :]), [], [('xin', b)])], sem='xin%d' % b)
        for half in range(2):
            pb = pp + half
            for kc in range(8):
                S.op('pe', lambda e, kc=kc, b=b, pb=pb, half=half: e.matmul(ps[pb], lhsT=MT[b][:, kc, :], rhs=Wout[:, kc, half * 512:(half + 1) * 512],
                                                                            start=(kc == 0), stop=(kc == 7)),
                     reads=[('MT', b), 'Wout'], writes=[PS[pb]], signal=(kc == 7))
            S.op('dve', lambda e, tl=tl, half=half, pb=pb, b=b: e.tensor_tensor(out=H[:, tl, half * 512:(half + 1) * 512], in0=ps[pb],
                                                                                 in1=xin[b][:, half * 512:(half + 1) * 512], op=ALU.add),
                 reads=[PS[pb], ('xin', b)], writes=[('H', tl)])
    def o_P(tl):
        b = tl % 2
        rms_tile(H[:, tl, :], [('H', tl)], b, 8, xn2T, ('xn2T', tl), tl * 128, xb_dst=M[:, tl, :], xb_key=('M', tl), stage='P')

    def o_Q(tl):
        b = tl % 2
        rms_tile(H[:, tl, :], [('H', tl)], b, 8, xn2T, ('xn2T', tl), tl * 128, xb_dst=M[:, tl, :], xb_key=('M', tl), stage='Q')

    def o_R(tl):
        b = tl % 2
        lg = lg2[tl % 2]
        r_ = r_2[tl % 2]
        oh = oh2[tl % 2]
        ge = ge2[tl % 2]
        elm = elm2[tl % 2]
        t8 = t82[tl % 2]
        pe_ = pe_2[tl % 2]
        pm = pm2[tl % 2]
        for kc in range(8):
            S.op('pe', lambda e, kc=kc, tl=tl: e.matmul(ps[4][:, 0:36], lhsT=xn2T[:, kc, tl * 128:(tl + 1) * 128], rhs=Wr[:, kc, :],
                                                        start=(kc == 0), stop=(kc == 7)),
                 reads=[('xn2T', tl), 'Wr'], writes=[PS[4]], signal=(kc == 7))
        S.op('dve', lambda e: e.tensor_tensor(out=lg, in0=ps[4][:, 0:36], in1=brb_t, op=ALU.add), reads=[PS[4], 'brb'], writes=[('lg', tl % 2)])
        S.op('dve', lambda e: e.reduce_max(out=r_[:, 0:1], in_=lg[:, 0:4], axis=AX.X), reads=[('lg', tl % 2)], writes=[('r_', tl % 2)])
        S.op('dve', lambda e: e.tensor_scalar(out=r_[:, 1:2], in0=r_[:, 0:1], scalar1=-1.0, scalar2=None, op0=ALU.mult), reads=[('r_', tl % 2)], writes=[('r_', tl % 2)])
        S.op('act', lambda e: e.activation(out=ge, in_=lg[:, 0:4], func=AF.Exp, bias=r_[:, 1:2], accum_out=r_[:, 2:3]),
             reads=[('lg', tl % 2), ('r_', tl % 2)], writes=[('ge', tl % 2), ('r_', tl % 2)])
        S.op('dve', lambda e: e.tensor_scalar(out=oh, in0=lg[:, 0:4], scalar1=r_[:, 0:1], scalar2=None, op0=ALU.is_ge), reads=[('lg', tl % 2), ('r_', tl % 2)], writes=[('oh', tl % 2)])
        S.op('dve', lambda e: e.tensor_scalar(out=oh, in0=oh, scalar1=BIGG, scalar2=-BIGG, op0=ALU.mult, op1=ALU.add), reads=[('oh', tl % 2)], writes=[('oh', tl % 2)])
        S.op('dve', lambda e: e.tensor_tensor(out=elm.rearrange("p (g x) -> p g x", g=4), in0=lg[:, 4:36].rearrange("p (g x) -> p g x", g=4),
                                              in1=oh.unsqueeze(2).to_broadcast([128, 4, 8]), op=ALU.add),
             reads=[('lg', tl % 2), ('oh', tl % 2)], writes=[('elm', tl % 2)])
        S.op('dve', lambda e: e.max(out=t8, in_=elm), reads=[('elm', tl % 2)], writes=[('t8', tl % 2)])
        S.op('dve', lambda e: e.tensor_scalar(out=r_[:, 6:7], in0=t8[:, 0:1], scalar1=-1.0, scalar2=None, op0=ALU.mult), reads=[('t8', tl % 2), ('r_', tl % 2)], writes=[('r_', tl % 2)])
        S.op('act', lambda e: e.activation(out=pe_, in_=elm, func=AF.Exp, bias=r_[:, 6:7]), reads=[('elm', tl % 2), ('r_', tl % 2)], writes=[('pe_', tl % 2)])
        S.op('dve', lambda e: e.scalar_tensor_tensor(out=pm, in0=elm, scalar=t8[:, 1:2], in1=pe_, op0=ALU.is_ge, op1=ALU.mult, accum_out=r_[:, 3:4]),
             reads=[('elm', tl % 2), ('t8', tl % 2), ('pe_', tl % 2), ('r_', tl % 2)], writes=[('pm', tl % 2), ('r_', tl % 2)])
        S.op('dve', lambda e: e.tensor_tensor(out=r_[:, 4:5], in0=r_[:, 3:4], in1=r_[:, 2:3], op=ALU.mult), reads=[('r_', tl % 2)], writes=[('r_', tl % 2)])
        S.op('dve', lambda e: e.reciprocal(out=r_[:, 5:6], in_=r_[:, 4:5]), reads=[('r_', tl % 2)], writes=[('r_', tl % 2)])
        S.op('dve', lambda e, tl=tl: e.tensor_scalar(out=COMB[:, tl, :], in0=pm, scalar1=r_[:, 5:6], scalar2=None, op0=ALU.mult),
             reads=[('pm', tl % 2), ('r_', tl % 2)], writes=[('COMB', tl)])
        S.op('dve', lambda e, tl=tl: e.tensor_scalar(out=MselF[:, tl, :], in0=elm, scalar1=t8[:, 1:2], scalar2=None, op0=ALU.is_ge),
             reads=[('elm', tl % 2), ('t8', tl % 2)], writes=[('MselF', tl)])
        S.op('dve', lambda e, tl=tl: e.tensor_copy(out=MselB[:, tl, :], in_=MselF[:, tl, :]), reads=[('MselF', tl)], writes=[('MselB', tl)])
    o_T1(0)
    o_P(0)
    for tl in range(NT_OWN):
        if tl + 1 < NT_OWN:
            o_T1(tl + 1)
        o_Q(tl)
        if tl + 1 < NT_OWN:
            o_P(tl + 1)
        o_R(tl)
    allMB = [('MselB', t) for t in range(NT_OWN)]
    for tl in range(NT_OWN):
        pb = 4 + tl % 2
        for j in range(tl):
            S.op('pe', lambda e, j=j, pb=pb: e.matmul(ps[pb][:, 0:NE], lhsT=ub_t[:, 128:256], rhs=MselB[:, j, :], start=(j == 0), stop=False),
                 reads=['ub', ('MselB', j)], writes=[PS[pb]], signal=False)
        S.op('pe', lambda e, tl=tl, pb=pb: e.matmul(ps[pb][:, 0:NE], lhsT=ub_t[:, 0:128], rhs=MselB[:, tl, :], start=(tl == 0), stop=True),
             reads=['ub', ('MselB', tl)], writes=[PS[pb]])
        S.op('dve', lambda e, tl=tl, pb=pb: e.tensor_copy(out=RANK[:, tl, :], in_=ps[pb][:, 0:NE]), reads=[PS[pb]], writes=[('RANK', tl)])
    for j in range(NT_OWN):
        S.op('pe', lambda e, j=j: e.matmul(ps[0][:, 0:NE], lhsT=ub_t[:, 128:256], rhs=MselB[:, j, :], start=(j == 0), stop=(j == NT_OWN - 1)),
             reads=['ub', ('MselB', j)], writes=[PS[0]], signal=(j == NT_OWN - 1))
    S.op('dve', lambda e: e.tensor_copy(out=cnt, in_=ps[0][:, 0:NE]), reads=[PS[0]], writes=['cnt'])
    S.op('dve', lambda e: e.memset(tle, 0.0), writes=['tle'])
    for m in range(16):
        S.op('dve', lambda e, m=m: e.scalar_tensor_tensor(out=tle, in0=cnt, scalar=float(128 * m), in1=tle, op0=ALU.is_gt, op1=ALU.add),
             reads=['cnt', 'tle'], writes=['tle'])
    S.op('dve', lambda e: e.tensor_copy(out=tlb, in_=tle), reads=['tle'], writes=['tlb'])
    S.op('pe', lambda e: e.transpose(out=pt[0][0:32, 0:128], in_=tlb, identity=ident), reads=['tlb', 'ident'], writes=[PT[0]])
    S.op('dve', lambda e: e.tensor_copy(out=tT, in_=pt[0][0:32, 0:128]), reads=[PT[0]], writes=['tT'])
    S.op('pe', lambda e: e.matmul(ps[1][:, 0:NE], lhsT=tT, rhs=tri32_t, start=True, stop=True), reads=['tT', 'tri32'], writes=[PS[1]])
    S.op('dve', lambda e: e.tensor_copy(out=start, in_=ps[1][:, 0:NE]), reads=[PS[1]], writes=['start'])
    S.op('dve', lambda e: e.tensor_scalar(out=start128, in0=start, scalar1=128.0, scalar2=1.0, op0=ALU.mult, op1=ALU.add),
         reads=['start'], writes=['start128'])
    S.op('dve', lambda e: e.memset(ej, -1.0), writes=['ej'])
    for ex in range(NE):
        S.op('dve', lambda e, ex=ex: e.scalar_tensor_tensor(out=ej, in0=cst_t[:, 0:NTILE], scalar=start[:, ex:ex + 1], in1=ej, op0=ALU.is_ge, op1=ALU.add),
             reads=['cst', 'start', 'ej'], writes=['ej'])
    S.op('dve', lambda e: e.tensor_scalar(out=ej, in0=ej, scalar1=128.0, scalar2=cst_t[:, 64:65], op0=ALU.mult, op1=ALU.add),
         reads=['ej', 'cst'], writes=['ej'])
    S.op('dve', lambda e: e.tensor_tensor(out=tmp32[:, 0:1], in0=start[:, NE - 1:NE], in1=tle[:, NE - 1:NE], op=ALU.add),
         reads=['start', 'tle'], writes=['tmp32'])
    S.op('dve', lambda e: e.tensor_scalar(out=ej2, in0=cst_t[:, 0:NTILE], scalar1=tmp32[:, 0:1], scalar2=float(OOB_ROW), op0=ALU.is_ge, op1=ALU.mult),
         reads=['cst', 'tmp32'], writes=['ej2'])
    S.op('dve', lambda e: e.tensor_tensor(out=IDXW, in0=ej, in1=ej2, op=ALU.add), reads=['ej', 'ej2'], writes=['IDXW'])
    for tl in range(NT_OWN):
        S.op('dve', lambda e, tl=tl: e.tensor_tensor(out=RANK[:, tl, :], in0=RANK[:, tl, :], in1=start128, op=ALU.add),
             reads=[('RANK', tl), 'start128'], writes=[('RANK', tl)])
        S.op('dve', lambda e, tl=tl: e.tensor_tensor(out=RANK[:, tl, :], in0=RANK[:, tl, :], in1=MselF[:, tl, :], op=ALU.mult),
             reads=[('RANK', tl), ('MselF', tl)], writes=[('RANK', tl)])
        S.op('dve', lambda e, tl=tl: e.max(out=p8, in_=RANK[:, tl, :]), reads=[('RANK', tl)], writes=['p8'])
        for k in range(2):
            S.op('dve', lambda e, tl=tl, k=k: e.scalar_tensor_tensor(out=tmp32, in0=RANK[:, tl, :], scalar=p8[:, k:k + 1], in1=COMB[:, tl, :],
                                                                    op0=ALU.is_equal, op1=ALU.mult, accum_out=CW2[:, tl, k:k + 1]),
                 reads=[('RANK', tl), 'p8', ('COMB', tl)], writes=['tmp32', ('CW2', tl)])
        S.op('dve', lambda e, tl=tl: e.tensor_scalar(out=POSI[:, tl, :], in0=p8[:, 0:2], scalar1=-1.0, scalar2=None, op0=ALU.add),
             reads=['p8'], writes=[('POSI', tl)])
        for k in range(2):
            S.dma('pool', [(lambda e, tl=tl, k=k: e.indirect_dma_start(out=xs_d, out_offset=bass.IndirectOffsetOnAxis(ap=POSI[:, tl, k:k + 1], axis=0),
                                                                       in_=M[:, tl, :], in_offset=None),
                            [('POSI', tl), ('M', tl)] + [('xs_zero', a) for a in range(NTILE)], [('xs_sc', tl, k)])], sem='sc')
    all_sc = [('xs_sc', tl, k) for tl in range(NT_OWN) for k in range(2)]
    S.barrier()
    if debug:
        S.dma('sp', [(lambda e: e.dma_start(out=dbg['H'], in_=H.rearrange("p t d -> p (t d)")), [('H', t) for t in range(16)], []),
                     (lambda e: e.dma_start(out=dbg['comb'], in_=COMB.rearrange("p t d -> p (t d)")), [('COMB', t) for t in range(16)], [])], sem='dbg')
        S.barrier()
    es_o.close()
    if stop == 'O':
        S.barrier(); S.build(); return nc

    es_m = ExitStack()
    W13g = [sb(es_m, "W13g%d" % i, [128, 8 * 512], BF16) for i in range(3)]
    W2g = [sb(es_m, "W2g%d" % i, [128, 2 * D], BF16) for i in range(3)]
    XS = [sb(es_m, "XS%d" % i, [128, D], BF16) for i in range(4)]
    xsT = [sb(es_m, "xsT%d" % i, [128, 8, 128], BF16) for i in range(2)]
    sa = [sb(es_m, "sa%d" % i, [128, 256], F32) for i in range(2)]
    hid = [sb(es_m, "hid%d" % i, [128, 256], BF16) for i in range(2)]
    hidT = [sb(es_m, "hidT%d" % i, [128, 2, 128], BF16) for i in range(2)]
    yt = [sb(es_m, "yt%d" % i, [128, D], F32) for i in range(2)]

    bcreg = {}
    all_wcast = [('wcast', ex_) for ex_ in range(NE)]

    def _mk_bcreg(e):
        bcreg['r'] = e.to_reg(NE * 128 - 1)
        return None
    S.ops['pool'].append(([], _mk_bcreg, None, 0))

    def moeA_pre(j):
        b = j % 2
        wb = j % 3
        S.dma('pool', [
            (lambda e, j=j, wb=wb: e.indirect_dma_start(out=W13g[wb], out_offset=None, in_=w13b,
                                                        in_offset=bass.IndirectOffsetOnAxis(ap=IDXW[:, j:j + 1], axis=0),
                                                        bounds_check=bcreg['r'], oob_is_err=False), ['IDXW'] + all_wcast, [('W13g', wb)]),
            (lambda e, j=j, wb=wb: e.indirect_dma_start(out=W2g[wb], out_offset=None, in_=w2b,
                                                        in_offset=bass.IndirectOffsetOnAxis(ap=IDXW[:, j:j + 1], axis=0),
                                                        bounds_check=bcreg['r'], oob_is_err=False), ['IDXW'], [('W2g', wb)]),
        ], sem='wg%d' % wb)
        xb4 = j % 4
        for kc in range(8):
            S.op('pe', lambda e, kc=kc, b=b, xb4=xb4: e.transpose(out=pt[b][:, kc * 128:(kc + 1) * 128], in_=XS[xb4][:, kc * 128:(kc + 1) * 128], identity=ident),
                 reads=[('XS', xb4), 'ident'], writes=[PT[b]], signal=(kc == 7))
        S.op('dve', lambda e, b=b: e.tensor_tensor(out=xsT[b], in0=pt[b].rearrange("p (k t) -> p k t", k=8),
                                                   in1=gT_t[:, 8:16].unsqueeze(2).to_broadcast([128, 8, 128]), op=ALU.mult),
             reads=[PT[b], 'gT'], writes=[('xsT', b)])

    def moeA_mm(j):
        b = j % 2
        wb = j % 3
        for kc in range(8):
            S.op('pe', lambda e, kc=kc, b=b, wb=wb: e.matmul(ps[b], lhsT=xsT[b][:, kc, :], rhs=W13g[wb][:, kc * 512:(kc + 1) * 512], start=(kc == 0), stop=(kc == 7)),
                 reads=[('xsT', b), ('W13g', wb)], writes=[PS[b]], signal=(kc == 7))
        S.op('act', lambda e, b=b: e.activation(out=sa[b], in_=ps[b][:, 0:256], func=AF.Silu), reads=[PS[b]], writes=[('sa', b)])
        S.op('dve', lambda e, b=b: e.tensor_tensor(out=hid[b], in0=sa[b], in1=ps[b][:, 256:512], op=ALU.mult),
             reads=[('sa', b), PS[b]], writes=[('hid', b)])

    def moeB(j):
        b = j % 2
        wb = j % 3
        for ft in range(2):
            S.op('pe', lambda e, ft=ft, b=b: e.transpose(out=pt[b][:, ft * 128:(ft + 1) * 128], in_=hid[b][:, ft * 128:(ft + 1) * 128], identity=ident),
                 reads=[('hid', b), 'ident'], writes=[PT[b]], signal=(ft == 1))
        S.op('act', lambda e, b=b: e.activation(out=hidT[b], in_=pt[b][:, 0:256].rearrange("p (f t) -> p f t", f=2), func=AF.Copy),
             reads=[PT[b]], writes=[('hidT', b)])
        for half in range(2):
            pb = 2 + 2 * b + half
            for ft in range(2):
                S.op('pe', lambda e, ft=ft, half=half, b=b, pb=pb, wb=wb: e.matmul(
                    ps[pb], lhsT=hidT[b][:, ft, :], rhs=W2g[wb][:, ft * D + half * 512:ft * D + (half + 1) * 512], start=(ft == 0), stop=(ft == 1)),
                    reads=[('hidT', b), ('W2g', wb)], writes=[PS[pb]], signal=(ft == 1))
            if half == 0:
                S.op('act', lambda e, b=b, pb=pb: e.activation(out=yt[b][:, 0:512], in_=ps[pb], func=AF.Copy), reads=[PS[pb]], writes=[('yt', b)])
            else:
                S.op('dve', lambda e, b=b, pb=pb: e.tensor_copy(out=yt[b][:, 512:1024], in_=ps[pb]), reads=[PS[pb]], writes=[('yt', b)])
        S.dma('sp', [(lambda e, j=j, b=b: e.dma_start(out=outs_d[j * 128:(j + 1) * 128, :], in_=yt[b]), [('yt', b)], [('outs', j)])], sem='ost')

    def xsload(j):
        xb4 = j % 4
        S.dma('act', [(lambda e, j=j, xb4=xb4: e.dma_start(out=XS[xb4], in_=xs_d[j * 128:(j + 1) * 128, :]), all_sc, [('XS', xb4)])], sem='xsl%d' % xb4)

    for j in range(3):
        xsload(j)
    moeA_pre(0)
    moeA_mm(0)
    for j in range(NTILE):
        if j + 3 < NTILE:
            xsload(j + 3)
        if j + 1 < NTILE:
            moeA_pre(j + 1)
        moeB(j)
        if j + 1 < NTILE:
            moeA_mm(j + 1)
    if os.environ.get('SBUFDBG'):
        print("sbuf remaining after MoE alloc", nc.sbuf_bytes_remaining)
    all_outs = [('outs', j) for j in range(NTILE)]

    O12 = [sb(es_m, "O12_%d" % i, [128, 2, D], F32) for i in range(2)]
    for tl in range(NT_OWN):
        b = tl % 2
        S.dma('pool', [
            (lambda e, tl=tl, b=b, k=k: e.indirect_dma_start(out=O12[b][:, k, :], out_offset=None, in_=outs_d,
                                                             in_offset=bass.IndirectOffsetOnAxis(ap=POSI[:, tl, k:k + 1], axis=0)),
             all_outs + [('POSI', tl)], [('O12', b)]) for k in range(2)], sem='og%d' % b)
        for k in range(2):
            S.op('dve', lambda e, tl=tl, b=b, k=k: e.scalar_tensor_tensor(out=H[:, tl, :], in0=O12[b][:, k, :], scalar=CW2[:, tl, k:k + 1], in1=H[:, tl, :],
                                                                         op0=ALU.mult, op1=ALU.add),
                 reads=[('O12', b), ('CW2', tl), ('H', tl)], writes=[('H', tl)])
        s_ = st[b]
        sk = ('st', b)
        S.op('dve', lambda e, tl=tl, s_=s_: e.scalar_tensor_tensor(out=junk, in0=H[:, tl, :], scalar=1.0, in1=H[:, tl, :], op0=ALU.mult, op1=ALU.mult,
                                                                  accum_out=s_[:, 0:1]), reads=[('H', tl)], writes=['junk', sk])
        S.op('dve', lambda e, s_=s_: e.tensor_scalar(out=s_[:, 1:2], in0=s_[:, 0:1], scalar1=1.0 / D, scalar2=EPS, op0=ALU.mult, op1=ALU.add),
             reads=[sk], writes=[sk])
        S.op('act', lambda e, s_=s_: e.activation(out=s_[:, 3:4], in_=s_[:, 1:2], func=AF.Sqrt), reads=[sk], writes=[sk])
        S.op('dve', lambda e, s_=s_: e.reciprocal(out=s_[:, 2:3], in_=s_[:, 3:4]), reads=[sk], writes=[sk])
        S.op('dve', lambda e, tl=tl, s_=s_, b=b: e.scalar_tensor_tensor(out=xin[b], in0=H[:, tl, :], scalar=s_[:, 2:3], in1=gf_t, op0=ALU.mult, op1=ALU.mult),
             reads=[('H', tl), sk, 'gf'], writes=[('xin', b)])
        S.dma('sp', [(lambda e, tl=tl, b=b: e.dma_start(out=out[tl * 128:(tl + 1) * 128, :], in_=xin[b]), [('xin', b)], [])], sem='out')
    S.barrier()
    S.build()
    return nc


def _t5_bucket_np(n):
    n = np.maximum(n, 0)
    nf = np.maximum(n, 16).astype(np.float32)
    large = 16 + (np.log(nf / np.float32(16)) / np.float32(math.log(128 / 16)) * np.float32(16)).astype(np.int32)
    large = np.minimum(large, 31)
    return np.where(n < 16, n, large)


_PROG = {}


def _get_prog(debug=False):
    if debug not in _PROG:
        _PROG[debug] = build_program(debug)
    return _PROG[debug]


def make_in_maps(x, norm_mix_g, w_in, b_gates, gmlp_ln_g, gmlp_ln_b, w_spatial, b_spatial, rel_bias,
                 w_out, norm_ffn_g, w_group_router, b_group_router, w_expert_router, b_expert_router,
                 w1, w3, w2, norm_final_g):
    f = lambda a: np.ascontiguousarray(np.asarray(a), dtype=np.float32)
    x = f(x)
    w_in0 = f(w_in[0]); w_out0 = f(w_out[0]); w1_0 = f(w1[0]); w3_0 = f(w3[0]); w2_0 = f(w2[0])
    w13 = np.concatenate([w1_0, w3_0], axis=2).reshape(NE, 8, 128, 512)
    w13r = np.ascontiguousarray(np.transpose(w13, (0, 2, 1, 3))).reshape(NE * 128, 8 * 512)
    w2r = np.ascontiguousarray(np.transpose(w2_0.reshape(NE, 2, 128, D), (0, 2, 1, 3))).reshape(NE * 128, 2 * D)
    ub = np.concatenate([np.triu(np.ones((128, 128), np.float32), 1), np.ones((128, 128), np.float32)], axis=1).astype(ml_dtypes.bfloat16)
    tri32 = np.triu(np.ones((32, 32), np.float32), 1).astype(ml_dtypes.bfloat16)
    cst = np.zeros((128, 128), np.float32)
    cst[:, 0:64] = np.arange(64, dtype=np.float32)[None, :]
    cst[:, 64] = np.arange(128, dtype=np.float32)
    cst[:, 65:81] = (128.0 * np.arange(16, dtype=np.float32))[None, :]
    wr = np.concatenate([f(w_group_router[0]), np.transpose(f(w_expert_router[0]), (1, 0, 2)).reshape(D, 32)], axis=1)
    br = np.concatenate([f(b_group_router[0]), f(b_expert_router[0]).reshape(32)])
    brb = np.ascontiguousarray(np.broadcast_to(br[None, :], (128, 36)))
    gT = np.concatenate([f(norm_mix_g[0]).reshape(8, 128).T, f(norm_ffn_g[0]).reshape(8, 128).T], axis=1)
    bg = f(b_gates[0])
    rows = [f(gmlp_ln_g[0]), f(gmlp_ln_b[0]), bg[:D], bg[D:], f(norm_final_g), np.zeros(D, np.float32)]
    rowb = np.ascontiguousarray(np.stack([np.broadcast_to(r[None, :], (128, D)) for r in rows]))
    wsp = f(w_spatial[0])
    wspT = np.ascontiguousarray(np.transpose(wsp, (0, 2, 1)))
    bspT = np.ascontiguousarray(f(b_spatial[0]).T)
    rb = f(rel_bias)
    kk = np.arange(128)[:, None]
    qq = np.arange(128)[None, :]
    b0 = _t5_bucket_np(qq - kk)
    b1 = _t5_bucket_np(qq - kk + 128)
    rb0 = np.ascontiguousarray(np.stack([rb[b0, h] for h in range(8)]))
    rb1 = np.ascontiguousarray(np.stack([rb[b1, h] for h in range(8)]))
    chb = np.ascontiguousarray(np.broadcast_to(rb[31][None, :], (128, 8)))
    causal_add = np.where(qq >= kk, 0.0, -BIGM).astype(np.float32)
    tril = (np.arange(128)[None, :] <= np.arange(128)[:, None]).astype(np.float32)
    triu = np.ascontiguousarray(tril.T)
    cmk = np.ascontiguousarray(np.stack([causal_add, tril, triu]))
    ident = np.eye(128, dtype=np.float32).astype(ml_dtypes.bfloat16)
    e16 = np.zeros((16, 16, 128), np.float32)
    for s in range(16):
        e16[s, s, :] = 1.0
    e16 = e16.reshape(16, 16 * 128).astype(ml_dtypes.bfloat16)
    in_maps = []
    for c in range(8):
        b, p = c // 2, c % 2
        own = x[b, p * TOWN:(p + 1) * TOWN]
        oth = x[b, (1 - p) * TOWN:(2 - p) * TOWN]
        xa = np.ascontiguousarray(np.concatenate([own, oth], axis=0))
        pnm = np.full((16, 16), -BIGG, np.float32)
        for qt in range(16):
            i = qt // 2
            pnm[qt, :i] = 0.0
            if p == 1:
                pnm[qt, 8:] = 0.0
        pn = np.ascontiguousarray(np.broadcast_to(pnm.reshape(1, 256), (128, 256)))
        in_maps.append({
            "xa": xa, "w_in": w_in0, "w_out": w_out0, "w13r": w13r, "w2r": w2r, "ub": ub, "tri32": tri32, "cst": cst, "wr": np.ascontiguousarray(wr),
            "pn": pn, "gT": np.ascontiguousarray(gT), "rowb": rowb, "brb": brb, "wsp": wsp, "wspT": wspT, "bspT": bspT,
            "rb0": rb0, "rb1": rb1, "chb": chb, "cmk": cmk, "ident": ident, "e16": e16,
        })
    return in_maps


def kernel(**inputs):
    nc = _get_prog(False)
    in_maps = make_in_maps(**inputs)
    res = run_bass_kernel_spmd(nc, in_maps, core_ids=list(range(8)))
    outp = np.empty((4, SEQ, D), np.float32)
    for c in range(8):
        b, p = c // 2, c % 2
        outp[b, p * TOWN:(p + 1) * TOWN] = np.asarray(res.results[c]["out"], dtype=np.float32)
    return outp
```

```python
import math
from contextlib import ExitStack

import numpy as np
import ml_dtypes
import concourse.bass as bass
import concourse.mybir as mybir
from concourse.bass_utils import run_bass_kernel_spmd

F32 = mybir.dt.float32
BF16 = mybir.dt.bfloat16
ALU = mybir.AluOpType
AF = mybir.ActivationFunctionType
AX = mybir.AxisListType

D = 1024
SEQ = 4096
NB = 16
TOWN = 2048
NT_OWN = 16
NT_ALL = 32
EPS = 1e-6
SCALE = 128 ** -0.5
BIGM = 30000.0
BIGG = 1.0e9
NE = 32
NTILE = 64
OOB_ROW = 8192
NSLOT = NTILE * 128
I32 = mybir.dt.int32
DEBUG = False
import os
SKIP = set(os.environ.get('SKIP', '').split(','))


class Sched:
    def __init__(self, nc):
        self.nc = nc
        self.names = ['pe', 'act', 'dve', 'pool', 'sp']
        self.ops = {k: [] for k in self.names}
        self.sem = {k: nc.alloc_semaphore("s_" + k) for k in self.names}
        self.cnt = {k: 0 for k in self.names}
        self.last_w = {}
        self.readers = {}
        self.dsem = {}
        self.dcnt = {}
        self.waited = {k: {} for k in self.names}

    def _semh(self, sk):
        return self.sem[sk] if sk in self.sem else self.dsem[sk]

    def _deps(self, eng, reads, writes):
        need = {}

        def add(sk, v):
            if need.get(sk, 0) < v:
                need[sk] = v
        for k in reads:
            if k in self.last_w:
                add(*self.last_w[k])
        for k in writes:
            if k in self.last_w:
                add(*self.last_w[k])
            for sk, v in self.readers.get(k, {}).items():
                add(sk, v)
        waits = []
        for sk, v in need.items():
            if sk == eng and v > self.cnt[eng]:
                continue
            if self.waited[eng].get(sk, 0) >= v:
                continue
            self.waited[eng][sk] = v
            waits.append((sk, v))
        return waits

    def _record(self, tag, reads, writes):
        for k in reads:
            r = self.readers.setdefault(k, {})
            if r.get(tag[0], 0) < tag[1]:
                r[tag[0]] = tag[1]
        for k in writes:
            self.last_w[k] = tag
            self.readers[k] = {}

    def op(self, eng, fn, reads=(), writes=(), signal=True):
        waits = self._deps(eng, reads, writes)
        if signal:
            self.cnt[eng] += 1
            seq = self.cnt[eng]
        else:
            seq = self.cnt[eng] + 1
        self._record((eng, seq), reads, writes)
        self.ops[eng].append((waits, fn, self.sem[eng] if signal else None, 1))

    def dma(self, q, items, sem):
        if sem not in self.dsem:
            self.dsem[sem] = self.nc.alloc_semaphore("d_" + sem)
            self.dcnt[sem] = 0
        allr, allw = [], []
        for fn, reads, writes in items:
            allr += list(reads)
            allw += list(writes)
        waits = self._deps(q, allr, allw)
        first = True
        for fn, reads, writes in items:
            self.dcnt[sem] += 16
            self.ops[q].append((waits if first else [], fn, self.dsem[sem], 16))
            first = False
        self._record((sem, self.dcnt[sem]), allr, allw)

    def barrier(self):
        for e in self.names:
            waits = []
            for o in self.names:
                if self.cnt[o] > self.waited[e].get(o, 0):
                    self.waited[e][o] = self.cnt[o]
                    waits.append((o, self.cnt[o]))
            for s, c in self.dcnt.items():
                if c > self.waited[e].get(s, 0):
                    self.waited[e][s] = c
                    waits.append((s, c))
            self.ops[e].append((waits, None, None, 0))

    def build(self):
        nc = self.nc
        with nc.Block() as block:
            def mk(name):
                def body(e):
                    for waits, fn, sem, inc in self.ops[name]:
                        for sk, v in waits:
                            e.wait_ge(self._semh(sk), v)
                        if fn is not None:
                            ins = fn(e)
                            if sem is not None:
                                ins.then_inc(sem, inc)
                return body
            block.tensor(mk('pe'))
            block.scalar(mk('act'))
            block.vector(mk('dve'))
            block.gpsimd(mk('pool'))
            block.sync(mk('sp'))


def build_program(debug=False, stop=None):
    nc = bass.Bass("TRN2", target_bir_lowering=False)

    def din(name, shape, dt=F32):
        return nc.dram_tensor(name, list(shape), dt, kind="ExternalInput").ap()

    xa = din("xa", [SEQ, D])
    w_in = din("w_in", [D, 7 * D])
    w_out = din("w_out", [D, D])
    w13r = din("w13r", [NE * 128, 8 * 512])
    w2r = din("w2r", [NE * 128, 2 * D])
    ub_d = din("ub", [128, 256], BF16)
    tri32_d = din("tri32", [32, 32], BF16)
    cst_d = din("cst", [128, 128])
    xs_d = nc.dram_tensor("xs_scr", [NSLOT, D], BF16).ap()
    w13b = nc.dram_tensor("w13b_scr", [NE * 128, 8 * 512], BF16).ap()
    w2b = nc.dram_tensor("w2b_scr", [NE * 128, 2 * D], BF16).ap()
    outs_d = nc.dram_tensor("outs_scr", [NSLOT, D], F32).ap()
    wr = din("wr", [D, 36])
    pn = din("pn", [128, 16 * 16])
    gT = din("gT", [128, 16])
    rowb = din("rowb", [6, 128, D])
    brb = din("brb", [128, 36])
    wsp = din("wsp", [8, 128, 128])
    wspT = din("wspT", [8, 128, 128])
    bspT = din("bspT", [128, 8])
    rb0 = din("rb0", [8, 128, 128])
    rb1 = din("rb1", [8, 128, 128])
    chb = din("chb", [128, 8])
    cmk = din("cmk", [3, 128, 128])
    ident_d = din("ident", [128, 128], BF16)
    e16_d = din("e16", [16, 16 * 128], BF16)
    out = nc.dram_tensor("out", [TOWN, D], F32, kind="ExternalOutput").ap()
    dbg = {}
    if debug:
        dbg['yb'] = nc.dram_tensor("dbg_yb", [128, NT_OWN * D], BF16, kind="ExternalOutput").ap()
        dbg['M'] = nc.dram_tensor("dbg_M", [128, NT_OWN * D], BF16, kind="ExternalOutput").ap()
        dbg['H'] = nc.dram_tensor("dbg_H", [128, NT_OWN * D], F32, kind="ExternalOutput").ap()
        dbg['comb'] = nc.dram_tensor("dbg_comb", [128, NT_OWN * NE], F32, kind="ExternalOutput").ap()

    S = Sched(nc)
    es_all = ExitStack()

    def sb(es, name, shape, dt):
        return es.enter_context(nc.sbuf_tensor("sb_" + name, list(shape), dt)).ap()

    pt = [nc.alloc_psum_tensor("pt%d" % i, [128, 1024], BF16).ap() for i in range(2)]
    ps = [nc.alloc_psum_tensor("ps%d" % i, [128, 512], F32).ap() for i in range(6)]
    PT = [('pt', i) for i in range(2)]
    PS = [('ps', i) for i in range(6)]

    ident = sb(es_all, "ident", [128, 128], BF16)
    e16 = sb(es_all, "e16", [16, 16 * 128], BF16)
    cmk_t = sb(es_all, "cmk", [128, 3, 128], F32)
    gT_t = sb(es_all, "gT", [128, 16], F32)
    chb_t = sb(es_all, "chb", [128, 8], F32)
    pn_t = sb(es_all, "pn", [128, 256], F32)
    st = [sb(es_all, "st%d" % i, [128, 8], F32) for i in range(2)]
    M = sb(es_all, "M", [128, NT_OWN, D], BF16)
    xin = [sb(es_all, "xin%d" % i, [128, D], F32) for i in range(2)]
    junk = sb(es_all, "junk", [128, D], F32)
    zt = junk.bitcast(BF16)[:, 0:D]
    xb = [sb(es_all, "xb%d" % i, [128, D], BF16) for i in range(2)]

    S.dma('sp', [
        (lambda e: e.dma_start(out=ident, in_=ident_d), [], ['ident']),
        (lambda e: e.dma_start(out=e16, in_=e16_d), [], ['e16']),
        (lambda e: e.dma_start(out=cmk_t, in_=cmk.rearrange("a p q -> p a q")), [], ['cmk']),
        (lambda e: e.dma_start(out=gT_t, in_=gT), [], ['gT']),
        (lambda e: e.dma_start(out=chb_t, in_=chb), [], ['chb']),
        (lambda e: e.dma_start(out=pn_t, in_=pn), [], ['pn']),
    ], sem='const')

    def rms_tile(src_fn, src_keys, b, gcol, dstT, dst_key, tok0, q='sp', xb_dst=None, xb_key=None, stage='PQ'):
        xt = src_fn
        xbd = xb[b] if xb_dst is None else xb_dst
        xbk = ('xb', b) if xb_key is None else xb_key
        s_ = st[b]
        sk = ('st', b)
        if 'P' in stage:
            S.op('dve', lambda e: e.scalar_tensor_tensor(out=junk, in0=xt, scalar=1.0, in1=xt, op0=ALU.mult, op1=ALU.mult,
                                                         accum_out=s_[:, 0:1]), reads=src_keys, writes=['junk', sk])
            S.op('dve', lambda e: e.tensor_scalar(out=s_[:, 1:2], in0=s_[:, 0:1], scalar1=1.0 / D, scalar2=EPS,
                                                  op0=ALU.mult, op1=ALU.add), reads=[sk], writes=[sk])
            S.op('act', lambda e: e.activation(out=s_[:, 3:4], in_=s_[:, 1:2], func=AF.Sqrt), reads=[sk], writes=[sk])
            S.op('dve', lambda e: e.reciprocal(out=s_[:, 2:3], in_=s_[:, 3:4]), reads=[sk], writes=[sk])
            S.op('act', lambda e: e.activation(out=xbd, in_=xt, func=AF.Copy, scale=s_[:, 2:3]),
                 reads=src_keys + [sk], writes=[xbk])
        if 'Q' not in stage:
            return
        for kc in range(8):
            S.op('pe', lambda e, kc=kc: e.transpose(out=pt[b][:, kc * 128:(kc + 1) * 128], in_=xbd[:, kc * 128:(kc + 1) * 128],
                                                    identity=ident),
                 reads=[xbk, 'ident'], writes=[PT[b]], signal=(kc == 7))
        S.op('dve', lambda e: e.tensor_tensor(out=dstT[:, :, tok0:tok0 + 128],
                                              in0=pt[b].rearrange("p (k t) -> p k t", k=8),
                                              in1=gT_t[:, gcol:gcol + 8].unsqueeze(2).to_broadcast([128, 8, 128]), op=ALU.mult),
             reads=[PT[b], 'gT'], writes=[dst_key])

    es_x = ExitStack()
    xnT_own = sb(es_x, "xnT_own", [128, 8, TOWN], BF16)
    es_x2 = ExitStack()
    xnT_oth = sb(es_x2, "xnT_oth", [128, 8, TOWN], BF16)

    def xs(kc, tok0, n):
        if tok0 < TOWN:
            return xnT_own[:, kc, tok0:tok0 + n]
        return xnT_oth[:, kc, tok0 - TOWN:tok0 - TOWN + n]
    es_a = ExitStack()
    V4 = sb(es_a, "V4", [128, NT_ALL, 4, 130], BF16)
    Wv4 = sb(es_a, "Wv4", [128, 8, 512], BF16)
    KT = sb(es_a, "KT", [128, SEQ], BF16)
    QT = sb(es_a, "QT", [128, TOWN], BF16)
    Wqk = [sb(es_a, "Wqk%d" % i, [128, 2, 8, 128], BF16) for i in range(2)]
    Tt = [sb(es_a, "Tt%d" % i, [128, 2, 256], F32) for i in range(2)]
    kmsf = sb(es_a, "kmsf", [128, 16], F32)
    kms = sb(es_a, "kms", [128, 16], BF16)
    gm = sb(es_a, "gm", [128, 16, 16], F32)
    ga = sb(es_a, "ga", [128, 16, 16], F32)
    gb_ = sb(es_a, "gb_", [128, 16, 16], F32)
    top8 = sb(es_a, "top8", [128, 16, 8], F32)
    PTs = [sb(es_a, "PTs%d" % i, [128, 512], BF16) for i in range(4)]
    stmp = [sb(es_a, "stmp%d" % i, [128, 256], F32) for i in range(2)]
    rden = sb(es_a, "rden", [128, 4], F32)
    acc_sb = sb(es_a, "acc_sb", [128, 4, 130], F32)

    S.op('pool', lambda e: e.memset(V4[:, :, :, 128:130], 1.0), writes=['V4ones'])
    S.op('pool', lambda e: e.memset(junk.bitcast(BF16), 0.0), writes=['junk'])

    def v4_tile(tt):
        pb = tt % 2
        for kc in range(8):
            S.op('pe', lambda e, kc=kc, tt=tt, pb=pb: e.matmul(ps[pb], lhsT=xs(kc, tt * 128, 128), rhs=Wv4[:, kc, :],
                                                               start=(kc == 0), stop=(kc == 7)),
                 reads=[('xnT', tt), 'Wv4'], writes=[PS[pb]], signal=(kc == 7))
        S.op('dve', lambda e, tt=tt, pb=pb: e.tensor_copy(out=V4[:, tt, :, 0:128], in_=ps[pb].rearrange("p (h c) -> p h c", h=4)),
             reads=[PS[pb]], writes=[('V4', tt)])

    S.dma('pool', [(lambda e: e.dma_start(out=Wv4, in_=w_in[:, 4096:4096 + 512].rearrange("(k p) c -> p k c", p=128)), [], ['Wv4'])], sem='wv')
    def a1(tt, stage):
        b = tt % 2
        if 'P' in stage:
            S.dma('sp', [(lambda e, tt=tt, b=b: e.dma_start(out=xin[b], in_=xa[tt * 128:(tt + 1) * 128, :]), [], [('xin', b)])],
                  sem='xin%d' % b)
        rms_tile(xin[b], [('xin', b)], b, 0, xnT_own if tt < 16 else xnT_oth, ('xnT', tt), (tt % 16) * 128, stage=stage)

    a1(0, 'P')
    for tt in range(NT_ALL):
        if tt + 1 < NT_ALL:
            a1(tt + 1, 'P')
        a1(tt, 'Q')
        if tt >= 1:
            v4_tile(tt - 1)
    v4_tile(NT_ALL - 1)
    all_xnT = [('xnT', tt) for tt in range(NT_ALL)]
    stb = [ps[0], ps[1], pt[0].bitcast(F32), pt[1].bitcast(F32)]
    STK = [PS[0], PS[1], PT[0], PT[1]]
    ptc = 0
    psc = 0
    stc = 0
    gsel = 0
    for hg in range(2):
        c0 = 4096 + hg * 512
        if hg == 1:
            S.dma('pool', [(lambda e, c0=c0: e.dma_start(out=Wv4, in_=w_in[:, c0:c0 + 512].rearrange("(k p) c -> p k c", p=128)),
                            [], ['Wv4'])], sem='wv')
            for tt in range(NT_ALL):
                v4_tile(tt)
        if stop == 'V':
            S.barrier(); S.build(); return nc
        for hh in range(4):
            h = hg * 4 + hh
            wb = h % 2
            cq = 2048 + h * 128
            ck = 3072 + h * 128
            S.dma('pool', [
                (lambda e, cq=cq, wb=wb: e.dma_start(out=Wqk[wb][:, 0], in_=w_in[:, cq:cq + 128].rearrange("(k p) c -> p k c", p=128)), [], [('Wqk', wb)]),
                (lambda e, ck=ck, wb=wb: e.dma_start(out=Wqk[wb][:, 1], in_=w_in[:, ck:ck + 128].rearrange("(k p) c -> p k c", p=128)), [], []),
            ], sem='wqk%d' % wb)
            for ex_ in range(4 * h, 4 * h + 4):
                S.dma('pool', [
                    (lambda e, ex_=ex_: e.dma_start(out=w13b[ex_ * 128:(ex_ + 1) * 128, :], in_=w13r[ex_ * 128:(ex_ + 1) * 128, :]), [], [('wcast', ex_)]),
                    (lambda e, ex_=ex_: e.dma_start(out=w2b[ex_ * 128:(ex_ + 1) * 128, :], in_=w2r[ex_ * 128:(ex_ + 1) * 128, :]), [], []),
                ], sem='wcast')
            S.dma('sp', [(lambda e, a=a: e.dma_start(out=xs_d[a * 128:(a + 1) * 128, :], in_=zt), ['junk'], [('xs_zero', a)])
                         for a in range(8 * h, 8 * h + 8)], sem='xz')
            S.dma('sp', [
                (lambda e, h=h, wb=wb: e.dma_start(out=Tt[wb][:, 0, 0:128], in_=rb0[h]), [], [('Tt', wb)]),
                (lambda e, h=h, wb=wb: e.dma_start(out=Tt[wb][:, 0, 128:256], in_=rb1[h]), [], []),
                (lambda e, h=h, wb=wb: e.dma_start(out=Tt[wb][:, 1, 0:128], in_=rb1[h]), [], []),
            ], sem='tt%d' % wb)
            if 'tt1' not in SKIP:
              S.op('dve', lambda e, wb=wb: e.tensor_tensor(out=Tt[wb][:, 0, 0:128], in0=Tt[wb][:, 0, 0:128], in1=cmk_t[:, 0, :], op=ALU.add),
                 reads=[('Tt', wb), 'cmk'], writes=[('Tt', wb)])
            if 'tt2' not in SKIP:
              S.op('dve', lambda e, wb=wb, h=h: e.tensor_scalar(out=Tt[wb][:, 1, 128:256], in0=cmk_t[:, 1, :], scalar1=0.0, scalar2=chb_t[:, h:h + 1],
                                                            op0=ALU.mult, op1=ALU.add),
                 reads=[('Tt', wb), 'cmk', 'chb'], writes=[('Tt', wb)])
            for tg in range(8):
                pb = tg % 2
                for kc in range(8):
                    S.op('pe', lambda e, kc=kc, tg=tg, pb=pb, wb=wb: e.matmul(ps[pb], lhsT=Wqk[wb][:, 1, kc, :], rhs=xs(kc, tg * 512, 512),
                                                                              start=(kc == 0), stop=(kc == 7)),
                         reads=all_xnT[tg * 4:(tg + 1) * 4] + [('Wqk', wb)], writes=[PS[pb]], signal=(kc == 7))
                for bk in range(2):
                    S.op('act', lambda e, tg=tg, pb=pb, bk=bk: e.activation(out=KT[:, tg * 512 + bk * 256:tg * 512 + (bk + 1) * 256],
                                                                         in_=ps[pb][:, bk * 256:(bk + 1) * 256], func=AF.Copy,
                                                                         accum_out=kmsf[:, 2 * tg + bk:2 * tg + bk + 1]),
                         reads=[PS[pb]], writes=[('KT', tg), 'kmsf'])
            S.op('dve', lambda e: e.tensor_copy(out=kms, in_=kmsf), reads=['kmsf'], writes=['kms'])
            for tg in range(4):
                pb = tg % 2
                for kc in range(8):
                    S.op('pe', lambda e, kc=kc, tg=tg, pb=pb, wb=wb: e.matmul(ps[pb], lhsT=Wqk[wb][:, 0, kc, :], rhs=xs(kc, tg * 512, 512),
                                                                              start=(kc == 0), stop=(kc == 7)),
                         reads=all_xnT[tg * 4:(tg + 1) * 4] + [('Wqk', wb)], writes=[PS[pb]], signal=(kc == 7))
                S.op('dve', lambda e, tg=tg, pb=pb: e.tensor_copy(out=QT[:, tg * 512:(tg + 1) * 512], in_=ps[pb]),
                     reads=[PS[pb]], writes=[('QT', tg)])
            all_QT = [('QT', tg) for tg in range(4)]
            if stop == 'KQ':
                S.barrier(); S.build(); return nc
            for qt in range(16):
                S.op('pe', lambda e, qt=qt: e.matmul(ps[5][:, qt * 16:(qt + 1) * 16], lhsT=QT[:, qt * 128:(qt + 1) * 128], rhs=kms, start=True, stop=True),
                     reads=[('QT', qt // 4), 'kms'], writes=[PS[5]], signal=(qt == 15))
            S.op('dve', lambda e: e.tensor_tensor(out=gm, in0=ps[5][:, 0:256].rearrange("p (a b) -> p a b", a=16),
                                                  in1=pn_t.rearrange("p (a b) -> p a b", a=16), op=ALU.add),
                 reads=[PS[5], 'pn'], writes=['gm'])
            for qt in range(16):
                S.op('dve', lambda e, qt=qt: e.max(out=top8[:, qt, :], in_=gm[:, qt, :]), reads=['gm'], writes=['top8'])
            S.op('dve', lambda e: e.tensor_tensor(out=ga, in0=gm, in1=top8[:, :, 2:3].to_broadcast([128, 16, 16]), op=ALU.is_ge),
                 reads=['gm', 'top8'], writes=['ga'])
            S.op('dve', lambda e: e.tensor_scalar(out=gb_, in0=gm, scalar1=-0.5 * BIGG, scalar2=None, op0=ALU.is_gt),
                 reads=['gm'], writes=['gb_'])
            S.op('dve', lambda e: e.tensor_tensor(out=ga, in0=ga, in1=gb_, op=ALU.mult), reads=['ga', 'gb_'], writes=['ga'])
            if stop == 'SEL':
                S.barrier(); S.build(); return nc
            for pr in range(4):
                i0, i1 = 2 * pr, 2 * pr + 1
                adj0 = i0 - 1 if i0 >= 1 else 15
                groups = []
                for s_ in [x for x in range(i0)] + [x for x in range(8, 16)]:
                    us = []
                    for kt in range(2):
                        sp = [('T1', 0, 256)] if (s_ == adj0 and kt == 1) else []
                        us.append(dict(q0=i0 * 256, n=512, ktile=s_ * 2 + kt, special=sp, accs=[(0, 0), (1, 128), (2, 256), (3, 384)]))
                    groups.append(dict(units=us, sel=s_, accs=[0, 1, 2, 3]))
                groups.append(dict(units=[
                    dict(q0=i1 * 256, n=256, ktile=i0 * 2, special=[], accs=[(2, 0), (3, 128)]),
                    dict(q0=i1 * 256, n=256, ktile=i0 * 2 + 1, special=[('T1', 0, 256)], accs=[(2, 0), (3, 128)])], sel=i0, accs=[2, 3]))
                for ii, ab in ((i0, 0), (i1, 2)):
                    groups.append(dict(units=[
                        dict(q0=ii * 256, n=256, ktile=ii * 2, special=[('T0', 0, 256)], accs=[(ab, 0), (ab + 1, 128)]),
                        dict(q0=ii * 256 + 128, n=128, ktile=ii * 2 + 1, special=[('T0', 0, 128)], accs=[(ab + 1, 0)])], sel=None, accs=[ab, ab + 1]))
                units = []
                for gi, g_ in enumerate(groups):
                    g_['bs'] = gsel % 2
                    gsel += 1
                    for u in g_['units']:
                        u['g'] = g_
                        units.append(u)
                    g_['last'] = units[-1]
                    g_['banks_started'] = set()
                    g_['lastmm'] = {}
                    for u in g_['units']:
                        for a_, _ in u['accs']:
                            g_['lastmm'][a_] = id(u)
                inited = set()

                def pbank(g_, a_):
                    return 2 + 2 * g_['bs'] + a_ // 2

                def pacc(g_, a_):
                    return ps[pbank(g_, a_)][:, (a_ % 2) * 256:(a_ % 2) * 256 + 129]

                def stage1(u):
                    pb = u['pb']
                    n, q0, kt_ = u['n'], u['q0'], u['ktile']
                    qkeys = [('QT', (q0 + c) // 512) for c in range(0, n, 256)] if n >= 256 else [('QT', q0 // 512)]
                    S.op('pe', lambda e, pb=pb, n=n, q0=q0, kt_=kt_: e.matmul(
                        stb[pb][:, 0:n], lhsT=KT[:, kt_ * 128:(kt_ + 1) * 128], rhs=QT[:, q0:q0 + n], start=True, stop=True),
                        reads=[('KT', kt_ // 4)] + qkeys, writes=[STK[pb]], signal=True)

                def stage2(u):
                    pb, pj, n = u['pb'], u['pj'], u['n']
                    c_done = 0
                    for (tk, c0_, c1_) in u['special']:
                        sj = u['sj']
                        tsel = 0 if tk == 'T0' else 1
                        S.op('dve', lambda e, pb=pb, sj=sj, c0_=c0_, c1_=c1_, tsel=tsel, wb=wb: e.scalar_tensor_tensor(
                            out=stmp[sj][:, c0_:c1_], in0=stb[pb][:, c0_:c1_], scalar=SCALE, in1=Tt[wb][:, tsel, 0:c1_ - c0_], op0=ALU.mult, op1=ALU.add),
                            reads=[STK[pb], ('Tt', wb)], writes=[('stmp', sj)])
                        S.op('act', lambda e, sj=sj, pj=pj, c0_=c0_, c1_=c1_: e.activation(out=PTs[pj][:, c0_:c1_], in_=stmp[sj][:, c0_:c1_], func=AF.Exp),
                             reads=[('stmp', sj)], writes=[('PTs', pj)])
                        c_done = c1_
                    if c_done < n:
                        S.op('act', lambda e, pb=pb, pj=pj, c_done=c_done, n=n, h=h: e.activation(
                            out=PTs[pj][:, c_done:n], in_=stb[pb][:, c_done:n], func=AF.Exp, bias=chb_t[:, h:h + 1], scale=SCALE),
                            reads=[STK[pb], 'chb'], writes=[('PTs', pj)])

                def stage3(g_):
                    for a_ in g_['accs']:
                        bk = pbank(g_, a_)
                        ua = [(u, off) for u in g_['units'] for (aa, off) in u['accs'] if aa == a_]
                        for k_, (u, off) in enumerate(ua):
                            pj, kt_ = u['pj'], u['ktile']
                            S.op('pe', lambda e, g_=g_, a_=a_, off=off, pj=pj, kt_=kt_, st_=(k_ == 0), sp_=(k_ == len(ua) - 1), hh=hh: e.matmul(
                                pacc(g_, a_), lhsT=PTs[pj][:, off:off + 128], rhs=V4[:, kt_, hh, 0:129], start=st_, stop=sp_),
                                reads=[('PTs', pj), ('V4', kt_), 'V4ones'], writes=[PS[bk]], signal=(k_ == len(ua) - 1))
                    for a_ in g_['accs']:
                        qt = i0 * 2 + a_
                        bk = pbank(g_, a_)
                        dst = acc_sb[:, a_, 0:129]
                        if g_['sel'] is None:
                            S.op('dve', lambda e, g_=g_, a_=a_, dst=dst: e.tensor_tensor(out=dst, in0=pacc(g_, a_), in1=dst, op=ALU.add),
                                 reads=[PS[bk], ('acc_sb', a_)], writes=[('acc_sb', a_)])
                        elif a_ not in inited:
                            S.op('dve', lambda e, g_=g_, a_=a_, dst=dst, qt=qt: e.tensor_scalar(
                                out=dst, in0=pacc(g_, a_), scalar1=ga[:, qt, g_['sel']:g_['sel'] + 1], scalar2=None, op0=ALU.mult),
                                reads=[PS[bk], 'ga'], writes=[('acc_sb', a_)])
                        else:
                            S.op('dve', lambda e, g_=g_, a_=a_, dst=dst, qt=qt: e.scalar_tensor_tensor(
                                out=dst, in0=pacc(g_, a_), scalar=ga[:, qt, g_['sel']:g_['sel'] + 1], in1=dst, op0=ALU.mult, op1=ALU.add),
                                reads=[PS[bk], 'ga', ('acc_sb', a_)], writes=[('acc_sb', a_)])
                        inited.add(a_)

                def s12(g_):
                    nonlocal_counters = None
                    for u in g_['units']:
                        stage1(u)
                        stage2(u)

                for g_ in groups:
                    for u in g_['units']:
                        u['pb'] = psc % 4
                        psc += 1
                        u['pj'] = ptc % 4
                        ptc += 1
                        if u['special']:
                            u['sj'] = stc % 2
                            stc += 1
                s12(groups[0])
                for gi, g_ in enumerate(groups):
                    if gi + 1 < len(groups):
                        s12(groups[gi + 1])
                    stage3(g_)
                for a_ in range(4):
                    tl = i0 * 2 + a_
                    S.op('dve', lambda e, a_=a_: e.reciprocal(out=rden[:, a_:a_ + 1], in_=acc_sb[:, a_, 128:129]),
                         reads=[('acc_sb', a_)], writes=['rden'])
                    S.op('dve', lambda e, a_=a_, tl=tl, h=h: e.tensor_scalar(out=M[:, tl, h * 128:(h + 1) * 128], in0=acc_sb[:, a_, 0:128],
                                                                            scalar1=rden[:, a_:a_ + 1], scalar2=None, op0=ALU.mult),
                         reads=[('acc_sb', a_), 'rden'], writes=[('M', tl)])
    S.barrier()
    if debug:
        S.dma('sp', [(lambda e: e.dma_start(out=dbg['yb'], in_=M.rearrange("p t d -> p (t d)")), [('M', t) for t in range(16)], [])], sem='dbg')
        S.barrier()
    es_a.close()
    es_x2.close()
    if stop == 'ATT':
        S.barrier(); S.build(); return nc

    es_g = ExitStack()
    Wseg = [sb(es_g, "Wseg%d" % i, [128, 8, D], BF16) for i in range(2)]
    rows = sb(es_g, "rows", [128, 4, D], F32)
    lnbt = sb(es_g, "lnbt", [128, D], F32)
    wtmp = sb(es_g, "wtmp", [128, 8, 128], F32)
    wsT = sb(es_g, "wsT", [128, 8, 128], BF16)
    rs = sb(es_g, "rs", [128, 8], F32)
    bsp_t = sb(es_g, "bsp", [128, 8], F32)
    vg2 = [sb(es_g, "vg%d" % i, [128, D], F32) for i in range(2)]
    vhat2 = [sb(es_g, "vhat%d" % i, [128, D], BF16) for i in range(2)]
    sm2 = [sb(es_g, "sm%d" % i, [128, 8], F32) for i in range(2)]
    tmpa = [sb(es_g, "tmpa%d" % i, [128, 512], F32) for i in range(2)]
    tmpb2 = [[sb(es_g, "tmpb%d_%d" % (t, i), [128, 512], F32) for i in range(2)] for t in range(2)]
    M2 = sb(es_g, "M2", [128, NT_OWN, D], BF16)

    S.dma('sp', [
        (lambda e: e.dma_start(out=rows[:, 0, :], in_=rowb[0]), [], ['rows']),
        (lambda e: e.dma_start(out=lnbt, in_=rowb[1]), [], ['lnbt']),
        (lambda e: e.dma_start(out=rows[:, 2, :], in_=rowb[2]), [], []),
        (lambda e: e.dma_start(out=rows[:, 3, :], in_=rowb[3]), [], []),
        (lambda e: e.dma_start(out=bsp_t, in_=bspT), [], ['bsp']),
        (lambda e: e.dma_start(out=wtmp, in_=wsp.rearrange("g t s -> t g s")), [], ['wtmp']),
    ], sem='gconst')
    S.op('dve', lambda e: e.tensor_tensor(out=wtmp, in0=wtmp, in1=cmk_t[:, 1:2, :].to_broadcast([128, 8, 128]), op=ALU.mult),
         reads=['wtmp', 'cmk'], writes=['wtmp'])
    S.op('dve', lambda e: e.reduce_sum(out=rs, in_=wtmp, axis=AX.X), reads=['wtmp'], writes=['rs'])
    for g in range(8):
        S.op('dve', lambda e, g=g: e.tensor_scalar(out=rows[:, 1, g * 128:(g + 1) * 128], in0=lnbt[:, g * 128:(g + 1) * 128],
                                                  scalar1=rs[:, g:g + 1], scalar2=bsp_t[:, g:g + 1], op0=ALU.mult, op1=ALU.add),
             reads=['lnbt', 'rs', 'bsp', 'rows'], writes=['rows'])
    S.dma('sp', [(lambda e: e.dma_start(out=wtmp, in_=wspT.rearrange("g s t -> s g t")), [], ['wtmp'])], sem='gconst')
    S.op('dve', lambda e: e.tensor_tensor(out=wsT, in0=wtmp, in1=cmk_t[:, 2:3, :].to_broadcast([128, 8, 128]), op=ALU.mult),
         reads=['wtmp', 'cmk'], writes=['wsT'])

    seg_cols = {'v': 1024, 'u': 0, 'ga': 5120, 'gb': 6144}
    pend_y = None
    pend_mix = None
    for si, seg in enumerate(['v', 'u', 'ga', 'gb']):
        wbuf = si % 2
        c0 = seg_cols[seg]
        S.dma('pool', [(lambda e, c0=c0, wbuf=wbuf: e.dma_start(out=Wseg[wbuf], in_=w_in[:, c0:c0 + D].rearrange("(k p) c -> p k c", p=128)),
                        [], [('Wseg', wbuf)])], sem='wseg%d' % wbuf)
        for tl in range(NT_OWN):
            pp = (tl % 2) * 2 if seg == 'v' else (tl % 3) * 2
            for half in range(2):
                pb = pp + half
                for kc in range(8):
                    S.op('pe', lambda e, kc=kc, tl=tl, pb=pb, half=half, wbuf=wbuf: e.matmul(
                        ps[pb], lhsT=xs(kc, tl * 128, 128), rhs=Wseg[wbuf][:, kc, half * 512:(half + 1) * 512],
                        start=(kc == 0), stop=(kc == 7)),
                        reads=[('xnT', tl), ('Wseg', wbuf)], writes=[PS[pb]], signal=(kc == 7))
            if seg == 'v':
                vb = tl % 2
                vg, vhat, sm = vg2[vb], vhat2[vb], sm2[vb]
                kvg, kvh, ksm = ('vg', vb), ('vhat', vb), ('sm', vb)
                for half in range(2):
                    pb = pp + half
                    S.op('act', lambda e, half=half, pb=pb, vg=vg, sm=sm: e.activation(out=vg[:, half * 512:(half + 1) * 512], in_=ps[pb], func=AF.Gelu,
                                                                                     accum_out=sm[:, half:half + 1]),
                         reads=[PS[pb]], writes=[kvg, ksm])
                S.op('dve', lambda e, vg=vg, sm=sm: e.scalar_tensor_tensor(out=junk, in0=vg, scalar=1.0, in1=vg, op0=ALU.mult, op1=ALU.mult, accum_out=sm[:, 2:3]),
                     reads=[kvg], writes=['junk', ksm])
                S.op('dve', lambda e, sm=sm: e.tensor_tensor(out=sm[:, 3:4], in0=sm[:, 0:1], in1=sm[:, 1:2], op=ALU.add), reads=[ksm], writes=[ksm])
                S.op('dve', lambda e, sm=sm: e.tensor_scalar(out=sm[:, 3:4], in0=sm[:, 3:4], scalar1=1.0 / D, scalar2=None, op0=ALU.mult), reads=[ksm], writes=[ksm])
                S.op('dve', lambda e, sm=sm: e.tensor_tensor(out=sm[:, 4:5], in0=sm[:, 3:4], in1=sm[:, 3:4], op=ALU.mult), reads=[ksm], writes=[ksm])
                S.op('dve', lambda e, sm=sm: e.scalar_tensor_tensor(out=sm[:, 5:6], in0=sm[:, 2:3], scalar=1.0 / D, in1=sm[:, 4:5], op0=ALU.mult, op1=ALU.subtract),
                     reads=[ksm], writes=[ksm])
                S.op('dve', lambda e, sm=sm: e.tensor_scalar(out=sm[:, 5:6], in0=sm[:, 5:6], scalar1=EPS, scalar2=None, op0=ALU.add), reads=[ksm], writes=[ksm])
                S.op('act', lambda e, sm=sm: e.activation(out=sm[:, 6:7], in_=sm[:, 5:6], func=AF.Sqrt), reads=[ksm], writes=[ksm])
                S.op('dve', lambda e, sm=sm: e.reciprocal(out=sm[:, 7:8], in_=sm[:, 6:7]), reads=[ksm], writes=[ksm])
                S.op('dve', lambda e, vg=vg, vhat=vhat, sm=sm: e.tensor_scalar(out=vhat, in0=vg, scalar1=sm[:, 3:4], scalar2=sm[:, 7:8], op0=ALU.subtract, op1=ALU.mult),
                     reads=[kvg, ksm], writes=[kvh])

                def mix(tl=tl, vhat=vhat, kvh=kvh):
                    for g in range(8):
                        S.op('pe', lambda e, g=g: e.matmul(ps[4 + g // 4][:, (g % 4) * 128:(g % 4 + 1) * 128], lhsT=wsT[:, g, :], rhs=vhat[:, g * 128:(g + 1) * 128],
                                                          start=True, stop=True),
                             reads=['wsT', kvh], writes=[PS[4 + g // 4]], signal=(g % 4 == 3))
                    for half in range(2):
                        S.op('dve', lambda e, half=half: e.tensor_tensor(out=tmpa[half], in0=ps[4 + half], in1=rows[:, 0, half * 512:(half + 1) * 512], op=ALU.mult),
                             reads=[PS[4 + half], 'rows'], writes=[('tmpa', half)])
                        S.op('dve', lambda e, half=half: e.tensor_tensor(out=M2[:, tl, half * 512:(half + 1) * 512], in0=tmpa[half],
                                                                        in1=rows[:, 1, half * 512:(half + 1) * 512], op=ALU.add),
                             reads=[('tmpa', half), 'rows'], writes=[('M2', tl)])
                if pend_mix is not None:
                    pend_mix()
                pend_mix = mix
                if tl == NT_OWN - 1:
                    pend_mix()
                    pend_mix = None
            else:
                tb = tl % 2
                for half in range(2):
                    pb = pp + half
                    if seg == 'u':
                        S.op('act', lambda e, half=half, pb=pb, tb=tb: e.activation(out=tmpb2[tb][half], in_=ps[pb], func=AF.Gelu),
                             reads=[PS[pb]], writes=[('tmpb', tb, half)])
                    else:
                        rsel = 2 if seg == 'ga' else 3
                        S.op('dve', lambda e, half=half, pb=pb, rsel=rsel: e.tensor_tensor(out=tmpa[half], in0=ps[pb], in1=rows[:, rsel, half * 512:(half + 1) * 512], op=ALU.add),
                             reads=[PS[pb], 'rows'], writes=[('tmpa', half)])
                        S.op('act', lambda e, half=half, tb=tb: e.activation(out=tmpb2[tb][half], in_=tmpa[half], func=AF.Sigmoid),
                             reads=[('tmpa', half)], writes=[('tmpb', tb, half)])

                def yst(tl=tl, tb=tb, seg=seg):
                    for half in range(2):
                        cs = slice(half * 512, (half + 1) * 512)
                        if seg in ('u', 'ga'):
                            S.op('dve', lambda e, half=half, cs=cs: e.tensor_tensor(out=M2[:, tl, cs], in0=M2[:, tl, cs], in1=tmpb2[tb][half], op=ALU.mult),
                                 reads=[('tmpb', tb, half), ('M2', tl)], writes=[('M2', tl)])
                        else:
                            S.op('dve', lambda e, half=half, cs=cs: e.tensor_tensor(out=tmpb2[tb][half], in0=tmpb2[tb][half], in1=M[:, tl, cs], op=ALU.mult),
                                 reads=[('tmpb', tb, half), ('M', tl)], writes=[('tmpb', tb, half)])
                            S.op('dve', lambda e, half=half, cs=cs: e.tensor_tensor(out=M[:, tl, cs], in0=tmpb2[tb][half], in1=M2[:, tl, cs], op=ALU.add),
                                 reads=[('tmpb', tb, half), ('M2', tl)], writes=[('M', tl)])
                if pend_y is not None:
                    pend_y()
                pend_y = yst
                if tl == NT_OWN - 1:
                    pend_y()
                    pend_y = None
    S.barrier()
    if debug:
        S.dma('sp', [(lambda e: e.dma_start(out=dbg['M'], in_=M.rearrange("p t d -> p (t d)")), [('M', t) for t in range(16)], [])], sem='dbg')
        S.barrier()
    es_g.close()
    es_x.close()
    if stop == 'G':
        S.barrier(); S.build(); return nc

    es_h = ExitStack()
    H = sb(es_h, "H", [128, NT_OWN, D], F32)
    COMB = sb(es_h, "COMB", [128, NT_OWN, NE], F32)
    gf_t = sb(es_h, "gf", [128, D], F32)
    MselF = sb(es_h, "MselF", [128, NT_OWN, NE], F32)
    MselB = sb(es_h, "MselB", [128, NT_OWN, NE], BF16)
    RANK = sb(es_h, "RANK", [128, NT_OWN, NE], F32)
    POSI = sb(es_h, "POSI", [128, NT_OWN, 2], I32)
    CW2 = sb(es_h, "CW2", [128, NT_OWN, 2], F32)
    IDXW = sb(es_h, "IDXW", [128, NTILE], I32)
    cst_t = sb(es_h, "cst", [128, 128], F32)
    es_o = ExitStack()
    xn2T = sb(es_o, "xn2T", [128, 8, TOWN], BF16)
    ub_t = sb(es_o, "ub", [128, 256], BF16)
    tri32_t = sb(es_o, "tri32", [32, 32], BF16)
    cnt = sb(es_o, "cnt", [128, NE], F32)
    tle = sb(es_o, "tle", [128, NE], F32)
    tlb = sb(es_o, "tlb", [128, NE], BF16)
    tT = sb(es_o, "tT", [32, 128], BF16)
    start = sb(es_o, "start", [128, NE], F32)
    start128 = sb(es_o, "start128", [128, NE], F32)
    ej = sb(es_o, "ej", [128, NTILE], F32)
    ej2 = sb(es_o, "ej2", [128, NTILE], F32)
    tmp32 = sb(es_o, "tmp32", [128, NE], F32)
    p8 = sb(es_o, "p8", [128, 8], F32)
    Wout = sb(es_o, "Wout", [128, 8, D], BF16)
    MT = [sb(es_o, "MT%d" % i, [128, 8, 128], BF16) for i in range(2)]
    Wr = sb(es_o, "Wr", [128, 8, 36], BF16)
    brb_t = sb(es_o, "brb", [128, 36], F32)
    lg2 = [sb(es_o, "lg%d" % i, [128, 36], F32) for i in range(2)]
    r_2 = [sb(es_o, "r_%d" % i, [128, 8], F32) for i in range(2)]
    oh2 = [sb(es_o, "oh%d" % i, [128, 4], F32) for i in range(2)]
    ge2 = [sb(es_o, "ge%d" % i, [128, 4], F32) for i in range(2)]
    elm2 = [sb(es_o, "elm%d" % i, [128, 32], F32) for i in range(2)]
    t82 = [sb(es_o, "t8%d" % i, [128, 8], F32) for i in range(2)]
    pe_2 = [sb(es_o, "pe_%d" % i, [128, 32], F32) for i in range(2)]
    pm2 = [sb(es_o, "pm%d" % i, [128, 32], F32) for i in range(2)]

    S.dma('pool', [
        (lambda e: e.dma_start(out=Wout, in_=w_out.rearrange("(k p) c -> p k c", p=128)), [], ['Wout']),
        (lambda e: e.dma_start(out=Wr, in_=wr.rearrange("(k p) c -> p k c", p=128)), [], ['Wr']),
    ], sem='wout')
    S.dma('sp', [
        (lambda e: e.dma_start(out=brb_t, in_=brb), [], ['brb']),
        (lambda e: e.dma_start(out=gf_t, in_=rowb[4]), [], ['gf']),
        (lambda e: e.dma_start(out=ub_t, in_=ub_d), [], ['ub']),
        (lambda e: e.dma_start(out=tri32_t, in_=tri32_d), [], ['tri32']),
        (lambda e: e.dma_start(out=cst_t, in_=cst_d), [], ['cst']),
    ], sem='oconst')
    def o_T1(tl):
        b = tl % 2
        pp = (tl % 2) * 2
        for kc in range(8):
            S.op('pe', lambda e, kc=kc, tl=tl, b=b: e.transpose(out=pt[b][:, kc * 128:(kc + 1) * 128], in_=M[:, tl, kc * 128:(kc + 1) * 128], identity=ident),
                 reads=[('M', tl), 'ident'], writes=[PT[b]], signal=(kc == 7))
        S.op('act', lambda e, b=b: e.activation(out=MT[b], in_=pt[b].rearrange("p (k t) -> p k t", k=8), func=AF.Copy),
             reads=[PT[b]], writes=[('MT', b)])
        S.dma('sp', [(lambda e, tl=tl, b=b: e.dma_start(out=xin[b], in_=xa[tl * 128:(tl + 1) * 128, :]), [], [('xin', b)])], sem='xin%d' % b)
        for half in range(2):
            pb = pp + half
            for kc in range(8):
                S.op('pe', lambda e, kc=kc, b=b, pb=pb, half=half: e.matmul(ps[pb], lhsT=MT[b][:, kc, :], rhs=Wout[:, kc, half * 512:(half + 1) * 512],
                                                                            start=(kc == 0), stop=(kc == 7)),
                     reads=[('MT', b), 'Wout'], writes=[PS[pb]], signal=(kc == 7))
            S.op('dve', lambda e, tl=tl, half=half, pb=pb, b=b: e.tensor_tensor(out=H[:, tl, half * 512:(half + 1) * 512], in0=ps[pb],
                                                                                 in1=xin[b][:, half * 512:(half + 1) * 512], op=ALU.add),
                 reads=[PS[pb], ('xin', b)], writes=[('H', tl)])
    def o_P(tl):
        b = tl % 2
        rms_tile(H[:, tl, :], [('H', tl)], b, 8, xn2T, ('xn2T', tl), tl * 128, xb_dst=M[:, tl, :], xb_key=('M', tl), stage='P')

    def o_Q(tl):
        b = tl % 2
        rms_tile(H[:, tl, :], [('H', tl)], b, 8, xn2T, ('xn2T', tl), tl * 128, xb_dst=M[:, tl, :], xb_key=('M', tl), stage='Q')

    def o_R(tl):
        b = tl % 2
        lg = lg2[tl % 2]
        r_ = r_2[tl % 2]
        oh = oh2[tl % 2]
        ge = ge2[tl % 2]
        elm = elm2[tl % 2]
        t8 = t82[tl % 2]
        pe_ = pe_2[tl % 2]
        pm = pm2[tl % 2]
        for kc in range(8):
            S.op('pe', lambda e, kc=kc, tl=tl: e.matmul(ps[4][:, 0:36], lhsT=xn2T[:, kc, tl * 128:(tl + 1) * 128], rhs=Wr[:, kc, :],
                                                        start=(kc == 0), stop=(kc == 7)),
                 reads=[('xn2T', tl), 'Wr'], writes=[PS[4]], signal=(kc == 7))
        S.op('dve', lambda e: e.tensor_tensor(out=lg, in0=ps[4][:, 0:36], in1=brb_t, op=ALU.add), reads=[PS[4], 'brb'], writes=[('lg', tl % 2)])
        S.op('dve', lambda e: e.reduce_max(out=r_[:, 0:1], in_=lg[:, 0:4], axis=AX.X), reads=[('lg', tl % 2)], writes=[('r_', tl % 2)])
        S.op('dve', lambda e: e.tensor_scalar(out=r_[:, 1:2], in0=r_[:, 0:1], scalar1=-1.0, scalar2=None, op0=ALU.mult), reads=[('r_', tl % 2)], writes=[('r_', tl % 2)])
        S.op('act', lambda e: e.activation(out=ge, in_=lg[:, 0:4], func=AF.Exp, bias=r_[:, 1:2], accum_out=r_[:, 2:3]),
             reads=[('lg', tl % 2), ('r_', tl % 2)], writes=[('ge', tl % 2), ('r_', tl % 2)])
        S.op('dve', lambda e: e.tensor_scalar(out=oh, in0=lg[:, 0:4], scalar1=r_[:, 0:1], scalar2=None, op0=ALU.is_ge), reads=[('lg', tl % 2), ('r_', tl % 2)], writes=[('oh', tl % 2)])
        S.op('dve', lambda e: e.tensor_scalar(out=oh, in0=oh, scalar1=BIGG, scalar2=-BIGG, op0=ALU.mult, op1=ALU.add), reads=[('oh', tl % 2)], writes=[('oh', tl % 2)])
        S.op('dve', lambda e: e.tensor_tensor(out=elm.rearrange("p (g x) -> p g x", g=4), in0=lg[:, 4:36].rearrange("p (g x) -> p g x", g=4),
                                              in1=oh.unsqueeze(2).to_broadcast([128, 4, 8]), op=ALU.add),
             reads=[('lg', tl % 2), ('oh', tl % 2)], writes=[('elm', tl % 2)])
        S.op('dve', lambda e: e.max(out=t8, in_=elm), reads=[('elm', tl % 2)], writes=[('t8', tl % 2)])
        S.op('dve', lambda e: e.tensor_scalar(out=r_[:, 6:7], in0=t8[:, 0:1], scalar1=-1.0, scalar2=None, op0=ALU.mult), reads=[('t8', tl % 2), ('r_', tl % 2)], writes=[('r_', tl % 2)])
        S.op('act', lambda e: e.activation(out=pe_, in_=elm, func=AF.Exp, bias=r_[:, 6:7]), reads=[('elm', tl % 2), ('r_', tl % 2)], writes=[('pe_', tl % 2)])
        S.op('dve', lambda e: e.scalar_tensor_tensor(out=pm, in0=elm, scalar=t8[:, 1:2], in1=pe_, op0=ALU.is_ge, op1=ALU.mult, accum_out=r_[:, 3:4]),
             reads=[('elm', tl % 2), ('t8', tl % 2), ('pe_', tl % 2), ('r_', tl % 2)], writes=[('pm', tl % 2), ('r_', tl % 2)])
        S.op('dve', lambda e: e.tensor_tensor(out=r_[:, 4:5], in0=r_[:, 3:4], in1=r_[:, 2:3], op=ALU.mult), reads=[('r_', tl % 2)], writes=[('r_', tl % 2)])
        S.op('dve', lambda e: e.reciprocal(out=r_[:, 5:6], in_=r_[:, 4:5]), reads=[('r_', tl % 2)], writes=[('r_', tl % 2)])
        S.op('dve', lambda e, tl=tl: e.tensor_scalar(out=COMB[:, tl, :], in0=pm, scalar1=r_[:, 5:6], scalar2=None, op0=ALU.mult),
             reads=[('pm', tl % 2), ('r_', tl % 2)], writes=[('COMB', tl)])
        S.op('dve', lambda e, tl=tl: e.tensor_scalar(out=MselF[:, tl, :], in0=elm, scalar1=t8[:, 1:2], scalar2=None, op0=ALU.is_ge),
             reads=[('elm', tl % 2), ('t8', tl % 2)], writes=[('MselF', tl)])
        S.op('dve', lambda e, tl=tl: e.tensor_copy(out=MselB[:, tl, :], in_=MselF[:, tl, :]), reads=[('MselF', tl)], writes=[('MselB', tl)])
    o_T1(0)
    o_P(0)
    for tl in range(NT_OWN):
        if tl + 1 < NT_OWN:
            o_T1(tl + 1)
        o_Q(tl)
        if tl + 1 < NT_OWN:
            o_P(tl + 1)
        o_R(tl)
    allMB = [('MselB', t) for t in range(NT_OWN)]
    for tl in range(NT_OWN):
        pb = 4 + tl % 2
        for j in range(tl):
            S.op('pe', lambda e, j=j, pb=pb: e.matmul(ps[pb][:, 0:NE], lhsT=ub_t[:, 128:256], rhs=MselB[:, j, :], start=(j == 0), stop=False),
                 reads=['ub', ('MselB', j)], writes=[PS[pb]], signal=False)
        S.op('pe', lambda e, tl=tl, pb=pb: e.matmul(ps[pb][:, 0:NE], lhsT=ub_t[:, 0:128], rhs=MselB[:, tl, :], start=(tl == 0), stop=True),
             reads=['ub', ('MselB', tl)], writes=[PS[pb]])
        S.op('dve', lambda e, tl=tl, pb=pb: e.tensor_copy(out=RANK[:, tl, :], in_=ps[pb][:, 0:NE]), reads=[PS[pb]], writes=[('RANK', tl)])
    for j in range(NT_OWN):
        S.op('pe', lambda e, j=j: e.matmul(ps[0][:, 0:NE], lhsT=ub_t[:, 128:256], rhs=MselB[:, j, :], start=(j == 0), stop=(j == NT_OWN - 1)),
             reads=['ub', ('MselB', j)], writes=[PS[0]], signal=(j == NT_OWN - 1))
    S.op('dve', lambda e: e.tensor_copy(out=cnt, in_=ps[0][:, 0:NE]), reads=[PS[0]], writes=['cnt'])
    S.op('dve', lambda e: e.memset(tle, 0.0), writes=['tle'])
    for m in range(16):
        S.op('dve', lambda e, m=m: e.scalar_tensor_tensor(out=tle, in0=cnt, scalar=float(128 * m), in1=tle, op0=ALU.is_gt, op1=ALU.add),
             reads=['cnt', 'tle'], writes=['tle'])
    S.op('dve', lambda e: e.tensor_copy(out=tlb, in_=tle), reads=['tle'], writes=['tlb'])
    S.op('pe', lambda e: e.transpose(out=pt[0][0:32, 0:128], in_=tlb, identity=ident), reads=['tlb', 'ident'], writes=[PT[0]])
    S.op('dve', lambda e: e.tensor_copy(out=tT, in_=pt[0][0:32, 0:128]), reads=[PT[0]], writes=['tT'])
    S.op('pe', lambda e: e.matmul(ps[1][:, 0:NE], lhsT=tT, rhs=tri32_t, start=True, stop=True), reads=['tT', 'tri32'], writes=[PS[1]])
    S.op('dve', lambda e: e.tensor_copy(out=start, in_=ps[1][:, 0:NE]), reads=[PS[1]], writes=['start'])
    S.op('dve', lambda e: e.tensor_scalar(out=start128, in0=start, scalar1=128.0, scalar2=1.0, op0=ALU.mult, op1=ALU.add),
         reads=['start'], writes=['start128'])
    S.op('dve', lambda e: e.memset(ej, -1.0), writes=['ej'])
    for ex in range(NE):
        S.op('dve', lambda e, ex=ex: e.scalar_tensor_tensor(out=ej, in0=cst_t[:, 0:NTILE], scalar=start[:, ex:ex + 1], in1=ej, op0=ALU.is_ge, op1=ALU.add),
             reads=['cst', 'start', 'ej'], writes=['ej'])
    S.op('dve', lambda e: e.tensor_scalar(out=ej, in0=ej, scalar1=128.0, scalar2=cst_t[:, 64:65], op0=ALU.mult, op1=ALU.add),
         reads=['ej', 'cst'], writes=['ej'])
    S.op('dve', lambda e: e.tensor_tensor(out=tmp32[:, 0:1], in0=start[:, NE - 1:NE], in1=tle[:, NE - 1:NE], op=ALU.add),
         reads=['start', 'tle'], writes=['tmp32'])
    S.op('dve', lambda e: e.tensor_scalar(out=ej2, in0=cst_t[:, 0:NTILE], scalar1=tmp32[:, 0:1], scalar2=float(OOB_ROW), op0=ALU.is_ge, op1=ALU.mult),
         reads=['cst', 'tmp32'], writes=['ej2'])
    S.op('dve', lambda e: e.tensor_tensor(out=IDXW, in0=ej, in1=ej2, op=ALU.add), reads=['ej', 'ej2'], writes=['IDXW'])
    for tl in range(NT_OWN):
        S.op('dve', lambda e, tl=tl: e.tensor_tensor(out=RANK[:, tl, :], in0=RANK[:, tl, :], in1=start128, op=ALU.add),
             reads=[('RANK', tl), 'start128'], writes=[('RANK', tl)])
        S.op('dve', lambda e, tl=tl: e.tensor_tensor(out=RANK[:, tl, :], in0=RANK[:, tl, :], in1=MselF[:, tl, :], op=ALU.mult),
             reads=[('RANK', tl), ('MselF', tl)], writes=[('RANK', tl)])
        S.op('dve', lambda e, tl=tl: e.max(out=p8, in_=RANK[:, tl, :]), reads=[('RANK', tl)], writes=['p8'])
        for k in range(2):
            S.op('dve', lambda e, tl=tl, k=k: e.scalar_tensor_tensor(out=tmp32, in0=RANK[:, tl, :], scalar=p8[:, k:k + 1], in1=COMB[:, tl, :],
                                                                    op0=ALU.is_equal, op1=ALU.mult, accum_out=CW2[:, tl, k:k + 1]),
                 reads=[('RANK', tl), 'p8', ('COMB', tl)], writes=['tmp32', ('CW2', tl)])
        S.op('dve', lambda e, tl=tl: e.tensor_scalar(out=POSI[:, tl, :], in0=p8[:, 0:2], scalar1=-1.0, scalar2=None, op0=ALU.add),
             reads=['p8'], writes=[('POSI', tl)])
        for k in range(2):
            S.dma('pool', [(lambda e, tl=tl, k=k: e.indirect_dma_start(out=xs_d, out_offset=bass.IndirectOffsetOnAxis(ap=POSI[:, tl, k:k + 1], axis=0),
                                                                       in_=M[:, tl, :], in_offset=None),
                            [('POSI', tl), ('M', tl)] + [('xs_zero', a) for a in range(NTILE)], [('xs_sc', tl, k)])], sem='sc')
    all_sc = [('xs_sc', tl, k) for tl in range(NT_OWN) for k in range(2)]
    S.barrier()
    if debug:
        S.dma('sp', [(lambda e: e.dma_start(out=dbg['H'], in_=H.rearrange("p t d -> p (t d)")), [('H', t) for t in range(16)], []),
                     (lambda e: e.dma_start(out=dbg['comb'], in_=COMB.rearrange("p t d -> p (t d)")), [('COMB', t) for t in range(16)], [])], sem='dbg')
        S.barrier()
    es_o.close()
    if stop == 'O':
        S.barrier(); S.build(); return nc

    es_m = ExitStack()
    W13g = [sb(es_m, "W13g%d" % i, [128, 8 * 512], BF16) for i in range(3)]
    W2g = [sb(es_m, "W2g%d" % i, [128, 2 * D], BF16) for i in range(3)]
    XS = [sb(es_m, "XS%d" % i, [128, D], BF16) for i in range(4)]
    xsT = [sb(es_m, "xsT%d" % i, [128, 8, 128], BF16) for i in range(2)]
    sa = [sb(es_m, "sa%d" % i, [128, 256], F32) for i in range(2)]
    hid = [sb(es_m, "hid%d" % i, [128, 256], BF16) for i in range(2)]
    hidT = [sb(es_m, "hidT%d" % i, [128, 2, 128], BF16) for i in range(2)]
    yt = [sb(es_m, "yt%d" % i, [128, D], F32) for i in range(2)]

    bcreg = {}
    all_wcast = [('wcast', ex_) for ex_ in range(NE)]

    def _mk_bcreg(e):
        bcreg['r'] = e.to_reg(NE * 128 - 1)
        return None
    S.ops['pool'].append(([], _mk_bcreg, None, 0))

    def moeA_pre(j):
        b = j % 2
        wb = j % 3
        S.dma('pool', [
            (lambda e, j=j, wb=wb: e.indirect_dma_start(out=W13g[wb], out_offset=None, in_=w13b,
                                                        in_offset=bass.IndirectOffsetOnAxis(ap=IDXW[:, j:j + 1], axis=0),
                                                        bounds_check=bcreg['r'], oob_is_err=False), ['IDXW'] + all_wcast, [('W13g', wb)]),
            (lambda e, j=j, wb=wb: e.indirect_dma_start(out=W2g[wb], out_offset=None, in_=w2b,
                                                        in_offset=bass.IndirectOffsetOnAxis(ap=IDXW[:, j:j + 1], axis=0),
                                                        bounds_check=bcreg['r'], oob_is_err=False), ['IDXW'], [('W2g', wb)]),
        ], sem='wg%d' % wb)
        xb4 = j % 4
        for kc in range(8):
            S.op('pe', lambda e, kc=kc, b=b, xb4=xb4: e.transpose(out=pt[b][:, kc * 128:(kc + 1) * 128], in_=XS[xb4][:, kc * 128:(kc + 1) * 128], identity=ident),
                 reads=[('XS', xb4), 'ident'], writes=[PT[b]], signal=(kc == 7))
        S.op('dve', lambda e, b=b: e.tensor_tensor(out=xsT[b], in0=pt[b].rearrange("p (k t) -> p k t", k=8),
                                                   in1=gT_t[:, 8:16].unsqueeze(2).to_broadcast([128, 8, 128]), op=ALU.mult),
             reads=[PT[b], 'gT'], writes=[('xsT', b)])

    def moeA_mm(j):
        b = j % 2
        wb = j % 3
        for kc in range(8):
            S.op('pe', lambda e, kc=kc, b=b, wb=wb: e.matmul(ps[b], lhsT=xsT[b][:, kc, :], rhs=W13g[wb][:, kc * 512:(kc + 1) * 512], start=(kc == 0), stop=(kc == 7)),
                 reads=[('xsT', b), ('W13g', wb)], writes=[PS[b]], signal=(kc == 7))
        S.op('act', lambda e, b=b: e.activation(out=sa[b], in_=ps[b][:, 0:256], func=AF.Silu), reads=[PS[b]], writes=[('sa', b)])
        S.op('dve', lambda e, b=b: e.tensor_tensor(out=hid[b], in0=sa[b], in1=ps[b][:, 256:512], op=ALU.mult),
             reads=[('sa', b), PS[b]], writes=[('hid', b)])

    def moeB(j):
        b = j % 2
        wb = j % 3
        for ft in range(2):
            S.op('pe', lambda e, ft=ft, b=b: e.transpose(out=pt[b][:, ft * 128:(ft + 1) * 128], in_=hid[b][:, ft * 128:(ft + 1) * 128], identity=ident),
                 reads=[('hid', b), 'ident'], writes=[PT[b]], signal=(ft == 1))
        S.op('act', lambda e, b=b: e.activation(out=hidT[b], in_=pt[b][:, 0:256].rearrange("p (f t) -> p f t", f=2), func=AF.Copy),
             reads=[PT[b]], writes=[('hidT', b)])
        for half in range(2):
            pb = 2 + 2 * b + half
            for ft in range(2):
                S.op('pe', lambda e, ft=ft, half=half, b=b, pb=pb, wb=wb: e.matmul(
                    ps[pb], lhsT=hidT[b][:, ft, :], rhs=W2g[wb][:, ft * D + half * 512:ft * D + (half + 1) * 512], start=(ft == 0), stop=(ft == 1)),
                    reads=[('hidT', b), ('W2g', wb)], writes=[PS[pb]], signal=(ft == 1))
            if half == 0:
                S.op('act', lambda e, b=b, pb=pb: e.activation(out=yt[b][:, 0:512], in_=ps[pb], func=AF.Copy), reads=[PS[pb]], writes=[('yt', b)])
            else:
                S.op('dve', lambda e, b=b, pb=pb: e.tensor_copy(out=yt[b][:, 512:1024], in_=ps[pb]), reads=[PS[pb]], writes=[('yt', b)])
        S.dma('sp', [(lambda e, j=j, b=b: e.dma_start(out=outs_d[j * 128:(j + 1) * 128, :], in_=yt[b]), [('yt', b)], [('outs', j)])], sem='ost')

    def xsload(j):
        xb4 = j % 4
        S.dma('act', [(lambda e, j=j, xb4=xb4: e.dma_start(out=XS[xb4], in_=xs_d[j * 128:(j + 1) * 128, :]), all_sc, [('XS', xb4)])], sem='xsl%d' % xb4)

    for j in range(3):
        xsload(j)
    moeA_pre(0)
    moeA_mm(0)
    for j in range(NTILE):
        if j + 3 < NTILE:
            xsload(j + 3)
        if j + 1 < NTILE:
            moeA_pre(j + 1)
        moeB(j)
        if j + 1 < NTILE:
            moeA_mm(j + 1)
    if os.environ.get('SBUFDBG'):
        print("sbuf remaining after MoE alloc", nc.sbuf_bytes_remaining)
    all_outs = [('outs', j) for j in range(NTILE)]

    O12 = [sb(es_m, "O12_%d" % i, [128, 2, D], F32) for i in range(2)]
    for tl in range(NT_OWN):
        b = tl % 2
        S.dma('pool', [
            (lambda e, tl=tl, b=b, k=k: e.indirect_dma_start(out=O12[b][:, k, :], out_offset=None, in_=outs_d,
                                                             in_offset=bass.IndirectOffsetOnAxis(ap=POSI[:, tl, k:k + 1], axis=0)),
             all_outs + [('POSI', tl)], [('O12', b)]) for k in range(2)], sem='og%d' % b)
        for k in range(2):
            S.op('dve', lambda e, tl=tl, b=b, k=k: e.scalar_tensor_tensor(out=H[:, tl, :], in0=O12[b][:, k, :], scalar=CW2[:, tl, k:k + 1], in1=H[:, tl, :],
                                                                         op0=ALU.mult, op1=ALU.add),
                 reads=[('O12', b), ('CW2', tl), ('H', tl)], writes=[('H', tl)])
        s_ = st[b]
        sk = ('st', b)
        S.op('dve', lambda e, tl=tl, s_=s_: e.scalar_tensor_tensor(out=junk, in0=H[:, tl, :], scalar=1.0, in1=H[:, tl, :], op0=ALU.mult, op1=ALU.mult,
                                                                  accum_out=s_[:, 0:1]), reads=[('H', tl)], writes=['junk', sk])
        S.op('dve', lambda e, s_=s_: e.tensor_scalar(out=s_[:, 1:2], in0=s_[:, 0:1], scalar1=1.0 / D, scalar2=EPS, op0=ALU.mult, op1=ALU.add),
             reads=[sk], writes=[sk])
        S.op('act', lambda e, s_=s_: e.activation(out=s_[:, 3:4], in_=s_[:, 1:2], func=AF.Sqrt), reads=[sk], writes=[sk])
        S.op('dve', lambda e, s_=s_: e.reciprocal(out=s_[:, 2:3], in_=s_[:, 3:4]), reads=[sk], writes=[sk])
        S.op('dve', lambda e, tl=tl, s_=s_, b=b: e.scalar_tensor_tensor(out=xin[b], in0=H[:, tl, :], scalar=s_[:, 2:3], in1=gf_t, op0=ALU.mult, op1=ALU.mult),
             reads=[('H', tl), sk, 'gf'], writes=[('xin', b)])
        S.dma('sp', [(lambda e, tl=tl, b=b: e.dma_start(out=out[tl * 128:(tl + 1) * 128, :], in_=xin[b]), [('xin', b)], [])], sem='out')
    S.barrier()
    S.build()
    return nc


def _t5_bucket_np(n):
    n = np.maximum(n, 0)
    nf = np.maximum(n, 16).astype(np.float32)
    large = 16 + (np.log(nf / np.float32(16)) / np.float32(math.log(128 / 16)) * np.float32(16)).astype(np.int32)
    large = np.minimum(large, 31)
    return np.where(n < 16, n, large)


_PROG = {}


def _get_prog(debug=False):
    if debug not in _PROG:
        _PROG[debug] = build_program(debug)
    return _PROG[debug]


def make_in_maps(x, norm_mix_g, w_in, b_gates, gmlp_ln_g, gmlp_ln_b, w_spatial, b_spatial, rel_bias,
                 w_out, norm_ffn_g, w_group_router, b_group_router, w_expert_router, b_expert_router,
                 w1, w3, w2, norm_final_g):
    f = lambda a: np.ascontiguousarray(np.asarray(a), dtype=np.float32)
    x = f(x)
    w_in0 = f(w_in[0]); w_out0 = f(w_out[0]); w1_0 = f(w1[0]); w3_0 = f(w3[0]); w2_0 = f(w2[0])
    w13 = np.concatenate([w1_0, w3_0], axis=2).reshape(NE, 8, 128, 512)
    w13r = np.ascontiguousarray(np.transpose(w13, (0, 2, 1, 3))).reshape(NE * 128, 8 * 512)
    w2r = np.ascontiguousarray(np.transpose(w2_0.reshape(NE, 2, 128, D), (0, 2, 1, 3))).reshape(NE * 128, 2 * D)
    ub = np.concatenate([np.triu(np.ones((128, 128), np.float32), 1), np.ones((128, 128), np.float32)], axis=1).astype(ml_dtypes.bfloat16)
    tri32 = np.triu(np.ones((32, 32), np.float32), 1).astype(ml_dtypes.bfloat16)
    cst = np.zeros((128, 128), np.float32)
    cst[:, 0:64] = np.arange(64, dtype=np.float32)[None, :]
    cst[:, 64] = np.arange(128, dtype=np.float32)
    cst[:, 65:81] = (128.0 * np.arange(16, dtype=np.float32))[None, :]
    wr = np.concatenate([f(w_group_router[0]), np.transpose(f(w_expert_router[0]), (1, 0, 2)).reshape(D, 32)], axis=1)
    br = np.concatenate([f(b_group_router[0]), f(b_expert_router[0]).reshape(32)])
    brb = np.ascontiguousarray(np.broadcast_to(br[None, :], (128, 36)))
    gT = np.concatenate([f(norm_mix_g[0]).reshape(8, 128).T, f(norm_ffn_g[0]).reshape(8, 128).T], axis=1)
    bg = f(b_gates[0])
    rows = [f(gmlp_ln_g[0]), f(gmlp_ln_b[0]), bg[:D], bg[D:], f(norm_final_g), np.zeros(D, np.float32)]
    rowb = np.ascontiguousarray(np.stack([np.broadcast_to(r[None, :], (128, D)) for r in rows]))
    wsp = f(w_spatial[0])
    wspT = np.ascontiguousarray(np.transpose(wsp, (0, 2, 1)))
    bspT = np.ascontiguousarray(f(b_spatial[0]).T)
    rb = f(rel_bias)
    kk = np.arange(128)[:, None]
    qq = np.arange(128)[None, :]
    b0 = _t5_bucket_np(qq - kk)
    b1 = _t5_bucket_np(qq - kk + 128)
    rb0 = np.ascontiguousarray(np.stack([rb[b0, h] for h in range(8)]))
    rb1 = np.ascontiguousarray(np.stack([rb[b1, h] for h in range(8)]))
    chb = np.ascontiguousarray(np.broadcast_to(rb[31][None, :], (128, 8)))
    causal_add = np.where(qq >= kk, 0.0, -BIGM).astype(np.float32)
    tril = (np.arange(128)[None, :] <= np.arange(128)[:, None]).astype(np.float32)
    triu = np.ascontiguousarray(tril.T)
    cmk = np.ascontiguousarray(np.stack([causal_add, tril, triu]))
    ident = np.eye(128, dtype=np.float32).astype(ml_dtypes.bfloat16)
    e16 = np.zeros((16, 16, 128), np.float32)
    for s in range(16):
        e16[s, s, :] = 1.0
    e16 = e16.reshape(16, 16 * 128).astype(ml_dtypes.bfloat16)
    in_maps = []
    for c in range(8):
        b, p = c // 2, c % 2
        own = x[b, p * TOWN:(p + 1) * TOWN]
        oth = x[b, (1 - p) * TOWN:(2 - p) * TOWN]
        xa = np.ascontiguousarray(np.concatenate([own, oth], axis=0))
        pnm = np.full((16, 16), -BIGG, np.float32)
        for qt in range(16):
            i = qt // 2
            pnm[qt, :i] = 0.0
            if p == 1:
                pnm[qt, 8:] = 0.0
        pn = np.ascontiguousarray(np.broadcast_to(pnm.reshape(1, 256), (128, 256)))
        in_maps.append({
            "xa": xa, "w_in": w_in0, "w_out": w_out0, "w13r": w13r, "w2r": w2r, "ub": ub, "tri32": tri32, "cst": cst, "wr": np.ascontiguousarray(wr),
            "pn": pn, "gT": np.ascontiguousarray(gT), "rowb": rowb, "brb": brb, "wsp": wsp, "wspT": wspT, "bspT": bspT,
            "rb0": rb0, "rb1": rb1, "chb": chb, "cmk": cmk, "ident": ident, "e16": e16,
        })
    return in_maps


def kernel(**inputs):
    nc = _get_prog(False)
    in_maps = make_in_maps(**inputs)
    res = run_bass_kernel_spmd(nc, in_maps, core_ids=list(range(8)))
    outp = np.empty((4, SEQ, D), np.float32)
    for c in range(8):
        b, p = c // 2, c % 2
        outp[b, p * TOWN:(p + 1) * TOWN] = np.asarray(res.results[c]["out"], dtype=np.float32)
    return outp
```

```python
import math
from contextlib import ExitStack

import numpy as np
import ml_dtypes
import concourse.bass as bass
import concourse.mybir as mybir
from concourse.bass_utils import run_bass_kernel_spmd

F32 = mybir.dt.float32
BF16 = mybir.dt.bfloat16
ALU = mybir.AluOpType
AF = mybir.ActivationFunctionType
AX = mybir.AxisListType

D = 1024
SEQ = 4096
NB = 16
TOWN = 2048
NT_OWN = 16
NT_ALL = 32
EPS = 1e-6
SCALE = 128 ** -0.5
BIGM = 30000.0
BIGG = 1.0e9
NE = 32
NTILE = 64
OOB_ROW = 8192
NSLOT = NTILE * 128
I32 = mybir.dt.int32
DEBUG = False
import os
SKIP = set(os.environ.get('SKIP', '').split(','))


class Sched:
    def __init__(self, nc):
        self.nc = nc
        self.names = ['pe', 'act', 'dve', 'pool', 'sp']
        self.ops = {k: [] for k in self.names}
        self.sem = {k: nc.alloc_semaphore("s_" + k) for k in self.names}
        self.cnt = {k: 0 for k in self.names}
        self.last_w = {}
        self.readers = {}
        self.dsem = {}
        self.dcnt = {}
        self.waited = {k: {} for k in self.names}

    def _semh(self, sk):
        return self.sem[sk] if sk in self.sem else self.dsem[sk]

    def _deps(self, eng, reads, writes):
        need = {}

        def add(sk, v):
            if need.get(sk, 0) < v:
                need[sk] = v
        for k in reads:
            if k in self.last_w:
                add(*self.last_w[k])
        for k in writes:
            if k in self.last_w:
                add(*self.last_w[k])
            for sk, v in self.readers.get(k, {}).items():
                add(sk, v)
        waits = []
        for sk, v in need.items():
            if sk == eng and v > self.cnt[eng]:
                continue
            if self.waited[eng].get(sk, 0) >= v:
                continue
            self.waited[eng][sk] = v
            waits.append((sk, v))
        return waits

    def _record(self, tag, reads, writes):
        for k in reads:
            r = self.readers.setdefault(k, {})
            if r.get(tag[0], 0) < tag[1]:
                r[tag[0]] = tag[1]
        for k in writes:
            self.last_w[k] = tag
            self.readers[k] = {}

    def op(self, eng, fn, reads=(), writes=(), signal=True):
        waits = self._deps(eng, reads, writes)
        if signal:
            self.cnt[eng] += 1
            seq = self.cnt[eng]
        else:
            seq = self.cnt[eng] + 1
        self._record((eng, seq), reads, writes)
        self.ops[eng].append((waits, fn, self.sem[eng] if signal else None, 1))

    def dma(self, q, items, sem):
        if sem not in self.dsem:
            self.dsem[sem] = self.nc.alloc_semaphore("d_" + sem)
            self.dcnt[sem] = 0
        allr, allw = [], []
        for fn, reads, writes in items:
            allr += list(reads)
            allw += list(writes)
        waits = self._deps(q, allr, allw)
        first = True
        for fn, reads, writes in items:
            self.dcnt[sem] += 16
            self.ops[q].append((waits if first else [], fn, self.dsem[sem], 16))
            first = False
        self._record((sem, self.dcnt[sem]), allr, allw)

    def barrier(self):
        for e in self.names:
            waits = []
            for o in self.names:
                if self.cnt[o] > self.waited[e].get(o, 0):
                    self.waited[e][o] = self.cnt[o]
                    waits.append((o, self.cnt[o]))
            for s, c in self.dcnt.items():
                if c > self.waited[e].get(s, 0):
                    self.waited[e][s] = c
                    waits.append((s, c))
            self.ops[e].append((waits, None, None, 0))

    def build(self):
        nc = self.nc
        with nc.Block() as block:
            def mk(name):
                def body(e):
                    for waits, fn, sem, inc in self.ops[name]:
                        for sk, v in waits:
                            e.wait_ge(self._semh(sk), v)
                        if fn is not None:
                            ins = fn(e)
                            if sem is not None:
                                ins.then_inc(sem, inc)
                return body
            block.tensor(mk('pe'))
            block.scalar(mk('act'))
            block.vector(mk('dve'))
            block.gpsimd(mk('pool'))
            block.sync(mk('sp'))


def build_program(debug=False, stop=None):
    nc = bass.Bass("TRN2", target_bir_lowering=False)

    def din(name, shape, dt=F32):
        return nc.dram_tensor(name, list(shape), dt, kind="ExternalInput").ap()

    xa = din("xa", [SEQ, D])
    w_in = din("w_in", [D, 7 * D])
    w_out = din("w_out", [D, D])
    w13r = din("w13r", [NE * 128, 8 * 512])
    w2r = din("w2r", [NE * 128, 2 * D])
    ub_d = din("ub", [128, 256], BF16)
    tri32_d = din("tri32", [32, 32], BF16)
    cst_d = din("cst", [128, 128])
    xs_d = nc.dram_tensor("xs_scr", [NSLOT, D], BF16).ap()
    w13b = nc.dram_tensor("w13b_scr", [NE * 128, 8 * 512], BF16).ap()
    w2b = nc.dram_tensor("w2b_scr", [NE * 128, 2 * D], BF16).ap()
    outs_d = nc.dram_tensor("outs_scr", [NSLOT, D], F32).ap()
    wr = din("wr", [D, 36])
    pn = din("pn", [128, 16 * 16])
    gT = din("gT", [128, 16])
    rowb = din("rowb", [6, 128, D])
    brb = din("brb", [128, 36])
    wsp = din("wsp", [8, 128, 128])
    wspT = din("wspT", [8, 128, 128])
    bspT = din("bspT", [128, 8])
    rb0 = din("rb0", [8, 128, 128])
    rb1 = din("rb1", [8, 128, 128])
    chb = din("chb", [128, 8])
    cmk = din("cmk", [3, 128, 128])
    ident_d = din("ident", [128, 128], BF16)
    e16_d = din("e16", [16, 16 * 128], BF16)
    out = nc.dram_tensor("out", [TOWN, D], F32, kind="ExternalOutput").ap()
    dbg = {}
    if debug:
        dbg['yb'] = nc.dram_tensor("dbg_yb", [128, NT_OWN * D], BF16, kind="ExternalOutput").ap()
        dbg['M'] = nc.dram_tensor("dbg_M", [128, NT_OWN * D], BF16, kind="ExternalOutput").ap()
        dbg['H'] = nc.dram_tensor("dbg_H", [128, NT_OWN * D], F32, kind="ExternalOutput").ap()
        dbg['comb'] = nc.dram_tensor("dbg_comb", [128, NT_OWN * NE], F32, kind="ExternalOutput").ap()

    S = Sched(nc)
    es_all = ExitStack()

    def sb(es, name, shape, dt):
        return es.enter_context(nc.sbuf_tensor("sb_" + name, list(shape), dt)).ap()

    pt = [nc.alloc_psum_tensor("pt%d" % i, [128, 1024], BF16).ap() for i in range(2)]
    ps = [nc.alloc_psum_tensor("ps%d" % i, [128, 512], F32).ap() for i in range(6)]
    PT = [('pt', i) for i in range(2)]
    PS = [('ps', i) for i in range(6)]

    ident = sb(es_all, "ident", [128, 128], BF16)
    e16 = sb(es_all, "e16", [16, 16 * 128], BF16)
    cmk_t = sb(es_all, "cmk", [128, 3, 128], F32)
    gT_t = sb(es_all, "gT", [128, 16], F32)
    chb_t = sb(es_all, "chb", [128, 8], F32)
    pn_t = sb(es_all, "pn", [128, 256], F32)
    st = [sb(es_all, "st%d" % i, [128, 8], F32) for i in range(2)]
    M = sb(es_all, "M", [128, NT_OWN, D], BF16)
    xin = [sb(es_all, "xin%d" % i, [128, D], F32) for i in range(2)]
    junk = sb(es_all, "junk", [128, D], F32)
    zt = junk.bitcast(BF16)[:, 0:D]
    xb = [sb(es_all, "xb%d" % i, [128, D], BF16) for i in range(2)]

    S.dma('sp', [
        (lambda e: e.dma_start(out=ident, in_=ident_d), [], ['ident']),
        (lambda e: e.dma_start(out=e16, in_=e16_d), [], ['e16']),
        (lambda e: e.dma_start(out=cmk_t, in_=cmk.rearrange("a p q -> p a q")), [], ['cmk']),
        (lambda e: e.dma_start(out=gT_t, in_=gT), [], ['gT']),
        (lambda e: e.dma_start(out=chb_t, in_=chb), [], ['chb']),
        (lambda e: e.dma_start(out=pn_t, in_=pn), [], ['pn']),
    ], sem='const')

    def rms_tile(src_fn, src_keys, b, gcol, dstT, dst_key, tok0, q='sp', xb_dst=None, xb_key=None, stage='PQ'):
        xt = src_fn
        xbd = xb[b] if xb_dst is None else xb_dst
        xbk = ('xb', b) if xb_key is None else xb_key
        s_ = st[b]
        sk = ('st', b)
        if 'P' in stage:
            S.op('dve', lambda e: e.scalar_tensor_tensor(out=junk, in0=xt, scalar=1.0, in1=xt, op0=ALU.mult, op1=ALU.mult,
                                                         accum_out=s_[:, 0:1]), reads=src_keys, writes=['junk', sk])
            S.op('dve', lambda e: e.tensor_scalar(out=s_[:, 1:2], in0=s_[:, 0:1], scalar1=1.0 / D, scalar2=EPS,
                                                  op0=ALU.mult, op1=ALU.add), reads=[sk], writes=[sk])
            S.op('act', lambda e: e.activation(out=s_[:, 3:4], in_=s_[:, 1:2], func=AF.Sqrt), reads=[sk], writes=[sk])
            S.op('dve', lambda e: e.reciprocal(out=s_[:, 2:3], in_=s_[:, 3:4]), reads=[sk], writes=[sk])
            S.op('act', lambda e: e.activation(out=xbd, in_=xt, func=AF.Copy, scale=s_[:, 2:3]),
                 reads=src_keys + [sk], writes=[xbk])
        if 'Q' not in stage:
            return
        for kc in range(8):
            S.op('pe', lambda e, kc=kc: e.transpose(out=pt[b][:, kc * 128:(kc + 1) * 128], in_=xbd[:, kc * 128:(kc + 1) * 128],
                                                    identity=ident),
                 reads=[xbk, 'ident'], writes=[PT[b]], signal=(kc == 7))
        S.op('dve', lambda e: e.tensor_tensor(out=dstT[:, :, tok0:tok0 + 128],
                                              in0=pt[b].rearrange("p (k t) -> p k t", k=8),
                                              in1=gT_t[:, gcol:gcol + 8].unsqueeze(2).to_broadcast([128, 8, 128]), op=ALU.mult),
             reads=[PT[b], 'gT'], writes=[dst_key])

    es_x = ExitStack()
    xnT_own = sb(es_x, "xnT_own", [128, 8, TOWN], BF16)
    es_x2 = ExitStack()
    xnT_oth = sb(es_x2, "xnT_oth", [128, 8, TOWN], BF16)

    def xs(kc, tok0, n):
        if tok0 < TOWN:
            return xnT_own[:, kc, tok0:tok0 + n]
        return xnT_oth[:, kc, tok0 - TOWN:tok0 - TOWN + n]
    es_a = ExitStack()
    V4 = sb(es_a, "V4", [128, NT_ALL, 4, 130], BF16)
    Wv4 = sb(es_a, "Wv4", [128, 8, 512], BF16)
    KT = sb(es_a, "KT", [128, SEQ], BF16)
    QT = sb(es_a, "QT", [128, TOWN], BF16)
    Wqk = [sb(es_a, "Wqk%d" % i, [128, 2, 8, 128], BF16) for i in range(2)]
    Tt = [sb(es_a, "Tt%d" % i, [128, 2, 256], F32) for i in range(2)]
    kmsf = sb(es_a, "kmsf", [128, 16], F32)
    kms = sb(es_a, "kms", [128, 16], BF16)
    gm = sb(es_a, "gm", [128, 16, 16], F32)
    ga = sb(es_a, "ga", [128, 16, 16], F32)
    gb_ = sb(es_a, "gb_", [128, 16, 16], F32)
    top8 = sb(es_a, "top8", [128, 16, 8], F32)
    PTs = [sb(es_a, "PTs%d" % i, [128, 512], BF16) for i in range(4)]
    stmp = [sb(es_a, "stmp%d" % i, [128, 256], F32) for i in range(2)]
    rden = sb(es_a, "rden", [128, 4], F32)
    acc_sb = sb(es_a, "acc_sb", [128, 4, 130], F32)

    S.op('pool', lambda e: e.memset(V4[:, :, :, 128:130], 1.0), writes=['V4ones'])
    S.op('pool', lambda e: e.memset(junk, 0.0), writes=['junk'])
    S.op('pool', lambda e: e.memset(junk.bitcast(BF16), 0.0), writes=['junk'])

    def v4_tile(tt):
        pb = tt % 2
        for kc in range(8):
            S.op('pe', lambda e, kc=kc, tt=tt, pb=pb: e.matmul(ps[pb], lhsT=xs(kc, tt * 128, 128), rhs=Wv4[:, kc, :],
                                                               start=(kc == 0), stop=(kc == 7)),
                 reads=[('xnT', tt), 'Wv4'], writes=[PS[pb]], signal=(kc == 7))
        S.op('dve', lambda e, tt=tt, pb=pb: e.tensor_copy(out=V4[:, tt, :, 0:128], in_=ps[pb].rearrange("p (h c) -> p h c", h=4)),
             reads=[PS[pb]], writes=[('V4', tt)])

    S.dma('pool', [(lambda e: e.dma_start(out=Wv4, in_=w_in[:, 4096:4096 + 512].rearrange("(k p) c -> p k c", p=128)), [], ['Wv4'])], sem='wv')
    def a1(tt, stage):
        b = tt % 2
        if 'P' in stage:
            S.dma('sp', [(lambda e, tt=tt, b=b: e.dma_start(out=xin[b], in_=xa[tt * 128:(tt + 1) * 128, :]), [], [('xin', b)])],
                  sem='xin%d' % b)
        rms_tile(xin[b], [('xin', b)], b, 0, xnT_own if tt < 16 else xnT_oth, ('xnT', tt), (tt % 16) * 128, stage=stage)

    a1(0, 'P')
    for tt in range(NT_ALL):
        if tt + 1 < NT_ALL:
            a1(tt + 1, 'P')
        a1(tt, 'Q')
        if tt >= 1:
            v4_tile(tt - 1)
    v4_tile(NT_ALL - 1)
    all_xnT = [('xnT', tt) for tt in range(NT_ALL)]
    stb = [ps[0], ps[1], pt[0].bitcast(F32), pt[1].bitcast(F32)]
    STK = [PS[0], PS[1], PT[0], PT[1]]
    ptc = 0
    psc = 0
    stc = 0
    gsel = 0
    for hg in range(2):
        c0 = 4096 + hg * 512
        if hg == 1:
            S.dma('pool', [(lambda e, c0=c0: e.dma_start(out=Wv4, in_=w_in[:, c0:c0 + 512].rearrange("(k p) c -> p k c", p=128)),
                            [], ['Wv4'])], sem='wv')
            for tt in range(NT_ALL):
                v4_tile(tt)
        if stop == 'V':
            S.barrier(); S.build(); return nc
        for hh in range(4):
            h = hg * 4 + hh
            wb = h % 2
            cq = 2048 + h * 128
            ck = 3072 + h * 128
            S.dma('pool', [
                (lambda e, cq=cq, wb=wb: e.dma_start(out=Wqk[wb][:, 0], in_=w_in[:, cq:cq + 128].rearrange("(k p) c -> p k c", p=128)), [], [('Wqk', wb)]),
                (lambda e, ck=ck, wb=wb: e.dma_start(out=Wqk[wb][:, 1], in_=w_in[:, ck:ck + 128].rearrange("(k p) c -> p k c", p=128)), [], []),
            ], sem='wqk%d' % wb)
            for ex_ in range(4 * h, 4 * h + 4):
                S.dma('pool', [
                    (lambda e, ex_=ex_: e.dma_start(out=w13b[ex_ * 128:(ex_ + 1) * 128, :], in_=w13r[ex_ * 128:(ex_ + 1) * 128, :]), [], [('wcast', ex_)]),
                    (lambda e, ex_=ex_: e.dma_start(out=w2b[ex_ * 128:(ex_ + 1) * 128, :], in_=w2r[ex_ * 128:(ex_ + 1) * 128, :]), [], []),
                ], sem='wcast')
            S.dma('sp', [(lambda e, a=a: e.dma_start(out=xs_d[a * 128:(a + 1) * 128, :], in_=zt), ['junk'], [('xs_zero', a)])
                         for a in range(8 * h, 8 * h + 8)], sem='xz')
            S.dma('sp', [
                (lambda e, h=h, wb=wb: e.dma_start(out=Tt[wb][:, 0, 0:128], in_=rb0[h]), [], [('Tt', wb)]),
                (lambda e, h=h, wb=wb: e.dma_start(out=Tt[wb][:, 0, 128:256], in_=rb1[h]), [], []),
                (lambda e, h=h, wb=wb: e.dma_start(out=Tt[wb][:, 1, 0:128], in_=rb1[h]), [], []),
            ], sem='tt%d' % wb)
            if 'tt1' not in SKIP:
              S.op('dve', lambda e, wb=wb: e.tensor_tensor(out=Tt[wb][:, 0, 0:128], in0=Tt[wb][:, 0, 0:128], in1=cmk_t[:, 0, :], op=ALU.add),
                 reads=[('Tt', wb), 'cmk'], writes=[('Tt', wb)])
            if 'tt2' not in SKIP:
              S.op('dve', lambda e, wb=wb, h=h: e.tensor_scalar(out=Tt[wb][:, 1, 128:256], in0=cmk_t[:, 1, :], scalar1=0.0, scalar2=chb_t[:, h:h + 1],
                                                            op0=ALU.mult, op1=ALU.add),
                 reads=[('Tt', wb), 'cmk', 'chb'], writes=[('Tt', wb)])
            for tg in range(8):
                pb = tg % 2
                for kc in range(8):
                    S.op('pe', lambda e, kc=kc, tg=tg, pb=pb, wb=wb: e.matmul(ps[pb], lhsT=Wqk[wb][:, 1, kc, :], rhs=xs(kc, tg * 512, 512),
                                                                              start=(kc == 0), stop=(kc == 7)),
                         reads=all_xnT[tg * 4:(tg + 1) * 4] + [('Wqk', wb)], writes=[PS[pb]], signal=(kc == 7))
                for bk in range(2):
                    S.op('act', lambda e, tg=tg, pb=pb, bk=bk: e.activation(out=KT[:, tg * 512 + bk * 256:tg * 512 + (bk + 1) * 256],
                                                                         in_=ps[pb][:, bk * 256:(bk + 1) * 256], func=AF.Copy,
                                                                         accum_out=kmsf[:, 2 * tg + bk:2 * tg + bk + 1]),
                         reads=[PS[pb]], writes=[('KT', tg), 'kmsf'])
            S.op('dve', lambda e: e.tensor_copy(out=kms, in_=kmsf), reads=['kmsf'], writes=['kms'])
            for tg in range(4):
                pb = tg % 2
                for kc in range(8):
                    S.op('pe', lambda e, kc=kc, tg=tg, pb=pb, wb=wb: e.matmul(ps[pb], lhsT=Wqk[wb][:, 0, kc, :], rhs=xs(kc, tg * 512, 512),
                                                                              start=(kc == 0), stop=(kc == 7)),
                         reads=all_xnT[tg * 4:(tg + 1) * 4] + [('Wqk', wb)], writes=[PS[pb]], signal=(kc == 7))
                S.op('dve', lambda e, tg=tg, pb=pb: e.tensor_copy(out=QT[:, tg * 512:(tg + 1) * 512], in_=ps[pb]),
                     reads=[PS[pb]], writes=[('QT', tg)])
            all_QT = [('QT', tg) for tg in range(4)]
            if stop == 'KQ':
                S.barrier(); S.build(); return nc
            for qt in range(16):
                S.op('pe', lambda e, qt=qt: e.matmul(ps[5][:, qt * 16:(qt + 1) * 16], lhsT=QT[:, qt * 128:(qt + 1) * 128], rhs=kms, start=True, stop=True),
                     reads=[('QT', qt // 4), 'kms'], writes=[PS[5]], signal=(qt == 15))
            S.op('dve', lambda e: e.tensor_tensor(out=gm, in0=ps[5][:, 0:256].rearrange("p (a b) -> p a b", a=16),
                                                  in1=pn_t.rearrange("p (a b) -> p a b", a=16), op=ALU.add),
                 reads=[PS[5], 'pn'], writes=['gm'])
            for qt in range(16):
                S.op('dve', lambda e, qt=qt: e.max(out=top8[:, qt, :], in_=gm[:, qt, :]), reads=['gm'], writes=['top8'])
            S.op('dve', lambda e: e.tensor_tensor(out=ga, in0=gm, in1=top8[:, :, 2:3].to_broadcast([128, 16, 16]), op=ALU.is_ge),
                 reads=['gm', 'top8'], writes=['ga'])
            S.op('dve', lambda e: e.tensor_scalar(out=gb_, in0=gm, scalar1=-0.5 * BIGG, scalar2=None, op0=ALU.is_gt),
                 reads=['gm'], writes=['gb_'])
            S.op('dve', lambda e: e.tensor_tensor(out=ga, in0=ga, in1=gb_, op=ALU.mult), reads=['ga', 'gb_'], writes=['ga'])
            if stop == 'SEL':
                S.barrier(); S.build(); return nc
            for pr in range(4):
                i0, i1 = 2 * pr, 2 * pr + 1
                adj0 = i0 - 1 if i0 >= 1 else 15
                groups = []
                for s_ in [x for x in range(i0)] + [x for x in range(8, 16)]:
                    us = []
                    for kt in range(2):
                        sp = [('T1', 0, 256)] if (s_ == adj0 and kt == 1) else []
                        us.append(dict(q0=i0 * 256, n=512, ktile=s_ * 2 + kt, special=sp, accs=[(0, 0), (1, 128), (2, 256), (3, 384)]))
                    groups.append(dict(units=us, sel=s_, accs=[0, 1, 2, 3]))
                groups.append(dict(units=[
                    dict(q0=i1 * 256, n=256, ktile=i0 * 2, special=[], accs=[(2, 0), (3, 128)]),
                    dict(q0=i1 * 256, n=256, ktile=i0 * 2 + 1, special=[('T1', 0, 256)], accs=[(2, 0), (3, 128)])], sel=i0, accs=[2, 3]))
                for ii, ab in ((i0, 0), (i1, 2)):
                    groups.append(dict(units=[
                        dict(q0=ii * 256, n=256, ktile=ii * 2, special=[('T0', 0, 256)], accs=[(ab, 0), (ab + 1, 128)]),
                        dict(q0=ii * 256 + 128, n=128, ktile=ii * 2 + 1, special=[('T0', 0, 128)], accs=[(ab + 1, 0)])], sel=None, accs=[ab, ab + 1]))
                units = []
                for gi, g_ in enumerate(groups):
                    g_['bs'] = gsel % 2
                    gsel += 1
                    for u in g_['units']:
                        u['g'] = g_
                        units.append(u)
                    g_['last'] = units[-1]
                    g_['banks_started'] = set()
                    g_['lastmm'] = {}
                    for u in g_['units']:
                        for a_, _ in u['accs']:
                            g_['lastmm'][a_] = id(u)
                inited = set()

                def pbank(g_, a_):
                    return 2 + 2 * g_['bs'] + a_ // 2

                def pacc(g_, a_):
                    return ps[pbank(g_, a_)][:, (a_ % 2) * 256:(a_ % 2) * 256 + 129]

                def stage1(u):
                    pb = u['pb']
                    n, q0, kt_ = u['n'], u['q0'], u['ktile']
                    qkeys = [('QT', (q0 + c) // 512) for c in range(0, n, 256)] if n >= 256 else [('QT', q0 // 512)]
                    S.op('pe', lambda e, pb=pb, n=n, q0=q0, kt_=kt_: e.matmul(
                        stb[pb][:, 0:n], lhsT=KT[:, kt_ * 128:(kt_ + 1) * 128], rhs=QT[:, q0:q0 + n], start=True, stop=True),
                        reads=[('KT', kt_ // 4)] + qkeys, writes=[STK[pb]], signal=True)

                def stage2(u):
                    pb, pj, n = u['pb'], u['pj'], u['n']
                    c_done = 0
                    for (tk, c0_, c1_) in u['special']:
                        sj = u['sj']
                        tsel = 0 if tk == 'T0' else 1
                        S.op('dve', lambda e, pb=pb, sj=sj, c0_=c0_, c1_=c1_, tsel=tsel, wb=wb: e.scalar_tensor_tensor(
                            out=stmp[sj][:, c0_:c1_], in0=stb[pb][:, c0_:c1_], scalar=SCALE, in1=Tt[wb][:, tsel, 0:c1_ - c0_], op0=ALU.mult, op1=ALU.add),
                            reads=[STK[pb], ('Tt', wb)], writes=[('stmp', sj)])
                        S.op('act', lambda e, sj=sj, pj=pj, c0_=c0_, c1_=c1_: e.activation(out=PTs[pj][:, c0_:c1_], in_=stmp[sj][:, c0_:c1_], func=AF.Exp),
                             reads=[('stmp', sj)], writes=[('PTs', pj)])
                        c_done = c1_
                    if c_done < n:
                        S.op('act', lambda e, pb=pb, pj=pj, c_done=c_done, n=n, h=h: e.activation(
                            out=PTs[pj][:, c_done:n], in_=stb[pb][:, c_done:n], func=AF.Exp, bias=chb_t[:, h:h + 1], scale=SCALE),
                            reads=[STK[pb], 'chb'], writes=[('PTs', pj)])

                def stage3(g_):
                    for a_ in g_['accs']:
                        bk = pbank(g_, a_)
                        ua = [(u, off) for u in g_['units'] for (aa, off) in u['accs'] if aa == a_]
                        for k_, (u, off) in enumerate(ua):
                            pj, kt_ = u['pj'], u['ktile']
                            S.op('pe', lambda e, g_=g_, a_=a_, off=off, pj=pj, kt_=kt_, st_=(k_ == 0), sp_=(k_ == len(ua) - 1), hh=hh: e.matmul(
                                pacc(g_, a_), lhsT=PTs[pj][:, off:off + 128], rhs=V4[:, kt_, hh, 0:129], start=st_, stop=sp_),
                                reads=[('PTs', pj), ('V4', kt_), 'V4ones'], writes=[PS[bk]], signal=(k_ == len(ua) - 1))
                    for a_ in g_['accs']:
                        qt = i0 * 2 + a_
                        bk = pbank(g_, a_)
                        dst = acc_sb[:, a_, 0:129]
                        if g_['sel'] is None:
                            S.op('dve', lambda e, g_=g_, a_=a_, dst=dst: e.tensor_tensor(out=dst, in0=pacc(g_, a_), in1=dst, op=ALU.add),
                                 reads=[PS[bk], ('acc_sb', a_)], writes=[('acc_sb', a_)])
                        elif a_ not in inited:
                            S.op('dve', lambda e, g_=g_, a_=a_, dst=dst, qt=qt: e.tensor_scalar(
                                out=dst, in0=pacc(g_, a_), scalar1=ga[:, qt, g_['sel']:g_['sel'] + 1], scalar2=None, op0=ALU.mult),
                                reads=[PS[bk], 'ga'], writes=[('acc_sb', a_)])
                        else:
                            S.op('dve', lambda e, g_=g_, a_=a_, dst=dst, qt=qt: e.scalar_tensor_tensor(
                                out=dst, in0=pacc(g_, a_), scalar=ga[:, qt, g_['sel']:g_['sel'] + 1], in1=dst, op0=ALU.mult, op1=ALU.add),
                                reads=[PS[bk], 'ga', ('acc_sb', a_)], writes=[('acc_sb', a_)])
                        inited.add(a_)

                def s12(g_):
                    nonlocal_counters = None
                    for u in g_['units']:
                        stage1(u)
                        stage2(u)

                for g_ in groups:
                    for u in g_['units']:
                        u['pb'] = psc % 4
                        psc += 1
                        u['pj'] = ptc % 4
                        ptc += 1
                        if u['special']:
                            u['sj'] = stc % 2
                            stc += 1
                s12(groups[0])
                for gi, g_ in enumerate(groups):
                    if gi + 1 < len(groups):
                        s12(groups[gi + 1])
                    stage3(g_)
                for a_ in range(4):
                    tl = i0 * 2 + a_
                    S.op('dve', lambda e, a_=a_: e.reciprocal(out=rden[:, a_:a_ + 1], in_=acc_sb[:, a_, 128:129]),
                         reads=[('acc_sb', a_)], writes=['rden'])
                    S.op('dve', lambda e, a_=a_, tl=tl, h=h: e.tensor_scalar(out=M[:, tl, h * 128:(h + 1) * 128], in0=acc_sb[:, a_, 0:128],
                                                                            scalar1=rden[:, a_:a_ + 1], scalar2=None, op0=ALU.mult),
                         reads=[('acc_sb', a_), 'rden'], writes=[('M', tl)])
    S.barrier()
    if debug:
        S.dma('sp', [(lambda e: e.dma_start(out=dbg['yb'], in_=M.rearrange("p t d -> p (t d)")), [('M', t) for t in range(16)], [])], sem='dbg')
        S.barrier()
    es_a.close()
    es_x2.close()
    if stop == 'ATT':
        S.barrier(); S.build(); return nc

    es_g = ExitStack()
    Wseg = [sb(es_g, "Wseg%d" % i, [128, 8, D], BF16) for i in range(2)]
    rows = sb(es_g, "rows", [128, 4, D], F32)
    lnbt = sb(es_g, "lnbt", [128, D], F32)
    wtmp = sb(es_g, "wtmp", [128, 8, 128], F32)
    wsT = sb(es_g, "wsT", [128, 8, 128], BF16)
    rs = sb(es_g, "rs", [128, 8], F32)
    bsp_t = sb(es_g, "bsp", [128, 8], F32)
    vg2 = [sb(es_g, "vg%d" % i, [128, D], F32) for i in range(2)]
    vhat2 = [sb(es_g, "vhat%d" % i, [128, D], BF16) for i in range(2)]
    sm2 = [sb(es_g, "sm%d" % i, [128, 8], F32) for i in range(2)]
    tmpa = [sb(es_g, "tmpa%d" % i, [128, 512], F32) for i in range(2)]
    tmpb2 = [[sb(es_g, "tmpb%d_%d" % (t, i), [128, 512], F32) for i in range(2)] for t in range(2)]
    M2 = sb(es_g, "M2", [128, NT_OWN, D], BF16)

    S.dma('sp', [
        (lambda e: e.dma_start(out=rows[:, 0, :], in_=rowb[0]), [], ['rows']),
        (lambda e: e.dma_start(out=lnbt, in_=rowb[1]), [], ['lnbt']),
        (lambda e: e.dma_start(out=rows[:, 2, :], in_=rowb[2]), [], []),
        (lambda e: e.dma_start(out=rows[:, 3, :], in_=rowb[3]), [], []),
        (lambda e: e.dma_start(out=bsp_t, in_=bspT), [], ['bsp']),
        (lambda e: e.dma_start(out=wtmp, in_=wsp.rearrange("g t s -> t g s")), [], ['wtmp']),
    ], sem='gconst')
    S.op('dve', lambda e: e.tensor_tensor(out=wtmp, in0=wtmp, in1=cmk_t[:, 1:2, :].to_broadcast([128, 8, 128]), op=ALU.mult),
         reads=['wtmp', 'cmk'], writes=['wtmp'])
    S.op('dve', lambda e: e.reduce_sum(out=rs, in_=wtmp, axis=AX.X), reads=['wtmp'], writes=['rs'])
    for g in range(8):
        S.op('dve', lambda e, g=g: e.tensor_scalar(out=rows[:, 1, g * 128:(g + 1) * 128], in0=lnbt[:, g * 128:(g + 1) * 128],
                                                  scalar1=rs[:, g:g + 1], scalar2=bsp_t[:, g:g + 1], op0=ALU.mult, op1=ALU.add),
             reads=['lnbt', 'rs', 'bsp', 'rows'], writes=['rows'])
    S.dma('sp', [(lambda e: e.dma_start(out=wtmp, in_=wspT.rearrange("g s t -> s g t")), [], ['wtmp'])], sem='gconst')
    S.op('dve', lambda e: e.tensor_tensor(out=wsT, in0=wtmp, in1=cmk_t[:, 2:3, :].to_broadcast([128, 8, 128]), op=ALU.mult),
         reads=['wtmp', 'cmk'], writes=['wsT'])

    seg_cols = {'v': 1024, 'u': 0, 'ga': 5120, 'gb': 6144}
    pend_y = None
    pend_mix = None
    for si, seg in enumerate(['v', 'u', 'ga', 'gb']):
        wbuf = si % 2
        c0 = seg_cols[seg]
        S.dma('pool', [(lambda e, c0=c0, wbuf=wbuf: e.dma_start(out=Wseg[wbuf], in_=w_in[:, c0:c0 + D].rearrange("(k p) c -> p k c", p=128)),
                        [], [('Wseg', wbuf)])], sem='wseg%d' % wbuf)
        for tl in range(NT_OWN):
            pp = (tl % 2) * 2 if seg == 'v' else (tl % 3) * 2
            for half in range(2):
                pb = pp + half
                for kc in range(8):
                    S.op('pe', lambda e, kc=kc, tl=tl, pb=pb, half=half, wbuf=wbuf: e.matmul(
                        ps[pb], lhsT=xs(kc, tl * 128, 128), rhs=Wseg[wbuf][:, kc, half * 512:(half + 1) * 512],
                        start=(kc == 0), stop=(kc == 7)),
                        reads=[('xnT', tl), ('Wseg', wbuf)], writes=[PS[pb]], signal=(kc == 7))
            if seg == 'v':
                vb = tl % 2
                vg, vhat, sm = vg2[vb], vhat2[vb], sm2[vb]
                kvg, kvh, ksm = ('vg', vb), ('vhat', vb), ('sm', vb)
                for half in range(2):
                    pb = pp + half
                    S.op('act', lambda e, half=half, pb=pb, vg=vg, sm=sm: e.activation(out=vg[:, half * 512:(half + 1) * 512], in_=ps[pb], func=AF.Gelu,
                                                                                     accum_out=sm[:, half:half + 1]),
                         reads=[PS[pb]], writes=[kvg, ksm])
                S.op('dve', lambda e, vg=vg, sm=sm: e.scalar_tensor_tensor(out=junk, in0=vg, scalar=1.0, in1=vg, op0=ALU.mult, op1=ALU.mult, accum_out=sm[:, 2:3]),
                     reads=[kvg], writes=['junk', ksm])
                S.op('dve', lambda e, sm=sm: e.tensor_tensor(out=sm[:, 3:4], in0=sm[:, 0:1], in1=sm[:, 1:2], op=ALU.add), reads=[ksm], writes=[ksm])
                S.op('dve', lambda e, sm=sm: e.tensor_scalar(out=sm[:, 3:4], in0=sm[:, 3:4], scalar1=1.0 / D, scalar2=None, op0=ALU.mult), reads=[ksm], writes=[ksm])
                S.op('dve', lambda e, sm=sm: e.tensor_tensor(out=sm[:, 4:5], in0=sm[:, 3:4], in1=sm[:, 3:4], op=ALU.mult), reads=[ksm], writes=[ksm])
                S.op('dve', lambda e, sm=sm: e.scalar_tensor_tensor(out=sm[:, 5:6], in0=sm[:, 2:3], scalar=1.0 / D, in1=sm[:, 4:5], op0=ALU.mult, op1=ALU.subtract),
                     reads=[ksm], writes=[ksm])
                S.op('dve', lambda e, sm=sm: e.tensor_scalar(out=sm[:, 5:6], in0=sm[:, 5:6], scalar1=EPS, scalar2=None, op0=ALU.add), reads=[ksm], writes=[ksm])
                S.op('act', lambda e, sm=sm: e.activation(out=sm[:, 6:7], in_=sm[:, 5:6], func=AF.Sqrt), reads=[ksm], writes=[ksm])
                S.op('dve', lambda e, sm=sm: e.reciprocal(out=sm[:, 7:8], in_=sm[:, 6:7]), reads=[ksm], writes=[ksm])
                S.op('dve', lambda e, vg=vg, vhat=vhat, sm=sm: e.tensor_scalar(out=vhat, in0=vg, scalar1=sm[:, 3:4], scalar2=sm[:, 7:8], op0=ALU.subtract, op1=ALU.mult),
                     reads=[kvg, ksm], writes=[kvh])

                def mix(tl=tl, vhat=vhat, kvh=kvh):
                    for g in range(8):
                        S.op('pe', lambda e, g=g: e.matmul(ps[4 + g // 4][:, (g % 4) * 128:(g % 4 + 1) * 128], lhsT=wsT[:, g, :], rhs=vhat[:, g * 128:(g + 1) * 128],
                                                          start=True, stop=True),
                             reads=['wsT', kvh], writes=[PS[4 + g // 4]], signal=(g % 4 == 3))
                    for half in range(2):
                        S.op('dve', lambda e, half=half: e.tensor_tensor(out=tmpa[half], in0=ps[4 + half], in1=rows[:, 0, half * 512:(half + 1) * 512], op=ALU.mult),
                             reads=[PS[4 + half], 'rows'], writes=[('tmpa', half)])
                        S.op('dve', lambda e, half=half: e.tensor_tensor(out=M2[:, tl, half * 512:(half + 1) * 512], in0=tmpa[half],
                                                                        in1=rows[:, 1, half * 512:(half + 1) * 512], op=ALU.add),
                             reads=[('tmpa', half), 'rows'], writes=[('M2', tl)])
                if pend_mix is not None:
                    pend_mix()
                pend_mix = mix
                if tl == NT_OWN - 1:
                    pend_mix()
                    pend_mix = None
            else:
                tb = tl % 2
                for half in range(2):
                    pb = pp + half
                    if seg == 'u':
                        S.op('act', lambda e, half=half, pb=pb, tb=tb: e.activation(out=tmpb2[tb][half], in_=ps[pb], func=AF.Gelu),
                             reads=[PS[pb]], writes=[('tmpb', tb, half)])
                    else:
                        rsel = 2 if seg == 'ga' else 3
                        S.op('dve', lambda e, half=half, pb=pb, rsel=rsel: e.tensor_tensor(out=tmpa[half], in0=ps[pb], in1=rows[:, rsel, half * 512:(half + 1) * 512], op=ALU.add),
                             reads=[PS[pb], 'rows'], writes=[('tmpa', half)])
                        S.op('act', lambda e, half=half, tb=tb: e.activation(out=tmpb2[tb][half], in_=tmpa[half], func=AF.Sigmoid),
                             reads=[('tmpa', half)], writes=[('tmpb', tb, half)])

                def yst(tl=tl, tb=tb, seg=seg):
                    for half in range(2):
                        cs = slice(half * 512, (half + 1) * 512)
                        if seg in ('u', 'ga'):
                            S.op('dve', lambda e, half=half, cs=cs: e.tensor_tensor(out=M2[:, tl, cs], in0=M2[:, tl, cs], in1=tmpb2[tb][half], op=ALU.mult),
                                 reads=[('tmpb', tb, half), ('M2', tl)], writes=[('M2', tl)])
                        else:
                            S.op('dve', lambda e, half=half, cs=cs: e.tensor_tensor(out=tmpb2[tb][half], in0=tmpb2[tb][half], in1=M[:, tl, cs], op=ALU.mult),
                                 reads=[('tmpb', tb, half), ('M', tl)], writes=[('tmpb', tb, half)])
                            S.op('dve', lambda e, half=half, cs=cs: e.tensor_tensor(out=M[:, tl, cs], in0=tmpb2[tb][half], in1=M2[:, tl, cs], op=ALU.add),
                                 reads=[('tmpb', tb, half), ('M2', tl)], writes=[('M', tl)])
                if pend_y is not None:
                    pend_y()
                pend_y = yst
                if tl == NT_OWN - 1:
                    pend_y()
                    pend_y = None
    S.barrier()
    if debug:
        S.dma('sp', [(lambda e: e.dma_start(out=dbg['M'], in_=M.rearrange("p t d -> p (t d)")), [('M', t) for t in range(16)], [])], sem='dbg')
        S.barrier()
    es_g.close()
    es_x.close()
    if stop == 'G':
        S.barrier(); S.build(); return nc

    es_h = ExitStack()
    H = sb(es_h, "H", [128, NT_OWN, D], F32)
    COMB = sb(es_h, "COMB", [128, NT_OWN, NE], F32)
    gf_t = sb(es_h, "gf", [128, D], F32)
    MselF = sb(es_h, "MselF", [128, NT_OWN, NE], F32)
    MselB = sb(es_h, "MselB", [128, NT_OWN, NE], BF16)
    RANK = sb(es_h, "RANK", [128, NT_OWN, NE], F32)
    POSI = sb(es_h, "POSI", [128, NT_OWN, 2], I32)
    CW2 = sb(es_h, "CW2", [128, NT_OWN, 2], F32)
    IDXW = sb(es_h, "IDXW", [128, NTILE], I32)
    cst_t = sb(es_h, "cst", [128, 128], F32)
    es_o = ExitStack()
    xn2T = sb(es_o, "xn2T", [128, 8, TOWN], BF16)
    ub_t = sb(es_o, "ub", [128, 256], BF16)
    tri32_t = sb(es_o, "tri32", [32, 32], BF16)
    cnt = sb(es_o, "cnt", [128, NE], F32)
    tle = sb(es_o, "tle", [128, NE], F32)
    tlb = sb(es_o, "tlb", [128, NE], BF16)
    tT = sb(es_o, "tT", [32, 128], BF16)
    start = sb(es_o, "start", [128, NE], F32)
    start128 = sb(es_o, "start128", [128, NE], F32)
    ej = sb(es_o, "ej", [128, NTILE], F32)
    ej2 = sb(es_o, "ej2", [128, NTILE], F32)
    tmp32 = sb(es_o, "tmp32", [128, NE], F32)
    p8 = sb(es_o, "p8", [128, 8], F32)
    Wout = sb(es_o, "Wout", [128, 8, D], BF16)
    MT = [sb(es_o, "MT%d" % i, [128, 8, 128], BF16) for i in range(2)]
    Wr = sb(es_o, "Wr", [128, 8, 36], BF16)
    brb_t = sb(es_o, "brb", [128, 36], F32)
    lg2 = [sb(es_o, "lg%d" % i, [128, 36], F32) for i in range(2)]
    r_2 = [sb(es_o, "r_%d" % i, [128, 8], F32) for i in range(2)]
    oh2 = [sb(es_o, "oh%d" % i, [128, 4], F32) for i in range(2)]
    ge2 = [sb(es_o, "ge%d" % i, [128, 4], F32) for i in range(2)]
    elm2 = [sb(es_o, "elm%d" % i, [128, 32], F32) for i in range(2)]
    t82 = [sb(es_o, "t8%d" % i, [128, 8], F32) for i in range(2)]
    pe_2 = [sb(es_o, "pe_%d" % i, [128, 32], F32) for i in range(2)]
    pm2 = [sb(es_o, "pm%d" % i, [128, 32], F32) for i in range(2)]

    S.dma('pool', [
        (lambda e: e.dma_start(out=Wout, in_=w_out.rearrange("(k p) c -> p k c", p=128)), [], ['Wout']),
        (lambda e: e.dma_start(out=Wr, in_=wr.rearrange("(k p) c -> p k c", p=128)), [], ['Wr']),
    ], sem='wout')
    S.dma('sp', [
        (lambda e: e.dma_start(out=brb_t, in_=brb), [], ['brb']),
        (lambda e: e.dma_start(out=gf_t, in_=rowb[4]), [], ['gf']),
        (lambda e: e.dma_start(out=ub_t, in_=ub_d), [], ['ub']),
        (lambda e: e.dma_start(out=tri32_t, in_=tri32_d), [], ['tri32']),
        (lambda e: e.dma_start(out=cst_t, in_=cst_d), [], ['cst']),
    ], sem='oconst')
    def o_T1(tl):
        b = tl % 2
        pp = (tl % 2) * 2
        for kc in range(8):
            S.op('pe', lambda e, kc=kc, tl=tl, b=b: e.transpose(out=pt[b][:, kc * 128:(kc + 1) * 128], in_=M[:, tl, kc * 128:(kc + 1) * 128], identity=ident),
                 reads=[('M', tl), 'ident'], writes=[PT[b]], signal=(kc == 7))
        S.op('act', lambda e, b=b: e.activation(out=MT[b], in_=pt[b].rearrange("p (k t) -> p k t", k=8), func=AF.Copy),
             reads=[PT[b]], writes=[('MT', b)])
        S.dma('sp', [(lambda e, tl=tl, b=b: e.dma_start(out=xin[b], in_=xa[tl * 128:(tl + 1) * 128, :]), [], [('xin', b)])], sem='xin%d' % b)
        for half in range(2):
            pb = pp + half
            for kc in range(8):
                S.op('pe', lambda e, kc=kc, b=b, pb=pb, half=half: e.matmul(ps[pb], lhsT=MT[b][:, kc, :], rhs=Wout[:, kc, half * 512:(half + 1) * 512],
                                                                            start=(kc == 0), stop=(kc == 7)),
                     reads=[('MT', b), 'Wout'], writes=[PS[pb]], signal=(kc == 7))
            S.op('dve', lambda e, tl=tl, half=half, pb=pb, b=b: e.tensor_tensor(out=H[:, tl, half * 512:(half + 1) * 512], in0=ps[pb],
                                                                                 in1=xin[b][:, half * 512:(half + 1) * 512], op=ALU.add),
                 reads=[PS[pb], ('xin', b)], writes=[('H', tl)])
    def o_P(tl):
        b = tl % 2
        rms_tile(H[:, tl, :], [('H', tl)], b, 8, xn2T, ('xn2T', tl), tl * 128, xb_dst=M[:, tl, :], xb_key=('M', tl), stage='P')

    def o_Q(tl):
        b = tl % 2
        rms_tile(H[:, tl, :], [('H', tl)], b, 8, xn2T, ('xn2T', tl), tl * 128, xb_dst=M[:, tl, :], xb_key=('M', tl), stage='Q')

    def o_R(tl):
        b = tl % 2
        lg = lg2[tl % 2]
        r_ = r_2[tl % 2]
        oh = oh2[tl % 2]
        ge = ge2[tl % 2]
        elm = elm2[tl % 2]
        t8 = t82[tl % 2]
        pe_ = pe_2[tl % 2]
        pm = pm2[tl % 2]
        for kc in range(8):
            S.op('pe', lambda e, kc=kc, tl=tl: e.matmul(ps[4][:, 0:36], lhsT=xn2T[:, kc, tl * 128:(tl + 1) * 128], rhs=Wr[:, kc, :],
                                                        start=(kc == 0), stop=(kc == 7)),
                 reads=[('xn2T', tl), 'Wr'], writes=[PS[4]], signal=(kc == 7))
        S.op('dve', lambda e: e.tensor_tensor(out=lg, in0=ps[4][:, 0:36], in1=brb_t, op=ALU.add), reads=[PS[4], 'brb'], writes=[('lg', tl % 2)])
        S.op('dve', lambda e: e.reduce_max(out=r_[:, 0:1], in_=lg[:, 0:4], axis=AX.X), reads=[('lg', tl % 2)], writes=[('r_', tl % 2)])
        S.op('dve', lambda e: e.tensor_scalar(out=r_[:, 1:2], in0=r_[:, 0:1], scalar1=-1.0, scalar2=None, op0=ALU.mult), reads=[('r_', tl % 2)], writes=[('r_', tl % 2)])
        S.op('act', lambda e: e.activation(out=ge, in_=lg[:, 0:4], func=AF.Exp, bias=r_[:, 1:2], accum_out=r_[:, 2:3]),
             reads=[('lg', tl % 2), ('r_', tl % 2)], writes=[('ge', tl % 2), ('r_', tl % 2)])
        S.op('dve', lambda e: e.tensor_scalar(out=oh, in0=lg[:, 0:4], scalar1=r_[:, 0:1], scalar2=None, op0=ALU.is_ge), reads=[('lg', tl % 2), ('r_', tl % 2)], writes=[('oh', tl % 2)])
        S.op('dve', lambda e: e.tensor_scalar(out=oh, in0=oh, scalar1=BIGG, scalar2=-BIGG, op0=ALU.mult, op1=ALU.add), reads=[('oh', tl % 2)], writes=[('oh', tl % 2)])
        S.op('dve', lambda e: e.tensor_tensor(out=elm.rearrange("p (g x) -> p g x", g=4), in0=lg[:, 4:36].rearrange("p (g x) -> p g x", g=4),
                                              in1=oh.unsqueeze(2).to_broadcast([128, 4, 8]), op=ALU.add),
             reads=[('lg', tl % 2), ('oh', tl % 2)], writes=[('elm', tl % 2)])
        S.op('dve', lambda e: e.max(out=t8, in_=elm), reads=[('elm', tl % 2)], writes=[('t8', tl % 2)])
        S.op('dve', lambda e: e.tensor_scalar(out=r_[:, 6:7], in0=t8[:, 0:1], scalar1=-1.0, scalar2=None, op0=ALU.mult), reads=[('t8', tl % 2), ('r_', tl % 2)], writes=[('r_', tl % 2)])
        S.op('act', lambda e: e.activation(out=pe_, in_=elm, func=AF.Exp, bias=r_[:, 6:7]), reads=[('elm', tl % 2), ('r_', tl % 2)], writes=[('pe_', tl % 2)])
        S.op('dve', lambda e: e.scalar_tensor_tensor(out=pm, in0=elm, scalar=t8[:, 1:2], in1=pe_, op0=ALU.is_ge, op1=ALU.mult, accum_out=r_[:, 3:4]),
             reads=[('elm', tl % 2), ('t8', tl % 2), ('pe_', tl % 2), ('r_', tl % 2)], writes=[('pm', tl % 2), ('r_', tl % 2)])
        S.op('dve', lambda e: e.tensor_tensor(out=r_[:, 4:5], in0=r_[:, 3:4], in1=r_[:, 2:3], op=ALU.mult), reads=[('r_', tl % 2)], writes=[('r_', tl % 2)])
        S.op('dve', lambda e: e.reciprocal(out=r_[:, 5:6], in_=r_[:, 4:5]), reads=[('r_', tl % 2)], writes=[('r_', tl % 2)])
        S.op('dve', lambda e, tl=tl: e.tensor_scalar(out=COMB[:, tl, :], in0=pm, scalar1=r_[:, 5:6], scalar2=None, op0=ALU.mult),
             reads=[('pm', tl % 2), ('r_', tl % 2)], writes=[('COMB', tl)])
        S.op('dve', lambda e, tl=tl: e.tensor_scalar(out=MselF[:, tl, :], in0=elm, scalar1=t8[:, 1:2], scalar2=None, op0=ALU.is_ge),
             reads=[('elm', tl % 2), ('t8', tl % 2)], writes=[('MselF', tl)])
        S.op('dve', lambda e, tl=tl: e.tensor_copy(out=MselB[:, tl, :], in_=MselF[:, tl, :]), reads=[('MselF', tl)], writes=[('MselB', tl)])
    o_T1(0)
    o_P(0)
    for tl in range(NT_OWN):
        if tl + 1 < NT_OWN:
            o_T1(tl + 1)
        o_Q(tl)
        if tl + 1 < NT_OWN:
            o_P(tl + 1)
        o_R(tl)
    allMB = [('MselB', t) for t in range(NT_OWN)]
    for tl in range(NT_OWN):
        pb = 4 + tl % 2
        for j in range(tl):
            S.op('pe', lambda e, j=j, pb=pb: e.matmul(ps[pb][:, 0:NE], lhsT=ub_t[:, 128:256], rhs=MselB[:, j, :], start=(j == 0), stop=False),
                 reads=['ub', ('MselB', j)], writes=[PS[pb]], signal=False)
        S.op('pe', lambda e, tl=tl, pb=pb: e.matmul(ps[pb][:, 0:NE], lhsT=ub_t[:, 0:128], rhs=MselB[:, tl, :], start=(tl == 0), stop=True),
             reads=['ub', ('MselB', tl)], writes=[PS[pb]])
        S.op('dve', lambda e, tl=tl, pb=pb: e.tensor_copy(out=RANK[:, tl, :], in_=ps[pb][:, 0:NE]), reads=[PS[pb]], writes=[('RANK', tl)])
    for j in range(NT_OWN):
        S.op('pe', lambda e, j=j: e.matmul(ps[0][:, 0:NE], lhsT=ub_t[:, 128:256], rhs=MselB[:, j, :], start=(j == 0), stop=(j == NT_OWN - 1)),
             reads=['ub', ('MselB', j)], writes=[PS[0]], signal=(j == NT_OWN - 1))
    S.op('dve', lambda e: e.tensor_copy(out=cnt, in_=ps[0][:, 0:NE]), reads=[PS[0]], writes=['cnt'])
    S.op('dve', lambda e: e.memset(tle, 0.0), writes=['tle'])
    for m in range(16):
        S.op('dve', lambda e, m=m: e.scalar_tensor_tensor(out=tle, in0=cnt, scalar=float(128 * m), in1=tle, op0=ALU.is_gt, op1=ALU.add),
             reads=['cnt', 'tle'], writes=['tle'])
    S.op('dve', lambda e: e.tensor_copy(out=tlb, in_=tle), reads=['tle'], writes=['tlb'])
    S.op('pe', lambda e: e.transpose(out=pt[0][0:32, 0:128], in_=tlb, identity=ident), reads=['tlb', 'ident'], writes=[PT[0]])
    S.op('dve', lambda e: e.tensor_copy(out=tT, in_=pt[0][0:32, 0:128]), reads=[PT[0]], writes=['tT'])
    S.op('pe', lambda e: e.matmul(ps[1][:, 0:NE], lhsT=tT, rhs=tri32_t, start=True, stop=True), reads=['tT', 'tri32'], writes=[PS[1]])
    S.op('dve', lambda e: e.tensor_copy(out=start, in_=ps[1][:, 0:NE]), reads=[PS[1]], writes=['start'])
    S.op('dve', lambda e: e.tensor_scalar(out=start128, in0=start, scalar1=128.0, scalar2=1.0, op0=ALU.mult, op1=ALU.add),
         reads=['start'], writes=['start128'])
    S.op('dve', lambda e: e.memset(ej, -1.0), writes=['ej'])
    for ex in range(NE):
        S.op('dve', lambda e, ex=ex: e.scalar_tensor_tensor(out=ej, in0=cst_t[:, 0:NTILE], scalar=start[:, ex:ex + 1], in1=ej, op0=ALU.is_ge, op1=ALU.add),
             reads=['cst', 'start', 'ej'], writes=['ej'])
    S.op('dve', lambda e: e.tensor_scalar(out=ej, in0=ej, scalar1=128.0, scalar2=cst_t[:, 64:65], op0=ALU.mult, op1=ALU.add),
         reads=['ej', 'cst'], writes=['ej'])
    S.op('dve', lambda e: e.tensor_tensor(out=tmp32[:, 0:1], in0=start[:, NE - 1:NE], in1=tle[:, NE - 1:NE], op=ALU.add),
         reads=['start', 'tle'], writes=['tmp32'])
    S.op('dve', lambda e: e.tensor_scalar(out=ej2, in0=cst_t[:, 0:NTILE], scalar1=tmp32[:, 0:1], scalar2=float(OOB_ROW), op0=ALU.is_ge, op1=ALU.mult),
         reads=['cst', 'tmp32'], writes=['ej2'])
    S.op('dve', lambda e: e.tensor_tensor(out=IDXW, in0=ej, in1=ej2, op=ALU.add), reads=['ej', 'ej2'], writes=['IDXW'])
    for tl in range(NT_OWN):
        S.op('dve', lambda e, tl=tl: e.tensor_tensor(out=RANK[:, tl, :], in0=RANK[:, tl, :], in1=start128, op=ALU.add),
             reads=[('RANK', tl), 'start128'], writes=[('RANK', tl)])
        S.op('dve', lambda e, tl=tl: e.tensor_tensor(out=RANK[:, tl, :], in0=RANK[:, tl, :], in1=MselF[:, tl, :], op=ALU.mult),
             reads=[('RANK', tl), ('MselF', tl)], writes=[('RANK', tl)])
        S.op('dve', lambda e, tl=tl: e.max(out=p8, in_=RANK[:, tl, :]), reads=[('RANK', tl)], writes=['p8'])
        for k in range(2):
            S.op('dve', lambda e, tl=tl, k=k: e.scalar_tensor_tensor(out=tmp32, in0=RANK[:, tl, :], scalar=p8[:, k:k + 1], in1=COMB[:, tl, :],
                                                                    op0=ALU.is_equal, op1=ALU.mult, accum_out=CW2[:, tl, k:k + 1]),
                 reads=[('RANK', tl), 'p8', ('COMB', tl)], writes=['tmp32', ('CW2', tl)])
        S.op('dve', lambda e, tl=tl: e.tensor_scalar(out=POSI[:, tl, :], in0=p8[:, 0:2], scalar1=-1.0, scalar2=None, op0=ALU.add),
             reads=['p8'], writes=[('POSI', tl)])
        for k in range(2):
            S.dma('pool', [(lambda e, tl=tl, k=k: e.indirect_dma_start(out=xs_d, out_offset=bass.IndirectOffsetOnAxis(ap=POSI[:, tl, k:k + 1], axis=0),
                                                                       in_=M[:, tl, :], in_offset=None),
                            [('POSI', tl), ('M', tl)] + [('xs_zero', a) for a in range(NTILE)], [('xs_sc', tl, k)])], sem='sc')
    all_sc = [('xs_sc', tl, k) for tl in range(NT_OWN) for k in range(2)]
    S.barrier()
    if debug:
        S.dma('sp', [(lambda e: e.dma_start(out=dbg['H'], in_=H.rearrange("p t d -> p (t d)")), [('H', t) for t in range(16)], []),
                     (lambda e: e.dma_start(out=dbg['comb'], in_=COMB.rearrange("p t d -> p (t d)")), [('COMB', t) for t in range(16)], [])], sem='dbg')
        S.barrier()
    es_o.close()
    if stop == 'O':
        S.barrier(); S.build(); return nc

    es_m = ExitStack()
    W13g = [sb(es_m, "W13g%d" % i, [128, 8 * 512], BF16) for i in range(3)]
    W2g = [sb(es_m, "W2g%d" % i, [128, 2 * D], BF16) for i in range(3)]
    XS = [sb(es_m, "XS%d" % i, [128, D], BF16) for i in range(4)]
    xsT = [sb(es_m, "xsT%d" % i, [128, 8, 128], BF16) for i in range(2)]
    sa = [sb(es_m, "sa%d" % i, [128, 256], F32) for i in range(2)]
    hid = [sb(es_m, "hid%d" % i, [128, 256], BF16) for i in range(2)]
    hidT = [sb(es_m, "hidT%d" % i, [128, 2, 128], BF16) for i in range(2)]
    yt = [sb(es_m, "yt%d" % i, [128, D], F32) for i in range(2)]

    bcreg = {}
    all_wcast = [('wcast', ex_) for ex_ in range(NE)]

    def _mk_bcreg(e):
        bcreg['r'] = e.to_reg(NE * 128 - 1)
        return None
    S.ops['pool'].append(([], _mk_bcreg, None, 0))

    def moeA_pre(j):
        b = j % 2
        wb = j % 3
        S.dma('pool', [
            (lambda e, j=j, wb=wb: e.indirect_dma_start(out=W13g[wb], out_offset=None, in_=w13b,
                                                        in_offset=bass.IndirectOffsetOnAxis(ap=IDXW[:, j:j + 1], axis=0),
                                                        bounds_check=bcreg['r'], oob_is_err=False), ['IDXW'] + all_wcast, [('W13g', wb)]),
            (lambda e, j=j, wb=wb: e.indirect_dma_start(out=W2g[wb], out_offset=None, in_=w2b,
                                                        in_offset=bass.IndirectOffsetOnAxis(ap=IDXW[:, j:j + 1], axis=0),
                                                        bounds_check=bcreg['r'], oob_is_err=False), ['IDXW'], [('W2g', wb)]),
        ], sem='wg%d' % wb)
        xb4 = j % 4
        for kc in range(8):
            S.op('pe', lambda e, kc=kc, b=b, xb4=xb4: e.transpose(out=pt[b][:, kc * 128:(kc + 1) * 128], in_=XS[xb4][:, kc * 128:(kc + 1) * 128], identity=ident),
                 reads=[('XS', xb4), 'ident'], writes=[PT[b]], signal=(kc == 7))
        S.op('dve', lambda e, b=b: e.tensor_tensor(out=xsT[b], in0=pt[b].rearrange("p (k t) -> p k t", k=8),
                                                   in1=gT_t[:, 8:16].unsqueeze(2).to_broadcast([128, 8, 128]), op=ALU.mult),
             reads=[PT[b], 'gT'], writes=[('xsT', b)])

    def moeA_mm(j):
        b = j % 2
        wb = j % 3
        for kc in range(8):
            S.op('pe', lambda e, kc=kc, b=b, wb=wb: e.matmul(ps[b], lhsT=xsT[b][:, kc, :], rhs=W13g[wb][:, kc * 512:(kc + 1) * 512], start=(kc == 0), stop=(kc == 7)),
                 reads=[('xsT', b), ('W13g', wb)], writes=[PS[b]], signal=(kc == 7))
        S.op('act', lambda e, b=b: e.activation(out=sa[b], in_=ps[b][:, 0:256], func=AF.Silu), reads=[PS[b]], writes=[('sa', b)])
        S.op('dve', lambda e, b=b: e.tensor_tensor(out=hid[b], in0=sa[b], in1=ps[b][:, 256:512], op=ALU.mult),
             reads=[('sa', b), PS[b]], writes=[('hid', b)])

    def moeB(j):
        b = j % 2
        wb = j % 3
        for ft in range(2):
            S.op('pe', lambda e, ft=ft, b=b: e.transpose(out=pt[b][:, ft * 128:(ft + 1) * 128], in_=hid[b][:, ft * 128:(ft + 1) * 128], identity=ident),
                 reads=[('hid', b), 'ident'], writes=[PT[b]], signal=(ft == 1))
        S.op('act', lambda e, b=b: e.activation(out=hidT[b], in_=pt[b][:, 0:256].rearrange("p (f t) -> p f t", f=2), func=AF.Copy),
             reads=[PT[b]], writes=[('hidT', b)])
        for half in range(2):
            pb = 2 + 2 * b + half
            for ft in range(2):
                S.op('pe', lambda e, ft=ft, half=half, b=b, pb=pb, wb=wb: e.matmul(
                    ps[pb], lhsT=hidT[b][:, ft, :], rhs=W2g[wb][:, ft * D + half * 512:ft * D + (half + 1) * 512], start=(ft == 0), stop=(ft == 1)),
                    reads=[('hidT', b), ('W2g', wb)], writes=[PS[pb]], signal=(ft == 1))
            if half == 0:
                S.op('act', lambda e, b=b, pb=pb: e.activation(out=yt[b][:, 0:512], in_=ps[pb], func=AF.Copy), reads=[PS[pb]], writes=[('yt', b)])
            else:
                S.op('dve', lambda e, b=b, pb=pb: e.tensor_copy(out=yt[b][:, 512:1024], in_=ps[pb]), reads=[PS[pb]], writes=[('yt', b)])
        S.dma('sp', [(lambda e, j=j, b=b: e.dma_start(out=outs_d[j * 128:(j + 1) * 128, :], in_=yt[b]), [('yt', b)], [('outs', j)])], sem='ost')

    def xsload(j):
        xb4 = j % 4
        S.dma('act', [(lambda e, j=j, xb4=xb4: e.dma_start(out=XS[xb4], in_=xs_d[j * 128:(j + 1) * 128, :]), all_sc, [('XS', xb4)])], sem='xsl%d' % xb4)

    for j in range(3):
        xsload(j)
    moeA_pre(0)
    moeA_mm(0)
    for j in range(NTILE):
        if j + 3 < NTILE:
            xsload(j + 3)
        if j + 1 < NTILE:
            moeA_pre(j + 1)
        moeB(j)
        if j + 1 < NTILE:
            moeA_mm(j + 1)
    if os.environ.get('SBUFDBG'):
        print("sbuf remaining after MoE alloc", nc.sbuf_bytes_remaining)
    all_outs = [('outs', j) for j in range(NTILE)]

    O12 = [sb(es_m, "O12_%d" % i, [128, 2, D], F32) for i in range(2)]
    for tl in range(NT_OWN):
        b = tl % 2
        S.dma('pool', [
            (lambda e, tl=tl, b=b, k=k: e.indirect_dma_start(out=O12[b][:, k, :], out_offset=None, in_=outs_d,
                                                             in_offset=bass.IndirectOffsetOnAxis(ap=POSI[:, tl, k:k + 1], axis=0)),
             all_outs + [('POSI', tl)], [('O12', b)]) for k in range(2)], sem='og%d' % b)
        for k in range(2):
            S.op('dve', lambda e, tl=tl, b=b, k=k: e.scalar_tensor_tensor(out=H[:, tl, :], in0=O12[b][:, k, :], scalar=CW2[:, tl, k:k + 1], in1=H[:, tl, :],
                                                                         op0=ALU.mult, op1=ALU.add),
                 reads=[('O12', b), ('CW2', tl), ('H', tl)], writes=[('H', tl)])
        s_ = st[b]
        sk = ('st', b)
        S.op('dve', lambda e, tl=tl, s_=s_: e.scalar_tensor_tensor(out=junk, in0=H[:, tl, :], scalar=1.0, in1=H[:, tl, :], op0=ALU.mult, op1=ALU.mult,
                                                                  accum_out=s_[:, 0:1]), reads=[('H', tl)], writes=['junk', sk])
        S.op('dve', lambda e, s_=s_: e.tensor_scalar(out=s_[:, 1:2], in0=s_[:, 0:1], scalar1=1.0 / D, scalar2=EPS, op0=ALU.mult, op1=ALU.add),
             reads=[sk], writes=[sk])
        S.op('act', lambda e, s_=s_: e.activation(out=s_[:, 3:4], in_=s_[:, 1:2], func=AF.Sqrt), reads=[sk], writes=[sk])
        S.op('dve', lambda e, s_=s_: e.reciprocal(out=s_[:, 2:3], in_=s_[:, 3:4]), reads=[sk], writes=[sk])
        S.op('dve', lambda e, tl=tl, s_=s_, b=b: e.scalar_tensor_tensor(out=xin[b], in0=H[:, tl, :], scalar=s_[:, 2:3], in1=gf_t, op0=ALU.mult, op1=ALU.mult),
             reads=[('H', tl), sk, 'gf'], writes=[('xin', b)])
        S.dma('sp', [(lambda e, tl=tl, b=b: e.dma_start(out=out[tl * 128:(tl + 1) * 128, :], in_=xin[b]), [('xin', b)], [])], sem='out')
    S.barrier()
    S.build()
    return nc


def _t5_bucket_np(n):
    n = np.maximum(n, 0)
    nf = np.maximum(n, 16).astype(np.float32)
    large = 16 + (np.log(nf / np.float32(16)) / np.float32(math.log(128 / 16)) * np.float32(16)).astype(np.int32)
    large = np.minimum(large, 31)
    return np.where(n < 16, n, large)


_PROG = {}


def _get_prog(debug=False):
    if debug not in _PROG:
        _PROG[debug] = build_program(debug)
    return _PROG[debug]


def make_in_maps(x, norm_mix_g, w_in, b_gates, gmlp_ln_g, gmlp_ln_b, w_spatial, b_spatial, rel_bias,
                 w_out, norm_ffn_g, w_group_router, b_group_router, w_expert_router, b_expert_router,
                 w1, w3, w2, norm_final_g):
    f = lambda a: np.ascontiguousarray(np.asarray(a), dtype=np.float32)
    x = f(x)
    w_in0 = f(w_in[0]); w_out0 = f(w_out[0]); w1_0 = f(w1[0]); w3_0 = f(w3[0]); w2_0 = f(w2[0])
    w13 = np.concatenate([w1_0, w3_0], axis=2).reshape(NE, 8, 128, 512)
    w13r = np.ascontiguousarray(np.transpose(w13, (0, 2, 1, 3))).reshape(NE * 128, 8 * 512)
    w2r = np.ascontiguousarray(np.transpose(w2_0.reshape(NE, 2, 128, D), (0, 2, 1, 3))).reshape(NE * 128, 2 * D)
    ub = np.concatenate([np.triu(np.ones((128, 128), np.float32), 1), np.ones((128, 128), np.float32)], axis=1).astype(ml_dtypes.bfloat16)
    tri32 = np.triu(np.ones((32, 32), np.float32), 1).astype(ml_dtypes.bfloat16)
    cst = np.zeros((128, 128), np.float32)
    cst[:, 0:64] = np.arange(64, dtype=np.float32)[None, :]
    cst[:, 64] = np.arange(128, dtype=np.float32)
    cst[:, 65:81] = (128.0 * np.arange(16, dtype=np.float32))[None, :]
    wr = np.concatenate([f(w_group_router[0]), np.transpose(f(w_expert_router[0]), (1, 0, 2)).reshape(D, 32)], axis=1)
    br = np.concatenate([f(b_group_router[0]), f(b_expert_router[0]).reshape(32)])
    brb = np.ascontiguousarray(np.broadcast_to(br[None, :], (128, 36)))
    gT = np.concatenate([f(norm_mix_g[0]).reshape(8, 128).T, f(norm_ffn_g[0]).reshape(8, 128).T], axis=1)
    bg = f(b_gates[0])
    rows = [f(gmlp_ln_g[0]), f(gmlp_ln_b[0]), bg[:D], bg[D:], f(norm_final_g), np.zeros(D, np.float32)]
    rowb = np.ascontiguousarray(np.stack([np.broadcast_to(r[None, :], (128, D)) for r in rows]))
    wsp = f(w_spatial[0])
    wspT = np.ascontiguousarray(np.transpose(wsp, (0, 2, 1)))
    bspT = np.ascontiguousarray(f(b_spatial[0]).T)
    rb = f(rel_bias)
    kk = np.arange(128)[:, None]
    qq = np.arange(128)[None, :]
    b0 = _t5_bucket_np(qq - kk)
    b1 = _t5_bucket_np(qq - kk + 128)
    rb0 = np.ascontiguousarray(np.stack([rb[b0, h] for h in range(8)]))
    rb1 = np.ascontiguousarray(np.stack([rb[b1, h] for h in range(8)]))
    chb = np.ascontiguousarray(np.broadcast_to(rb[31][None, :], (128, 8)))
    causal_add = np.where(qq >= kk, 0.0, -BIGM).astype(np.float32)
    tril = (np.arange(128)[None, :] <= np.arange(128)[:, None]).astype(np.float32)
    triu = np.ascontiguousarray(tril.T)
    cmk = np.ascontiguousarray(np.stack([causal_add, tril, triu]))
    ident = np.eye(128, dtype=np.float32).astype(ml_dtypes.bfloat16)
    e16 = np.zeros((16, 16, 128), np.float32)
    for s in range(16):
        e16[s, s, :] = 1.0
    e16 = e16.reshape(16, 16 * 128).astype(ml_dtypes.bfloat16)
    in_maps = []
    for c in range(8):
        b, p = c // 2, c % 2
        own = x[b, p * TOWN:(p + 1) * TOWN]
        oth = x[b, (1 - p) * TOWN:(2 - p) * TOWN]
        xa = np.ascontiguousarray(np.concatenate([own, oth], axis=0))
        pnm = np.full((16, 16), -BIGG, np.float32)
        for qt in range(16):
            i = qt // 2
            pnm[qt, :i] = 0.0
            if p == 1:
                pnm[qt, 8:] = 0.0
        pn = np.ascontiguousarray(np.broadcast_to(pnm.reshape(1, 256), (128, 256)))
        in_maps.append({
            "xa": xa, "w_in": w_in0, "w_out": w_out0, "w13r": w13r, "w2r": w2r, "ub": ub, "tri32": tri32, "cst": cst, "wr": np.ascontiguousarray(wr),
            "pn": pn, "gT": np.ascontiguousarray(gT), "rowb": rowb, "brb": brb, "wsp": wsp, "wspT": wspT, "bspT": bspT,
            "rb0": rb0, "rb1": rb1, "chb": chb, "cmk": cmk, "ident": ident, "e16": e16,
        })
    return in_maps


def kernel(**inputs):
    nc = _get_prog(False)
    in_maps = make_in_maps(**inputs)
    res = run_bass_kernel_spmd(nc, in_maps, core_ids=list(range(8)))
    outp = np.empty((4, SEQ, D), np.float32)
    for c in range(8):
        b, p = c // 2, c % 2
        outp[b, p * TOWN:(p + 1) * TOWN] = np.asarray(res.results[c]["out"], dtype=np.float32)
    return outp
```
